# Optimizing a Trainium2 kernel written in Bass

```python
import jax, jax.numpy as jnp
from jax import lax
import numpy as np

D_MODEL = 1024
BATCH = 8
SEQ = 2048
DEPTH = 1

N_HEADS_MLA = 8
QK_NOPE_DIM = 64
QK_ROPE_DIM = 32
V_HEAD_DIM = 64
Q_LORA_RANK = 256
KV_LORA_RANK = 128
QK_HEAD_DIM = QK_NOPE_DIM + QK_ROPE_DIM
ROPE_THETA = 10000.0
Q_BLOCK = 128
MLA_WIDTH = N_HEADS_MLA * V_HEAD_DIM

N_FOURIER_GROUPS = 8
FOURIER_GROUP_DIM = 64
FOURIER_WIDTH = N_FOURIER_GROUPS * FOURIER_GROUP_DIM

N_EXPERTS = 256
TOP_K = 8
EXPERT_DIM = 256
SHARED_DIM = 256
ROUTED_SCALE = 2.5
EXPERT_BLOCK = 128

EPS = 1e-6
N_ADA = 6

IN_SPLITS = (Q_LORA_RANK, KV_LORA_RANK, QK_ROPE_DIM, FOURIER_WIDTH, D_MODEL, D_MODEL)
IN_WIDTH = sum(IN_SPLITS)
IN_OFFSETS = tuple(int(o) for o in np.cumsum(IN_SPLITS)[:-1])

kernel_name = "hybrid_mla_fnet_moe_adaln_block"


def rms_norm(x, g):
    xf = x.astype(jnp.float32)
    y = xf * lax.rsqrt(jnp.mean(xf * xf, axis=-1, keepdims=True) + EPS)
    return (y * g.astype(jnp.float32)).astype(x.dtype)


def rope_tables(seq_len):
    pos = jnp.arange(seq_len, dtype=jnp.float32)
    inv_freq = ROPE_THETA ** (-jnp.arange(0, QK_ROPE_DIM, 2, dtype=jnp.float32) / QK_ROPE_DIM)
    ang = pos[:, None] * inv_freq[None, :]
    return jnp.cos(ang)[None, :, None, :], jnp.sin(ang)[None, :, None, :]


def apply_rope_tail(t, cos, sin):
    nope, rot = t[..., :QK_NOPE_DIM], t[..., QK_NOPE_DIM:]
    r1, r2 = jnp.split(rot, 2, axis=-1)
    cos = cos.astype(t.dtype)
    sin = sin.astype(t.dtype)
    rot = jnp.concatenate([r1 * cos - r2 * sin, r2 * cos + r1 * sin], axis=-1)
    return jnp.concatenate([nope, rot], axis=-1)


def mla_mixer(z_q, z_kv, z_kr, q_a_norm_g, w_uq, kv_a_norm_g, w_ukv, q_norm_g, k_norm_g):
    B, S, _ = z_q.shape
    H = N_HEADS_MLA
    cq = rms_norm(z_q, q_a_norm_g)
    q = (cq @ w_uq).reshape(B, S, H, QK_HEAD_DIM)
    ckv = rms_norm(z_kv, kv_a_norm_g)
    kv = (ckv @ w_ukv).reshape(B, S, H, QK_NOPE_DIM + V_HEAD_DIM)
    k_nope, v = kv[..., :QK_NOPE_DIM], kv[..., QK_NOPE_DIM:]
    k_rope = jnp.broadcast_to(z_kr[:, :, None, :], (B, S, H, QK_ROPE_DIM))
    k = jnp.concatenate([k_nope, k_rope], axis=-1)
    q = rms_norm(q, q_norm_g)
    k = rms_norm(k, k_norm_g)
    cos, sin = rope_tables(S)
    q = apply_rope_tail(q, cos, sin)
    k = apply_rope_tail(k, cos, sin)
    q = q * jnp.asarray(QK_HEAD_DIM ** -0.5, dtype=q.dtype)
    n_blocks = S // Q_BLOCK
    qb = q.reshape(B, n_blocks, Q_BLOCK, H, QK_HEAD_DIM).transpose(1, 0, 2, 3, 4)

    def attend(q_blk):
        s = jnp.einsum('bqhd,bkhd->bhqk', q_blk, k, preferred_element_type=jnp.float32)
        p = jax.nn.softmax(s, axis=-1).astype(v.dtype)
        return jnp.einsum('bhqk,bkhd->bqhd', p, v)

    o = lax.map(attend, qb)
    return o.transpose(1, 0, 2, 3, 4).reshape(B, S, MLA_WIDTH)


def fourier_mixer(z_f):
    B, S, _ = z_f.shape
    zg = z_f.astype(jnp.float32).reshape(B, S, N_FOURIER_GROUPS, FOURIER_GROUP_DIM)
    zf = jnp.fft.fft2(zg, axes=(1, 3), norm='ortho')
    return jnp.real(zf).astype(z_f.dtype).reshape(B, S, FOURIER_WIDTH)


def swiglu(x, w_gate, w_up, w_down):
    return (jax.nn.silu(x @ w_gate) * (x @ w_up)) @ w_down


def moe_ffn(h, w_router, router_bias, w_exp_gate, w_exp_up, w_exp_down,
            w_sh_gate, w_sh_up, w_sh_down):
    B, S, D = h.shape
    T = B * S
    P = T * TOP_K
    n_blk = -(-P // EXPERT_BLOCK) + N_EXPERTS
    hf = h.reshape(T, D)
    scores = jax.nn.sigmoid(hf.astype(jnp.float32) @ w_router.astype(jnp.float32))
    _, idx = lax.top_k(scores + router_bias.astype(jnp.float32)[None, :], TOP_K)
    w_sel = jnp.take_along_axis(scores, idx, axis=-1)
    w_sel = w_sel / jnp.sum(w_sel, axis=-1, keepdims=True) * ROUTED_SCALE

    e_flat = idx.reshape(P).astype(jnp.int32)
    tok_flat = jnp.repeat(jnp.arange(T, dtype=jnp.int32), TOP_K)
    w_flat = w_sel.reshape(P).astype(h.dtype)
    order = jnp.argsort(e_flat, stable=True)
    e_sorted = e_flat[order]
    counts = jnp.bincount(e_flat, length=N_EXPERTS)
    starts = jnp.cumsum(counts) - counts
    padded = ((counts + EXPERT_BLOCK - 1) // EXPERT_BLOCK) * EXPERT_BLOCK
    p_ends = jnp.cumsum(padded)
    p_starts = p_ends - padded
    rank = jnp.arange(P, dtype=jnp.int32) - starts[e_sorted]
    slot = p_starts[e_sorted] + rank
    n_slots = n_blk * EXPERT_BLOCK
    slot_tok = jnp.zeros((n_slots,), jnp.int32).at[slot].set(tok_flat[order])
    slot_w = jnp.zeros((n_slots,), h.dtype).at[slot].set(w_flat[order])
    blk_start = jnp.arange(n_blk, dtype=p_ends.dtype) * EXPERT_BLOCK
    blk_expert = jnp.clip(jnp.searchsorted(p_ends, blk_start, side='right'), 0, N_EXPERTS - 1)

    def run_block(args):
        e, tok, wt = args
        xb = hf[tok]
        yb = swiglu(xb, w_exp_gate[e], w_exp_up[e], w_exp_down[e])
        return yb * wt[:, None]

    ys = lax.map(run_block, (blk_expert, slot_tok.reshape(n_blk, EXPERT_BLOCK),
                             slot_w.reshape(n_blk, EXPERT_BLOCK)))
    routed = jnp.zeros((T, D), h.dtype).at[slot_tok].add(ys.reshape(n_slots, D))
    shared = swiglu(hf, w_sh_gate, w_sh_up, w_sh_down)
    return (routed + shared).reshape(B, S, D)


def setup_inputs(seed: int = 0) -> dict:
    key = jax.random.key(seed)
    ks = iter(jax.random.split(key, 32))
    L, D = DEPTH, D_MODEL

    def dense(shape, fan_in, gain=1.0):
        return jax.random.normal(next(ks), shape, jnp.float32) * (gain * fan_in ** -0.5)

    def norm_gain(shape):
        return 1.0 + 0.1 * jax.random.normal(next(ks), shape, jnp.float32)

    return {
        'x': jax.random.normal(next(ks), (BATCH, SEQ, D), jnp.float32),
        'c': jax.random.normal(next(ks), (BATCH, D), jnp.float32),
        'w_ada': dense((L, D, N_ADA * D), D, 0.5),
        'b_ada': 0.02 * jax.random.normal(next(ks), (L, N_ADA * D), jnp.float32),
        'norm1_g': norm_gain((L, D)),
        'w_in': dense((L, D, IN_WIDTH), D),
        'q_a_norm_g': norm_gain((L, Q_LORA_RANK)),
        'w_uq': dense((L, Q_LORA_RANK, N_HEADS_MLA * QK_HEAD_DIM), Q_LORA_RANK),
        'kv_a_norm_g': norm_gain((L, KV_LORA_RANK)),
        'w_ukv': dense((L, KV_LORA_RANK, N_HEADS_MLA * (QK_NOPE_DIM + V_HEAD_DIM)), KV_LORA_RANK),
        'q_norm_g': norm_gain((L, QK_HEAD_DIM)),
        'k_norm_g': norm_gain((L, QK_HEAD_DIM)),
        'w_proj_attn': dense((L, MLA_WIDTH, D), MLA_WIDTH),
        'w_proj_fourier': dense((L, FOURIER_WIDTH, D), FOURIER_WIDTH),
        'w_out': dense((L, D, D), D),
        'norm2_g': norm_gain((L, D)),
        'w_router': dense((L, D, N_EXPERTS), D),
        'router_bias': 0.01 * jax.random.normal(next(ks), (L, N_EXPERTS), jnp.float32),
        'w_exp_gate': dense((L, N_EXPERTS, D, EXPERT_DIM), D),
        'w_exp_up': dense((L, N_EXPERTS, D, EXPERT_DIM), D),
        'w_exp_down': dense((L, N_EXPERTS, EXPERT_DIM, D), EXPERT_DIM),
        'w_sh_gate': dense((L, D, SHARED_DIM), D),
        'w_sh_up': dense((L, D, SHARED_DIM), D),
        'w_sh_down': dense((L, SHARED_DIM, D), SHARED_DIM),
    }


def reference(x, c, w_ada, b_ada, norm1_g, w_in, q_a_norm_g, w_uq, kv_a_norm_g, w_ukv,
              q_norm_g, k_norm_g, w_proj_attn, w_proj_fourier, w_out, norm2_g,
              w_router, router_bias, w_exp_gate, w_exp_up, w_exp_down,
              w_sh_gate, w_sh_up, w_sh_down):
    for l in range(DEPTH):
        ada = jax.nn.silu(c) @ w_ada[l] + b_ada[l]
        sh1, sc1, g1, sh2, sc2, g2 = jnp.split(ada[:, None, :], N_ADA, axis=-1)

        h = rms_norm(x, norm1_g[l]) * (1 + sc1) + sh1
        z = h @ w_in[l]
        z_q, z_kv, z_kr, z_f, gate_a, gate_f = jnp.split(z, IN_OFFSETS, axis=-1)
        y_a = mla_mixer(z_q, z_kv, z_kr, q_a_norm_g[l], w_uq[l], kv_a_norm_g[l], w_ukv[l],
                        q_norm_g[l], k_norm_g[l]) @ w_proj_attn[l]
        y_f = fourier_mixer(z_f) @ w_proj_fourier[l]
        merged = jax.nn.sigmoid(gate_a) * y_a + jax.nn.sigmoid(gate_f) * y_f
        x = x + g1 * (merged @ w_out[l])

        h2 = rms_norm(x, norm2_g[l]) * (1 + sc2) + sh2
        x = x + g2 * moe_ffn(h2, w_router[l], router_bias[l], w_exp_gate[l], w_exp_up[l],
                             w_exp_down[l], w_sh_gate[l], w_sh_up[l], w_sh_down[l])
    return x
```

```python
import numpy as np
import ml_dtypes
from contextlib import ExitStack
import concourse.bass as bass
import concourse.mybir as mybir
from concourse.bass_utils import run_bass_kernel_spmd

F32 = mybir.dt.float32
BF16 = mybir.dt.bfloat16
I32 = mybir.dt.int32
ALU = mybir.AluOpType
AF = mybir.ActivationFunctionType
AX = mybir.AxisListType

S_TOK = 2048
D = 1024
NT = 16
KT = 8
NE = 256
C1 = 248
R_OVF = 12
ROW0 = 128
NROWS1 = ROW0 + NE * C1
NROWS = NROWS1 + R_OVF * 128
EPS = 1e-6
COMPUTE = ("pe", "act", "dve", "pool")


class Sched:
    def __init__(self, nc):
        self.nc = nc
        self.ops = []
        self.last_writer = {}
        self.readers = {}
        self.dma_last = {}
        self.last_on_eng = {}
        self.slotmap = {}

    def _add(self, eng, fn, reads, writes, dma_slot=None, extra_deps=()):
        i = len(self.ops)
        deps = set(extra_deps)
        for k in list(reads) + list(writes):
            w = self.last_writer.get(k)
            if w is not None:
                deps.add(w)
        for k in writes:
            for r in self.readers.get(k, ()):
                deps.add(r)
        if dma_slot is not None:
            qk = "sw" if eng == "pool" else "hw"
            sm = self.slotmap.setdefault(qk, {})
            dma_slot = (qk, sm.setdefault(dma_slot, len(sm)))
            p = self.dma_last.get(dma_slot)
            if p is not None:
                deps.add(p)
            self.dma_last[dma_slot] = i
        elif fn is not None and eng in COMPUTE:
            self.last_on_eng[eng] = i
        deps.discard(i)
        if eng == "pe" and dma_slot is None:
            deps = {d for d in deps if not (self.ops[d]["eng"] == "pe" and self.ops[d]["dma"] is None)}
        latest = {}
        keep = set()
        for d in deps:
            od = self.ops[d]
            if od["dma"] is None and od["fn"] is not None:
                if latest.get(od["eng"], -1) < d:
                    latest[od["eng"]] = d
            else:
                keep.add(d)
        deps = keep | set(latest.values())
        self.ops.append(dict(eng=eng, fn=fn, deps=deps, dma=dma_slot, signal=False))
        for k in writes:
            self.last_writer[k] = i
            self.readers[k] = []
        for k in reads:
            lst = self.readers.setdefault(k, [])
            if dma_slot is None:
                lst[:] = [r for r in lst if not (self.ops[r]["dma"] is None and self.ops[r]["eng"] == eng)]
            lst.append(i)
        return i

    def op(self, eng, fn, reads=(), writes=()):
        return self._add(eng, fn, reads, writes)

    def dma(self, queue, fn, reads=(), writes=(), slot=None):
        return self._add(queue, fn, reads, writes, dma_slot=slot)

    def barrier(self):
        deps = set(self.last_on_eng.values()) | set(self.dma_last.values())
        for e in ("pe", "act", "dve", "pool", "sp"):
            self._add(e, None, (), (), extra_deps=deps)
        self.last_writer = {}
        self.readers = {}
        self.slotmap = {}

    def emit(self):
        nc = self.nc
        ops = self.ops
        for o in ops:
            for d in o["deps"]:
                ops[d]["signal"] = True
        seq = {e: 0 for e in COMPUTE}
        dma_cnt = {}
        for o in ops:
            if o["dma"] is not None:
                dma_cnt[o["dma"]] = dma_cnt.get(o["dma"], 0) + 1
                o["semkey"] = ("dma", o["dma"])
                o["semval"] = 16 * dma_cnt[o["dma"]]
            elif o["signal"]:
                assert o["fn"] is not None
                seq[o["eng"]] += 1
                o["semkey"] = ("eng", o["eng"])
                o["semval"] = seq[o["eng"]]
        semkeys = [("eng", e) for e in COMPUTE] + [("dma", s) for s in dma_cnt]
        with ExitStack() as es:
            sems = {}
            for n, k in enumerate(semkeys):
                sems[k] = es.enter_context(nc.semaphore("sm%d" % n))
            streams = {e: [] for e in ("pe", "act", "dve", "pool", "sp")}
            for i, o in enumerate(ops):
                streams[o["eng"]].append(i)
            block = es.enter_context(nc.Block())
            engmap = {"pe": "tensor", "act": "scalar", "dve": "vector", "pool": "gpsimd", "sp": "sync"}

            def make(ename):
                def body(eng):
                    known = {}
                    for i in streams[ename]:
                        o = ops[i]
                        need = {}
                        for d in o["deps"]:
                            od = ops[d]
                            k, v = od["semkey"], od["semval"]
                            if known.get(k, 0) >= v:
                                continue
                            if need.get(k, 0) < v:
                                need[k] = v
                        for k, v in need.items():
                            eng.wait_ge(sems[k], v)
                            known[k] = v
                        if o["fn"] is None:
                            continue
                        ins = o["fn"](eng)
                        if o["dma"] is not None:
                            ins.then_inc(sems[o["semkey"]], 16)
                        elif o["signal"]:
                            ins.then_inc(sems[o["semkey"]], 1)
                    if ename == "sp":
                        for s, c in dma_cnt.items():
                            eng.wait_ge(sems[("dma", s)], 16 * c)
                        for e in COMPUTE:
                            if seq[e] > 0:
                                eng.wait_ge(sems[("eng", e)], seq[e])
                return body

            for ename, attr in engmap.items():
                getattr(block, attr)(make(ename))
        return dict(n_ops=len(ops), seq=seq, n_sems=len(semkeys))


class Arena:
    def __init__(self, ten, nunits):
        self.t = ten
        self.n = nunits
        self.off = 0
        self.peak = 0

    def alloc(self, shape, dtype, parts=128):
        size = {F32: 4, BF16: 2, I32: 4}[dtype]
        nel = int(np.prod(shape))
        units = (nel * size + 1) // 2
        units = (units + 31) // 32 * 32
        assert self.off + units <= self.n, ("arena overflow", self.off, units, self.n)
        v = self.t[0:parts, self.off:self.off + units]
        self.off += units
        self.peak = max(self.peak, self.off)
        if size == 4:
            v = v.bitcast(dtype)
        v = v[:, 0:nel]
        if len(shape) == 2:
            v = v.rearrange("p (a b) -> p a b", a=shape[0])
        elif len(shape) == 3:
            v = v.rearrange("p (a b c) -> p a b c", a=shape[0], b=shape[1])
        return v

    def mark(self):
        return self.off

    def release(self, m):
        self.off = m


def build(stage="full"):
    nc = bass.Bass("TRN2", target_bir_lowering=False)

    def din(name, shape, dt=F32):
        return nc.dram_tensor(name, list(shape), dt, kind="ExternalInput").ap()

    x = din("x", [S_TOK, D])
    c8 = din("c8", [128, 8])
    w_ada = din("w_ada", [D, 6 * D])
    b_ada = din("b_ada", [1, 6 * D])
    norm1_g = din("norm1_g", [1, D])
    w_in = din("w_in", [D, 2976])
    q_a_norm_g = din("q_a_norm_g", [1, 256])
    w_uq = din("w_uq", [256, 768])
    kv_a_norm_g = din("kv_a_norm_g", [1, 128])
    w_ukv = din("w_ukv", [128, 1024])
    q_norm_g = din("q_norm_g", [1, 96])
    k_norm_g = din("k_norm_g", [1, 96])
    w_pa = din("w_proj_attn", [512, D])
    w_pf = din("w_proj_fourier", [512, D])
    w_out = din("w_out", [D, D])
    norm2_g = din("norm2_g", [1, D])
    w_router = din("w_router", [D, NE])
    router_bias = din("router_bias", [1, NE])
    if stage == "full":
        w_eg = din("w_exp_gate", [NE, D, 256])
        w_eu = din("w_exp_up", [NE, D, 256])
        w_ed = din("w_exp_down", [NE, 256, D])
    w_sg = din("w_sh_gate", [D, 256])
    w_su = din("w_sh_up", [D, 256])
    w_sd = din("w_sh_down", [256, D])
    dftc = din("dftc", [S_TOK, S_TOK], BF16)
    dfts = din("dfts", [S_TOK, S_TOK], BF16)
    dft64 = din("dft64", [128, 256], BF16)
    rope = din("rope", [S_TOK, 32])
    ident_d = din("ident", [128, 128], BF16)
    tri_d = din("tri", [128, 128], BF16)
    ones_d = din("ones", [128, 128], BF16)
    iota1_d = din("iota1", [128, NE])
    iokp_d = din("iokp", [128, 8])

    out = nc.dram_tensor("out", [S_TOK, D], F32, kind="ExternalOutput").ap()

    def dscr(name, shape, dt):
        return nc.dram_tensor(name, list(shape), dt).ap()

    hT_scr = dscr("hT_scr", [128, KT * S_TOK], BF16)
    FT_scr = dscr("FT_scr", [128, 4 * S_TOK], BF16)
    AT_scr = dscr("AT_scr", [128, 4 * S_TOK], BF16)
    x1_scr = dscr("x1_scr", [S_TOK, D], F32)
    base_scr = dscr("base_scr", [S_TOK, D], F32)
    h2_scr = dscr("h2_scr", [S_TOK, D], BF16)
    xdisp = dscr("xdisp", [NROWS, D], BF16)
    yscr = dscr("yscr", [NROWS, D], F32)

    NUNITS = 205 * 512
    with ExitStack() as es:
        arena_t = es.enter_context(nc.sbuf_tensor("arena", [128, NUNITS], BF16))
        ps = [es.enter_context(nc.psum_tensor("ps%d" % i, [128, 512], F32)) for i in range(8)]
        psb = [p[:].bitcast(BF16) for p in ps]
        A = Arena(arena_t, NUNITS)
        S = Sched(nc)

        def PS(i):
            return ("ps", i)

        ident = A.alloc([128], BF16)
        tri = A.alloc([128], BF16)
        ones = A.alloc([128], BF16)
        S.dma("sp", lambda e: e.dma_start(out=ident, in_=ident_d), writes=["ident"], slot="c_ident")
        S.dma("sp", lambda e: e.dma_start(out=tri, in_=tri_d), writes=["tri"], slot="c_tri")
        S.dma("sp", lambda e: e.dma_start(out=ones, in_=ones_d), writes=["ones"], slot="c_ones")
        ada = A.alloc([6, D], F32)

        def bc_load(dst, src_row, key, slot):
            S.dma("sp", lambda e: e.dma_start(out=dst, in_=src_row.partition_broadcast(128)),
                  writes=[key], slot=slot)

        m0 = A.mark()
        hT = A.alloc([KT, S_TOK], BF16)
        m1 = A.mark()
        csil = A.alloc([8], F32)
        c_sb = A.alloc([8], F32)
        crep = A.alloc([8, 128], BF16)
        wa = [A.alloc([8, D], BF16) for _ in range(2)]
        bb = [A.alloc([D], F32) for _ in range(2)]
        gn = [A.alloc([D], F32) for _ in range(2)]
        S.dma("sp", lambda e: e.dma_start(out=c_sb, in_=c8), writes=["c_sb"], slot="c_sb")
        S.op("act", lambda e: e.activation(csil, c_sb, AF.Silu), reads=["c_sb"], writes=["csil"])
        S.op("dve", lambda e: e.tensor_copy(crep, csil.unsqueeze(2).to_broadcast([128, 8, 128])),
             reads=["csil"], writes=["crep"])
        bc_load(gn[0], norm1_g[0, :], "gn0", "gn0")
        bc_load(gn[1], norm2_g[0, :], "gn1", "gn1")
        for n, j in enumerate([1, 0, 2, 4, 3, 5]):
            b = n % 2
            S.dma("pool", lambda e, j=j, b=b: e.dma_start(
                out=wa[b], in_=w_ada[:, j * D:(j + 1) * D].rearrange("(kt p) n -> p kt n", p=128)),
                writes=[("wa", b)], slot="wa%d" % b)
            bc_load(bb[b], b_ada[0, j * D:(j + 1) * D], ("bb", b), "bb%d" % b)
            for half in range(2):
                for kt in range(KT):
                    S.op("pe", lambda e, b=b, half=half, kt=kt: e.matmul(
                        ps[half][:, :], crep[:, kt, :], wa[b][:, kt, half * 512:(half + 1) * 512],
                        start=(kt == 0), stop=(kt == KT - 1)),
                        reads=["crep", ("wa", b)], writes=[PS(half)])
                S.op("dve", lambda e, b=b, half=half, j=j: e.tensor_tensor(
                    ada[:, j, half * 512:(half + 1) * 512], ps[half][:, :], bb[b][:, half * 512:(half + 1) * 512], ALU.add),
                    reads=[PS(half), ("bb", b)], writes=[("ada", j, half)])
            if j in (1, 4):
                g = gn[0] if j == 1 else gn[1]
                gk = "gn0" if j == 1 else "gn1"
                S.op("dve", lambda e, j=j, g=g: e.scalar_tensor_tensor(
                    ada[:, j, :], ada[:, j, :], 1.0, g, ALU.add, ALU.mult),
                    reads=[("ada", j, 0), ("ada", j, 1), gk], writes=[("ada", j, 0), ("ada", j, 1)])
        ADA = lambda j: [("ada", j, 0), ("ada", j, 1)]

        xb = [A.alloc([D], F32) for _ in range(2)]
        tmpf = [A.alloc([D], F32) for _ in range(2)]
        hb = [A.alloc([D], BF16) for _ in range(2)]
        sq = A.alloc([D], F32)
        st1 = A.alloc([NT, 4], F32)
        for t in range(NT):
            b = t % 2
            S.dma("sp", lambda e, t=t, b=b: e.dma_start(out=xb[b], in_=x[t * 128:(t + 1) * 128, :]),
                  writes=[("xb", b)], slot="xb%d" % b)
            S.op("act", lambda e, t=t, b=b: e.activation(sq, xb[b], AF.Square, accum_out=st1[:, t, 0:1]),
                 reads=[("xb", b)], writes=["sq", ("st1", t)])
            S.op("act", lambda e, t=t: e.activation(st1[:, t, 1:2], st1[:, t, 0:1], AF.Sqrt, bias=EPS, scale=1.0 / D),
                 reads=[("st1", t)], writes=[("st1b", t)])
            S.op("dve", lambda e, t=t: e.reciprocal(st1[:, t, 2:3], st1[:, t, 1:2]),
                 reads=[("st1b", t)], writes=[("st1c", t)])
            S.op("dve", lambda e, t=t, b=b: e.scalar_tensor_tensor(
                tmpf[b], xb[b], st1[:, t, 2:3], ada[:, 1, :], ALU.mult, ALU.mult),
                reads=[("xb", b), ("st1c", t)] + ADA(1), writes=[("tmpf", b)])
            S.op("pool", lambda e, b=b: e.tensor_tensor(hb[b], tmpf[b], ada[:, 0, :], ALU.add),
                 reads=[("tmpf", b)] + ADA(0), writes=[("hb", b)])
            pb = 2 + b
            for kt in range(KT):
                S.op("pe", lambda e, b=b, kt=kt, pb=pb: e.transpose(
                    psb[pb][:, kt * 128:(kt + 1) * 128], hb[b][:, kt * 128:(kt + 1) * 128], ident),
                    reads=[("hb", b), "ident"], writes=[PS(pb)])
            S.op("act", lambda e, t=t, pb=pb: e.copy(
                hT[:, :, t * 128:(t + 1) * 128], psb[pb][:, :].rearrange("p (a b) -> p a b", a=8)),
                reads=[PS(pb)], writes=[("hT", t)])
        HT_ALL = [("hT", t) for t in range(NT)]
        S.dma("sp", lambda e: e.dma_start(out=hT_scr.rearrange("p (a b) -> p a b", a=KT), in_=hT),
              reads=HT_ALL, writes=["hT_scr"], slot="hT_st")
        S.barrier()
        A.release(m1)

        NORM = float(1.0 / np.sqrt(float(S_TOK * 64)))
        m2 = A.mark()
        win_f = A.alloc([KT, 512], BF16)
        d64 = A.alloc([256], BF16)
        zcs = A.alloc([NT, 4, 256], BF16)
        zft = [A.alloc([512], BF16) for _ in range(2)]
        S.dma("pool", lambda e: e.dma_start(out=win_f, in_=w_in[:, 416:928].rearrange("(kt p) n -> p kt n", p=128)),
              writes=["win_f"], slot="win_f")
        S.dma("sp", lambda e: e.dma_start(out=d64, in_=dft64), writes=["d64"], slot="d64")
        n = 0
        for cc in range(4):
            for qc in range(4):
                b = n % 2
                n += 1
                for kt in range(KT):
                    S.op("pe", lambda e, b=b, cc=cc, qc=qc, kt=kt: e.matmul(
                        ps[b][:, :], win_f[:, kt, cc * 128:(cc + 1) * 128], hT[:, kt, qc * 512:(qc + 1) * 512],
                        start=(kt == 0), stop=(kt == KT - 1)),
                        reads=["win_f"] + [("hT", qc * 4 + s) for s in range(4)], writes=[PS(b)])
                S.op("act", lambda e, b=b: e.copy(zft[b], ps[b][:, :]), reads=[PS(b)], writes=[("zft", b)])
                for sub in range(4):
                    t = qc * 4 + sub
                    pb = 2 + (sub % 2)
                    S.op("pe", lambda e, b=b, sub=sub, pb=pb: e.matmul(
                        ps[pb][:, 0:256], zft[b][:, sub * 128:(sub + 1) * 128], d64, start=True, stop=True),
                        reads=[("zft", b), "d64"], writes=[PS(pb)])
                    S.op("dve", lambda e, t=t, cc=cc, pb=pb: e.tensor_scalar_mul(
                        zcs[:, t, cc, :], ps[pb][:, 0:256], NORM),
                        reads=[PS(pb)], writes=[("zcs", t, cc)])
        FT = A.alloc([4, S_TOK], BF16)
        dbuf = [A.alloc([NT, 512], BF16) for _ in range(2)]
        n = 0
        for kc in range(4):
            for cs in range(2):
                b = n % 2
                n += 1
                src = dftc if cs == 0 else dfts
                S.dma("sp", lambda e, b=b, src=src, kc=kc: e.dma_start(
                    out=dbuf[b], in_=src[:, kc * 512:(kc + 1) * 512].rearrange("(nt p) k -> p nt k", p=128)),
                    writes=[("dbuf", b)], slot="dbuf%d" % b)
                for cc in range(4):
                    for nt in range(NT):
                        S.op("pe", lambda e, b=b, cc=cc, nt=nt, cs=cs: e.matmul(
                            ps[4 + cc][:, :], zcs[:, nt, cc, cs * 128:(cs + 1) * 128], dbuf[b][:, nt, :],
                            start=(cs == 0 and nt == 0), stop=(cs == 1 and nt == NT - 1)),
                            reads=[("zcs", nt, cc), ("dbuf", b)], writes=[PS(4 + cc)])
            for cc in range(4):
                eng = "act" if cc % 2 == 0 else "dve"
                if eng == "act":
                    S.op("act", lambda e, cc=cc, kc=kc: e.copy(FT[:, cc, kc * 512:(kc + 1) * 512], ps[4 + cc][:, :]),
                         reads=[PS(4 + cc)], writes=[("FT", cc, kc)])
                else:
                    S.op("dve", lambda e, cc=cc, kc=kc: e.tensor_copy(FT[:, cc, kc * 512:(kc + 1) * 512], ps[4 + cc][:, :]),
                         reads=[PS(4 + cc)], writes=[("FT", cc, kc)])
        S.dma("sp", lambda e: e.dma_start(out=FT_scr.rearrange("p (a b) -> p a b", a=4), in_=FT),
              reads=[("FT", cc, kc) for cc in range(4) for kc in range(4)], writes=["FT_scr"], slot="FT_st")
        S.barrier()
        A.release(m2)

        qT = A.alloc([8, S_TOK], BF16)
        kT = A.alloc([8, S_TOK], BF16)
        vb = A.alloc([NT, 8, 65], BF16)
        m3 = A.mark()
        win_a = A.alloc([KT, 416], BF16)
        wuq = A.alloc([2, 768], BF16)
        wukv = A.alloc([1024], BF16)
        ropet = A.alloc([NT, 32], F32)
        gqa = A.alloc([256], F32)
        gkva = A.alloc([128], F32)
        g96 = A.alloc([2, 96], F32)
        S.dma("pool", lambda e: e.dma_start(out=win_a, in_=w_in[:, 0:416].rearrange("(kt p) n -> p kt n", p=128)),
              writes=["win_a"], slot="win_a")
        S.dma("pool", lambda e: e.dma_start(out=wuq, in_=w_uq.rearrange("(kt p) n -> p kt n", p=128)),
              writes=["wuq"], slot="wuq")
        S.dma("pool", lambda e: e.dma_start(out=wukv, in_=w_ukv), writes=["wukv"], slot="wukv")
        S.dma("sp", lambda e: e.dma_start(out=ropet, in_=rope.rearrange("(t p) c -> p t c", p=128)),
              writes=["ropet"], slot="ropet")
        bc_load(gqa, q_a_norm_g[0, :], "gqa", "gqa")
        bc_load(gkva, kv_a_norm_g[0, :], "gkva", "gkva")
        bc_load(g96[:, 0, :], q_norm_g[0, :], "g96q", "g96q")
        bc_load(g96[:, 1, :], k_norm_g[0, :], "g96k", "g96k")
        S.op("dve", lambda e: e.tensor_scalar_mul(g96[:, 0, :], g96[:, 0, :], 96.0 ** -0.5), reads=["g96q"], writes=["g96q"])
        S.op("pool", lambda e: e.memset(vb, 1.0), writes=["vb_init"])
        NB2 = 2
        sqa = [A.alloc([768], F32) for _ in range(NB2)]
        st2 = A.alloc([NT, 40], F32)
        cqb = [A.alloc([384], BF16) for _ in range(NB2)]
        cT = [A.alloc([3, 128], BF16) for _ in range(NB2)]
        kr = [A.alloc([32], F32) for _ in range(NB2)]
        kraw = [A.alloc([8, 96], F32) for _ in range(NB2)]
        nbuf = [[A.alloc([8, 96], F32) for _ in range(2)] for _ in range(NB2)]
        rt = [[A.alloc([4, 8, 16], F32) for _ in range(2)] for _ in range(NB2)]
        qkb = [[A.alloc([8, 96], BF16) for _ in range(2)] for _ in range(NB2)]

        def norm_rope(t, which, src, src_keys, ss_col, dstT):
            w = which
            tb = t % NB2
            nb_ = nbuf[tb][w]
            rt_ = rt[tb][w]
            qb_ = qkb[tb][w]
            NK = ("nbuf", tb, w)
            gbc = g96[:, w:w + 1, :].to_broadcast([128, 8, 96])
            gkey = "g96q" if w == 0 else "g96k"
            S.op("act", lambda e: e.activation(st2[:, t, ss_col + 8:ss_col + 16], st2[:, t, ss_col:ss_col + 8],
                                               AF.Sqrt, bias=EPS, scale=1.0 / 96),
                 reads=[("ss", t, w)], writes=[("sd", t, w)])
            yield
            S.op("dve", lambda e: e.reciprocal(st2[:, t, ss_col:ss_col + 8], st2[:, t, ss_col + 8:ss_col + 16]),
                 reads=[("sd", t, w)], writes=[("rs", t, w)])
            yield
            S.op("dve", lambda e: e.tensor_tensor(
                nb_, src, st2[:, t, ss_col:ss_col + 8].unsqueeze(2).to_broadcast([128, 8, 96]), ALU.mult),
                reads=src_keys + [("rs", t, w)], writes=[NK])
            yield
            S.op("pool", lambda e: e.tensor_tensor(nb_, nb_, gbc, ALU.mult),
                 reads=[NK, gkey], writes=[NK])
            yield
            cosb = ropet[:, t, 0:16].unsqueeze(1).to_broadcast([128, 8, 16])
            sinb = ropet[:, t, 16:32].unsqueeze(1).to_broadcast([128, 8, 16])
            r1 = nb_[:, :, 64:80]
            r2 = nb_[:, :, 80:96]
            S.op("act", lambda e: e.copy(qb_[:, :, 0:64], nb_[:, :, 0:64]),
                 reads=[NK], writes=[("qkb", tb, w, 0)])
            S.op("dve", lambda e: e.tensor_tensor(rt_[:, 0, :, :], r1, cosb, ALU.mult),
                 reads=[NK, "ropet"], writes=[("rt", tb, w, 0)])
            S.op("pool", lambda e: e.tensor_tensor(rt_[:, 1, :, :], r2, sinb, ALU.mult),
                 reads=[NK, "ropet"], writes=[("rt", tb, w, 1)])
            yield
            S.op("dve", lambda e: e.tensor_tensor(rt_[:, 2, :, :], r2, cosb, ALU.mult),
                 reads=[NK, "ropet"], writes=[("rt", tb, w, 2)])
            S.op("pool", lambda e: e.tensor_tensor(rt_[:, 3, :, :], r1, sinb, ALU.mult),
                 reads=[NK, "ropet"], writes=[("rt", tb, w, 3)])
            yield
            S.op("dve", lambda e: e.tensor_tensor(qb_[:, :, 64:80], rt_[:, 0, :, :], rt_[:, 1, :, :], ALU.subtract),
                 reads=[("rt", tb, w, 0), ("rt", tb, w, 1)], writes=[("qkb", tb, w, 1)])
            S.op("pool", lambda e: e.tensor_tensor(qb_[:, :, 80:96], rt_[:, 2, :, :], rt_[:, 3, :, :], ALU.add),
                 reads=[("rt", tb, w, 2), ("rt", tb, w, 3)], writes=[("qkb", tb, w, 2)])
            yield
            pb = 6 + w
            for h in range(8):
                S.op("pe", lambda e, h=h: e.transpose(psb[pb][0:96, h * 128:(h + 1) * 128], qb_[:, h, :], ident),
                     reads=[("qkb", tb, w, 0), ("qkb", tb, w, 1), ("qkb", tb, w, 2), "ident"], writes=[PS(pb)])
            S.op("act", lambda e: e.copy(dstT[0:96, :, t * 128:(t + 1) * 128],
                                         psb[pb][0:96, :].rearrange("p (a b) -> p a b", a=8)),
                 reads=[PS(pb)], writes=[("qkT", w, t)])
            yield

        def prep_tile(t):
            tb = t % NB2
            sq_ = sqa[tb]
            SQ = ("sqa", tb)
            for kt in range(KT):
                S.op("pe", lambda e, kt=kt: e.matmul(
                    ps[0][:, 0:416], hT[:, kt, t * 128:(t + 1) * 128], win_a[:, kt, :],
                    start=(kt == 0), stop=(kt == KT - 1)),
                    reads=[("hT", t), "win_a"], writes=[PS(0)])
            S.op("act", lambda e: e.activation(sq_[:, 0:256], ps[0][:, 0:256], AF.Square, accum_out=st2[:, t, 0:1]),
                 reads=[PS(0)], writes=[SQ, ("s0", t)])
            S.op("act", lambda e: e.activation(sq_[:, 256:384], ps[0][:, 256:384], AF.Square, accum_out=st2[:, t, 1:2]),
                 reads=[PS(0)], writes=[SQ, ("s1", t)])
            S.op("act", lambda e: e.activation(st2[:, t, 4:5], st2[:, t, 0:1], AF.Sqrt, bias=EPS, scale=1.0 / 256),
                 reads=[("s0", t)], writes=[("s0b", t)])
            S.op("act", lambda e: e.activation(st2[:, t, 5:6], st2[:, t, 1:2], AF.Sqrt, bias=EPS, scale=1.0 / 128),
                 reads=[("s1", t)], writes=[("s1b", t)])
            S.op("dve", lambda e: e.reciprocal(st2[:, t, 6:8], st2[:, t, 4:6]),
                 reads=[("s0b", t), ("s1b", t)], writes=[("s01c", t)])
            S.op("dve", lambda e: e.scalar_tensor_tensor(
                cqb[tb][:, 0:256], ps[0][:, 0:256], st2[:, t, 6:7], gqa, ALU.mult, ALU.mult),
                reads=[PS(0), ("s01c", t), "gqa"], writes=[("cqb0", tb)])
            S.op("dve", lambda e: e.scalar_tensor_tensor(
                cqb[tb][:, 256:384], ps[0][:, 256:384], st2[:, t, 7:8], gkva, ALU.mult, ALU.mult),
                reads=[PS(0), ("s01c", t), "gkva"], writes=[("cqb1", tb)])
            S.op("act", lambda e: e.copy(kr[tb], ps[0][:, 384:416]), reads=[PS(0)], writes=[("kr", tb)])
            for j in range(3):
                S.op("pe", lambda e, j=j: e.transpose(psb[1][:, j * 128:(j + 1) * 128], cqb[tb][:, j * 128:(j + 1) * 128], ident),
                     reads=[("cqb0", tb), ("cqb1", tb), "ident"], writes=[PS(1)])
            S.op("act", lambda e: e.copy(cT[tb], psb[1][:, 0:384].rearrange("p (a b) -> p a b", a=3)),
                 reads=[PS(1)], writes=[("cT", tb)])
            for j in range(2):
                S.op("pe", lambda e, j=j: e.matmul(ps[2][:, :], cT[tb][:, j, :], wuq[:, j, 0:512], start=(j == 0), stop=(j == 1)),
                     reads=[("cT", tb), "wuq"], writes=[PS(2)])
            for j in range(2):
                S.op("pe", lambda e, j=j: e.matmul(ps[3][:, 0:256], cT[tb][:, j, :], wuq[:, j, 512:768], start=(j == 0), stop=(j == 1)),
                     reads=[("cT", tb), "wuq"], writes=[PS(3)])
            for hf in range(2):
                S.op("pe", lambda e, hf=hf: e.matmul(ps[4 + hf][:, :], cT[tb][:, 2, :], wukv[:, hf * 512:(hf + 1) * 512], start=True, stop=True),
                     reads=[("cT", tb), "wukv"], writes=[PS(4 + hf)])
            S.op("act", lambda e: e.copy(sq_[:, 0:512], ps[2][:, :]), reads=[PS(2)], writes=[SQ])
            S.op("act", lambda e: e.copy(sq_[:, 512:768], ps[3][:, 0:256]), reads=[PS(3)], writes=[SQ])
            qraw = sq_[:, 0:768].rearrange("p (a b) -> p a b", a=8)
            S.op("dve", lambda e: e.tensor_tensor(nbuf[tb][0], qraw, qraw, ALU.mult), reads=[SQ], writes=[("nbuf", tb, 0)])
            S.op("dve", lambda e: e.tensor_reduce(st2[:, t, 8:16], nbuf[tb][0], AX.X, ALU.add),
                 reads=[("nbuf", tb, 0)], writes=[("ss", t, 0)])
            for hf in range(2):
                S.op("act", lambda e, hf=hf: e.copy(
                    kraw[tb][:, hf * 4:(hf + 1) * 4, 0:64],
                    ps[4 + hf][:, :].rearrange("p (a b) -> p a b", a=4)[:, :, 0:64]),
                    reads=[PS(4 + hf)], writes=[("kraw", tb, hf)])
                S.op("dve", lambda e, hf=hf: e.tensor_copy(
                    vb[:, t, hf * 4:(hf + 1) * 4, 0:64],
                    ps[4 + hf][:, :].rearrange("p (a b) -> p a b", a=4)[:, :, 64:128]),
                    reads=[PS(4 + hf), "vb_init"], writes=[("vb", t, hf)])
            S.op("pool", lambda e: e.tensor_copy(kraw[tb][:, :, 64:96], kr[tb].unsqueeze(1).to_broadcast([128, 8, 32])),
                 reads=[("kr", tb)], writes=[("kraw", tb, 2)])
            KR = [("kraw", tb, 0), ("kraw", tb, 1), ("kraw", tb, 2)]
            S.op("dve", lambda e: e.tensor_tensor(nbuf[tb][1], kraw[tb], kraw[tb], ALU.mult),
                 reads=KR, writes=[("nbuf", tb, 1)])
            S.op("dve", lambda e: e.tensor_reduce(st2[:, t, 24:32], nbuf[tb][1], AX.X, ALU.add),
                 reads=[("nbuf", tb, 1)], writes=[("ss", t, 1)])
            gq = norm_rope(t, 0, qraw, [SQ], 8, qT)
            gk = norm_rope(t, 1, kraw[tb], KR, 24, kT)
            alive = [gq, gk]
            while alive:
                for g in list(alive):
                    try:
                        next(g)
                    except StopIteration:
                        alive.remove(g)

        for t in range(NT):
            prep_tile(t)
        S.barrier()
        A.release(m3)

        m4 = A.mark()
        A_tok = A.alloc([NT, 512], BF16)
        pT = [A.alloc([512], BF16) for _ in range(4)]
        rden = A.alloc([2, 4], F32)
        AT = A.alloc([4, S_TOK], BF16)
        QK_ALL = lambda w: [("qkT", w, t) for t in range(NT)]
        its = [(h, qc, j) for h in range(8) for qc in range(4) for j in range(NT)]
        LOOK = 2

        def emit_qk(i):
            h, qc, j = its[i]
            sbk = i % 4
            S.op("pe", lambda e: e.matmul(
                ps[sbk][:, :], kT[0:96, h, j * 128:(j + 1) * 128], qT[0:96, h, qc * 512:(qc + 1) * 512],
                start=True, stop=True),
                reads=[("qkT", 1, j)] + [("qkT", 0, qc * 4 + s_) for s_ in range(4)], writes=[PS(sbk)])
            S.op("act", lambda e: e.activation(pT[sbk], ps[sbk][:, :], AF.Exp),
                 reads=[PS(sbk)], writes=[("pT", sbk)])

        for i in range(min(LOOK, len(its))):
            emit_qk(i)
        for i, (h, qc, j) in enumerate(its):
            if i + LOOK < len(its):
                emit_qk(i + LOOK)
            sbk = i % 4
            grp = i // NT
            ob = 4 + (grp % 2)
            for sub in range(4):
                S.op("pe", lambda e, h=h, j=j, sbk=sbk, sub=sub, ob=ob: e.matmul(
                    ps[ob][:, sub * 128:sub * 128 + 65], pT[sbk][:, sub * 128:(sub + 1) * 128], vb[:, j, h, :],
                    start=(j == 0), stop=(j == NT - 1)),
                    reads=[("pT", sbk), ("vb", j, h // 4), "vb_init"], writes=[PS(ob)])
            if j == NT - 1:
                o4 = ps[ob][:, :].rearrange("p (a b) -> p a b", a=4)
                rb = grp % 2
                S.op("dve", lambda e, o4=o4, rb=rb: e.reciprocal(rden[:, rb, :].unsqueeze(2), o4[:, :, 64:65]),
                     reads=[PS(ob)], writes=[("rden", rb)])
                S.op("dve", lambda e, o4=o4, rb=rb, h=h, qc=qc: e.tensor_tensor(
                    A_tok[:, qc * 4:(qc + 1) * 4, h * 64:(h + 1) * 64], o4[:, :, 0:64],
                    rden[:, rb, :].unsqueeze(2).to_broadcast([128, 4, 64]), ALU.mult),
                    reads=[PS(ob), ("rden", rb)], writes=[("A_tok", qc, h)])
        for t in range(NT):
            pb = 6 + (t % 2)
            for j in range(4):
                S.op("pe", lambda e, t=t, j=j, pb=pb: e.transpose(
                    psb[pb][:, j * 128:(j + 1) * 128], A_tok[:, t, j * 128:(j + 1) * 128], ident),
                    reads=[("A_tok", t // 4, h) for h in range(8)] + ["ident"], writes=[PS(pb)])
            S.op("act", lambda e, t=t, pb=pb: e.copy(
                AT[:, :, t * 128:(t + 1) * 128], psb[pb][:, 0:512].rearrange("p (a b) -> p a b", a=4)),
                reads=[PS(pb)], writes=[("AT", t)])
        S.dma("sp", lambda e: e.dma_start(out=AT_scr.rearrange("p (a b) -> p a b", a=4), in_=AT),
              reads=[("AT", t) for t in range(NT)], writes=["AT_scr"], slot="AT_st")
        S.barrier()
        A.release(m0)

        m5 = A.mark()
        win_g = A.alloc([KT, 2048], BF16)
        wpa = A.alloc([4, D], BF16)
        wpf = A.alloc([4, D], BF16)
        wout = A.alloc([KT, D], BF16)
        hTc = [A.alloc([KT, 512], BF16) for _ in range(2)]
        ATc = [A.alloc([4, 512], BF16) for _ in range(2)]
        FTc = [A.alloc([4, 512], BF16) for _ in range(2)]
        mTc = [A.alloc([KT, 512], BF16) for _ in range(2)]
        sa = [A.alloc([512], F32) for _ in range(2)]
        sf = [A.alloc([512], F32) for _ in range(2)]
        mm1 = [A.alloc([512], F32) for _ in range(2)]
        mm2 = [A.alloc([512], F32) for _ in range(2)]
        xb5 = [A.alloc([D], F32) for _ in range(4)]
        t5 = [A.alloc([D], F32) for _ in range(2)]
        x1b = [A.alloc([D], F32) for _ in range(2)]
        for half in range(2):
            S.dma("pool", lambda e, half=half: e.dma_start(
                out=win_g[:, :, half * 1024:(half + 1) * 1024],
                in_=w_in[:, 928 + half * 1024:928 + (half + 1) * 1024].rearrange("(kt p) n -> p kt n", p=128)),
                writes=[("win_g", half)], slot="win_g%d" % half)
        S.dma("pool", lambda e: e.dma_start(out=wpa, in_=w_pa.rearrange("(kt p) n -> p kt n", p=128)), writes=["wpa"], slot="wpa")
        S.dma("pool", lambda e: e.dma_start(out=wpf, in_=w_pf.rearrange("(kt p) n -> p kt n", p=128)), writes=["wpf"], slot="wpf")
        S.dma("pool", lambda e: e.dma_start(out=wout, in_=w_out.rearrange("(kt p) n -> p kt n", p=128)), writes=["wout"], slot="wout")
        nfc = 0

        def load_chunk(qc):
            cb = qc % 2
            S.dma("sp", lambda e: e.dma_start(
                out=hTc[cb], in_=hT_scr.rearrange("p (a b) -> p a b", a=KT)[:, :, qc * 512:(qc + 1) * 512]),
                reads=["hT_scr"], writes=[("hTc", cb)], slot="hTc%d" % cb)
            S.dma("sp", lambda e: e.dma_start(
                out=ATc[cb], in_=AT_scr.rearrange("p (a b) -> p a b", a=4)[:, :, qc * 512:(qc + 1) * 512]),
                reads=["AT_scr"], writes=[("ATc", cb)], slot="ATc%d" % cb)
            S.dma("sp", lambda e: e.dma_start(
                out=FTc[cb], in_=FT_scr.rearrange("p (a b) -> p a b", a=4)[:, :, qc * 512:(qc + 1) * 512]),
                reads=["FT_scr"], writes=[("FTc", cb)], slot="FTc%d" % cb)

        load_chunk(0)
        for qc in range(4):
            cb = qc % 2
            if qc + 1 < 4:
                load_chunk(qc + 1)
            for sub in range(4):
                t_ = qc * 4 + sub
                S.dma("sp", lambda e, t_=t_, sub=sub: e.dma_start(out=xb5[sub], in_=x[t_ * 128:(t_ + 1) * 128, :]),
                      writes=[("xb5", sub)], slot="xb5%d" % sub)
            for fc in range(8):
                pbase = 4 * (nfc % 2)
                eb = nfc % 2
                nfc += 1
                for kt in range(KT):
                    S.op("pe", lambda e, cb=cb, fc=fc, kt=kt, pbase=pbase: e.matmul(
                        ps[pbase][:, :], win_g[:, kt, fc * 128:(fc + 1) * 128], hTc[cb][:, kt, :],
                        start=(kt == 0), stop=(kt == KT - 1)),
                        reads=[("win_g", 0), ("hTc", cb)], writes=[PS(pbase)])
                for kt in range(KT):
                    S.op("pe", lambda e, cb=cb, fc=fc, kt=kt, pbase=pbase: e.matmul(
                        ps[pbase + 1][:, :], win_g[:, kt, 1024 + fc * 128:1024 + (fc + 1) * 128], hTc[cb][:, kt, :],
                        start=(kt == 0), stop=(kt == KT - 1)),
                        reads=[("win_g", 1), ("hTc", cb)], writes=[PS(pbase + 1)])
                for j in range(4):
                    S.op("pe", lambda e, cb=cb, fc=fc, j=j, pbase=pbase: e.matmul(
                        ps[pbase + 2][:, :], wpa[:, j, fc * 128:(fc + 1) * 128], ATc[cb][:, j, :],
                        start=(j == 0), stop=(j == 3)),
                        reads=["wpa", ("ATc", cb)], writes=[PS(pbase + 2)])
                for j in range(4):
                    S.op("pe", lambda e, cb=cb, fc=fc, j=j, pbase=pbase: e.matmul(
                        ps[pbase + 3][:, :], wpf[:, j, fc * 128:(fc + 1) * 128], FTc[cb][:, j, :],
                        start=(j == 0), stop=(j == 3)),
                        reads=["wpf", ("FTc", cb)], writes=[PS(pbase + 3)])
                S.op("act", lambda e, eb=eb, pbase=pbase: e.activation(sa[eb], ps[pbase][:, :], AF.Sigmoid),
                     reads=[PS(pbase)], writes=[("sa", eb)])
                S.op("act", lambda e, eb=eb, pbase=pbase: e.activation(sf[eb], ps[pbase + 1][:, :], AF.Sigmoid),
                     reads=[PS(pbase + 1)], writes=[("sf", eb)])
                S.op("dve", lambda e, eb=eb, pbase=pbase: e.tensor_tensor(mm1[eb], ps[pbase + 2][:, :], sa[eb], ALU.mult),
                     reads=[PS(pbase + 2), ("sa", eb)], writes=[("mm1", eb)])
                S.op("dve", lambda e, eb=eb, pbase=pbase: e.tensor_tensor(mm2[eb], ps[pbase + 3][:, :], sf[eb], ALU.mult),
                     reads=[PS(pbase + 3), ("sf", eb)], writes=[("mm2", eb)])
                S.op("pool", lambda e, eb=eb, cb=cb, fc=fc: e.tensor_tensor(mTc[cb][:, fc, :], mm1[eb], mm2[eb], ALU.add),
                     reads=[("mm1", eb), ("mm2", eb)], writes=[("mTc", cb, fc)])
            for sub in range(4):
                t = qc * 4 + sub
                xbi = t % 2
                p0 = 2 * sub
                for hf in range(2):
                    for fc in range(8):
                        S.op("pe", lambda e, cb=cb, sub=sub, hf=hf, fc=fc, p0=p0: e.matmul(
                            ps[p0 + hf][:, :], mTc[cb][:, fc, sub * 128:(sub + 1) * 128], wout[:, fc, hf * 512:(hf + 1) * 512],
                            start=(fc == 0), stop=(fc == 7)),
                            reads=[("mTc", cb, fc), "wout"], writes=[PS(p0 + hf)])
                    S.op("dve", lambda e, xbi=xbi, hf=hf, p0=p0: e.tensor_tensor(
                        t5[xbi][:, hf * 512:(hf + 1) * 512], ps[p0 + hf][:, :], ada[:, 2, hf * 512:(hf + 1) * 512], ALU.mult),
                        reads=[PS(p0 + hf)] + ADA(2), writes=[("t5", xbi, hf)])
                S.op("pool", lambda e, xbi=xbi, sub=sub: e.tensor_tensor(x1b[xbi], t5[xbi], xb5[sub], ALU.add),
                     reads=[("t5", xbi, 0), ("t5", xbi, 1), ("xb5", sub)], writes=[("x1b", xbi)])
                dst = out if stage == "x1" else x1_scr
                S.dma("sp", lambda e, t=t, xbi=xbi, dst=dst: e.dma_start(out=dst[t * 128:(t + 1) * 128, :], in_=x1b[xbi]),
                      reads=[("x1b", xbi)], writes=[("x1_scr", t)], slot="x1st%d" % xbi)
        S.barrier()
        A.release(m5)
        if stage == "x1":
            st = S.emit()
            print("sched", st, "arena peak KiB", A.peak / 512.0)
            return nc

        gwk = A.alloc([NT, 8], F32)
        idx = A.alloc([NT, 8], I32)
        widx = A.alloc([R_OVF], I32)
        m6 = A.mark()
        Gw = A.alloc([NT, NE], F32)
        Pos = A.alloc([NT, NE], F32)
        run_bc = A.alloc([NE], F32)
        iota1 = A.alloc([NE], F32)
        m7 = A.mark()
        wr_f = A.alloc([KT, NE], F32)
        wr_hi = A.alloc([KT, NE], BF16)
        wr_lo = A.alloc([KT, NE], BF16)
        wsh = A.alloc([KT, 512], BF16)
        wshd = A.alloc([2, D], BF16)
        rbias = A.alloc([NE], F32)
        tmp2 = [A.alloc([D], F32) for _ in range(2)]
        h2hi = [A.alloc([D], BF16) for _ in range(2)]
        h2lo = [A.alloc([D], BF16) for _ in range(2)]
        h2T = [A.alloc([KT, 128], BF16) for _ in range(2)]
        h2loT = [A.alloc([KT, 128], BF16) for _ in range(2)]
        sq5_ = [A.alloc([D], F32)]
        st5 = A.alloc([NT, 8], F32)
        scb_ = [A.alloc([NE], F32) for _ in range(2)]
        biased_ = [A.alloc([NE], F32) for _ in range(2)]
        wsel_ = [A.alloc([NE], F32) for _ in range(2)]
        maskb_ = [A.alloc([NE], BF16) for _ in range(2)]
        top8 = A.alloc([NT, 8], F32)
        sg_ = [A.alloc([256], F32) for _ in range(2)]
        hsh_ = [A.alloc([256], BF16) for _ in range(2)]
        hshT_ = [A.alloc([2, 128], BF16) for _ in range(2)]
        t6 = [A.alloc([D], F32) for _ in range(2)]
        S.dma("sp", lambda e: e.dma_start(out=wr_f, in_=w_router.rearrange("(kt p) n -> p kt n", p=128)), writes=["wr_f"], slot="wr_f")
        S.op("act", lambda e: e.copy(wr_hi, wr_f), reads=["wr_f"], writes=["wr_hi"])
        S.op("dve", lambda e: e.tensor_tensor(wr_lo, wr_f, wr_hi, ALU.subtract), reads=["wr_f", "wr_hi"], writes=["wr_lo"])
        S.dma("pool", lambda e: e.dma_start(out=wsh[:, :, 0:256], in_=w_sg.rearrange("(kt p) n -> p kt n", p=128)), writes=["wsh0"], slot="wsh0")
        S.dma("pool", lambda e: e.dma_start(out=wsh[:, :, 256:512], in_=w_su.rearrange("(kt p) n -> p kt n", p=128)), writes=["wsh1"], slot="wsh1")
        S.dma("pool", lambda e: e.dma_start(out=wshd, in_=w_sd.rearrange("(kt p) n -> p kt n", p=128)), writes=["wshd"], slot="wshd")
        bc_load(rbias, router_bias[0, :], "rbias", "rbias")
        S.dma("sp", lambda e: e.dma_start(out=iota1, in_=iota1_d), writes=["iota1"], slot="iota1")
        S.op("dve", lambda e: e.memset(run_bc, 0.0), writes=["run_bc"])
        x1all = A.alloc([NT, D], F32)
        sgs_ = [A.alloc([256], F32) for _ in range(2)]
        for t in range(NT):
            S.dma("sp", lambda e, t=t: e.dma_start(out=x1all[:, t, :], in_=x1_scr[t * 128:(t + 1) * 128, :]),
                  writes=[("x1t", t)], slot="x1t%d" % (t % 4))
        for t in range(NT):
            S.op("act", lambda e, t=t: e.activation(sq5_[0], x1all[:, t, :], AF.Square, accum_out=st5[:, t, 0:1]),
                 reads=[("x1t", t)], writes=["sq5", ("st5", t)])
        ST5 = [("st5", t) for t in range(NT)]
        S.op("act", lambda e: e.activation(st5[:, :, 1], st5[:, :, 0], AF.Sqrt, bias=EPS, scale=1.0 / D),
             reads=ST5, writes=["st5b"])
        S.op("dve", lambda e: e.reciprocal(st5[:, :, 2], st5[:, :, 1]), reads=["st5b"], writes=["st5c"])

        def stage5A(t):
            b = t % 2
            S.op("dve", lambda e: e.scalar_tensor_tensor(
                tmp2[b], x1all[:, t, :], st5[:, t, 2:3], ada[:, 4, :], ALU.mult, ALU.mult),
                reads=[("x1t", t), "st5c"] + ADA(4), writes=[("tmp2", b)])
            S.op("pool", lambda e: e.tensor_tensor(tmp2[b], tmp2[b], ada[:, 3, :], ALU.add),
                 reads=[("tmp2", b)] + ADA(3), writes=[("tmp2", b)])
            S.op("act", lambda e: e.copy(h2hi[b], tmp2[b]), reads=[("tmp2", b)], writes=[("h2hi", b)])
            S.op("dve", lambda e: e.tensor_tensor(h2lo[b], tmp2[b], h2hi[b], ALU.subtract),
                 reads=[("tmp2", b), ("h2hi", b)], writes=[("h2lo", b)])
            S.dma("sp", lambda e: e.dma_start(out=h2_scr[t * 128:(t + 1) * 128, :], in_=h2hi[b]),
                  reads=[("h2hi", b)], writes=[("h2_scr", t)], slot="h2st%d" % b)
            for kt in range(KT):
                S.op("pe", lambda e, kt=kt: e.transpose(psb[0][:, kt * 128:(kt + 1) * 128], h2hi[b][:, kt * 128:(kt + 1) * 128], ident),
                     reads=[("h2hi", b), "ident"], writes=[PS(0)])
            S.op("act", lambda e: e.copy(h2T[b], psb[0][:, :].rearrange("p (a b) -> p a b", a=8)),
                 reads=[PS(0)], writes=[("h2T", b)])
            for kt in range(KT):
                S.op("pe", lambda e, kt=kt: e.transpose(psb[1][:, kt * 128:(kt + 1) * 128], h2lo[b][:, kt * 128:(kt + 1) * 128], ident),
                     reads=[("h2lo", b), "ident"], writes=[PS(1)])
            S.op("dve", lambda e: e.tensor_copy(h2loT[b], psb[1][:, :].rearrange("p (a b) -> p a b", a=8)),
                 reads=[PS(1)], writes=[("h2loT", b)])

        def stage5B(t):
            b = t % 2
            scb = scb_[b]; biased = biased_[b]; wsel = wsel_[b]; maskb = maskb_[b]
            sg = sg_[b]; sgs = sgs_[b]; hsh = hsh_[b]; hshT = hshT_[b]
            nmm = 0
            for (xi, wi) in [(0, 0), (0, 1), (1, 0)]:
                for kt in range(KT):
                    lt = h2T[b] if xi == 0 else h2loT[b]
                    wt = wr_hi if wi == 0 else wr_lo
                    S.op("pe", lambda e, lt=lt, wt=wt, kt=kt, nmm=nmm: e.matmul(
                        ps[2][:, 0:NE], lt[:, kt, :], wt[:, kt, :], start=(nmm == 0), stop=(nmm == 23)),
                        reads=[("h2T", b), ("h2loT", b), "wr_hi", "wr_lo"], writes=[PS(2)])
                    nmm += 1
            for kt in range(KT):
                S.op("pe", lambda e, kt=kt: e.matmul(
                    ps[3][:, :], h2T[b][:, kt, :], wsh[:, kt, :], start=(kt == 0), stop=(kt == KT - 1)),
                    reads=[("h2T", b), "wsh0", "wsh1"], writes=[PS(3)])
            S.op("act", lambda e: e.activation(scb, ps[2][:, 0:NE], AF.Sigmoid), reads=[PS(2)], writes=[("scb", b)])
            S.op("act", lambda e: e.activation(sgs, ps[3][:, 0:256], AF.Sigmoid), reads=[PS(3)], writes=[("sgs", b)])
            S.op("dve", lambda e: e.tensor_tensor(biased, scb, rbias, ALU.add), reads=[("scb", b), "rbias"], writes=[("biased", b)])
            S.op("dve", lambda e: e.max(top8[:, t, :], biased), reads=[("biased", b)], writes=[("top8", t)])
            S.op("dve", lambda e: e.scalar_tensor_tensor(
                wsel, biased, top8[:, t, 7:8], scb, ALU.is_ge, ALU.mult, accum_out=st5[:, t, 3:4]),
                reads=[("biased", b), ("top8", t), ("scb", b)], writes=[("wsel", b), ("den", t)])
            S.op("dve", lambda e: e.tensor_scalar(maskb, biased, top8[:, t, 7:8], None, ALU.is_ge),
                 reads=[("biased", b), ("top8", t)], writes=[("maskb", b)])
            S.op("dve", lambda e: e.reciprocal(st5[:, t, 4:5], st5[:, t, 3:4]), reads=[("den", t)], writes=[("rden5", t)])
            S.op("dve", lambda e: e.tensor_scalar(Gw[:, t, :], wsel, st5[:, t, 4:5], 2.5, ALU.mult, ALU.mult),
                 reads=[("wsel", b), ("rden5", t)], writes=[("Gw", t)])
            S.op("pe", lambda e: e.matmul(ps[4][:, 0:NE], tri, maskb, start=True, stop=True),
                 reads=["tri", ("maskb", b)], writes=[PS(4)])
            S.op("pe", lambda e: e.matmul(ps[4][:, NE:2 * NE], ones, maskb, start=True, stop=True),
                 reads=["ones", ("maskb", b)], writes=[PS(4)])
            S.op("dve", lambda e: e.tensor_tensor(Pos[:, t, :], ps[4][:, 0:NE], run_bc, ALU.add),
                 reads=[PS(4), "run_bc"], writes=[("Pos", t)])
            S.op("dve", lambda e: e.tensor_tensor(run_bc, ps[4][:, NE:2 * NE], run_bc, ALU.add),
                 reads=[PS(4), "run_bc"], writes=["run_bc"])
            S.op("dve", lambda e: e.tensor_tensor(sg, ps[3][:, 0:256], sgs, ALU.mult), reads=[PS(3), ("sgs", b)], writes=[("sg", b)])
            S.op("dve", lambda e: e.tensor_tensor(hsh, ps[3][:, 256:512], sg, ALU.mult), reads=[PS(3), ("sg", b)], writes=[("hsh", b)])
            for j in range(2):
                S.op("pe", lambda e, j=j: e.transpose(psb[5][:, j * 128:(j + 1) * 128], hsh[:, j * 128:(j + 1) * 128], ident),
                     reads=[("hsh", b), "ident"], writes=[PS(5)])
            S.op("act", lambda e: e.copy(hshT, psb[5][:, 0:256].rearrange("p (a b) -> p a b", a=2)), reads=[PS(5)], writes=[("hshT", b)])
            for hf in range(2):
                for j in range(2):
                    S.op("pe", lambda e, hf=hf, j=j: e.matmul(
                        ps[6 + hf][:, :], hshT[:, j, :], wshd[:, j, hf * 512:(hf + 1) * 512], start=(j == 0), stop=(j == 1)),
                        reads=[("hshT", b), "wshd"], writes=[PS(6 + hf)])
                S.op("dve", lambda e, hf=hf: e.tensor_tensor(
                    t6[b][:, hf * 512:(hf + 1) * 512], ps[6 + hf][:, :], ada[:, 5, hf * 512:(hf + 1) * 512], ALU.mult),
                    reads=[PS(6 + hf)] + ADA(5), writes=[("t6", b, hf)])
            S.op("pool", lambda e: e.tensor_tensor(t6[b], t6[b], x1all[:, t, :], ALU.add),
                 reads=[("t6", b, 0), ("t6", b, 1), ("x1t", t)], writes=[("t6", b, 0), ("t6", b, 1)])
            S.dma("sp", lambda e: e.dma_start(out=base_scr[t * 128:(t + 1) * 128, :], in_=t6[b]),
                  reads=[("t6", b, 0), ("t6", b, 1)], writes=[("base_scr", t)], slot="bst%d" % b)

        stage5A(0)
        for t in range(NT):
            if t + 1 < NT:
                stage5A(t + 1)
            stage5B(t)
        S.barrier()
        A.release(m7)

        a1_ = [A.alloc([NE], F32) for _ in range(2)]
        a2_ = [A.alloc([NE], F32) for _ in range(2)]
        maddr_ = [A.alloc([NE], F32) for _ in range(2)]
        junk6 = [A.alloc([NE], F32) for _ in range(2)]
        top8a = A.alloc([NT, 8], F32)
        h2r = [A.alloc([D], BF16) for _ in range(2)]
        nex = A.alloc([NE], F32)
        pfx = [A.alloc([NE], F32) for _ in range(2)]
        base2 = A.alloc([NE], F32)
        d12 = A.alloc([NE], F32)
        bexp = A.alloc([R_OVF], F32)
        iokp = A.alloc([8], F32)
        wif = A.alloc([R_OVF], F32)
        onesf = A.alloc([NE], F32)
        S.op("pool", lambda e: e.memset(onesf, 1.0), writes=["onesf"])
        S.dma("sp", lambda e: e.dma_start(out=iokp, in_=iokp_d), writes=["iokp"], slot="iokp")
        S.op("dve", lambda e: e.tensor_scalar(nex, run_bc, float(C1), None, ALU.is_gt), reads=["run_bc"], writes=["nex"])
        for j in range(1, -(-(S_TOK - C1) // 128)):
            S.op("dve", lambda e, j=j: e.scalar_tensor_tensor(nex, run_bc, float(C1 + 128 * j), nex, ALU.is_gt, ALU.add),
                 reads=["run_bc", "nex"], writes=["nex"])
        S.op("dve", lambda e: e.tensor_copy(pfx[0], nex), reads=["nex"], writes=[("pfx", 0)])
        cur = 0
        sh = 1
        while sh < NE:
            nxt = 1 - cur
            S.op("dve", lambda e, cur=cur, nxt=nxt, sh=sh: e.tensor_tensor(pfx[nxt][:, sh:NE], pfx[cur][:, sh:NE], pfx[cur][:, 0:NE - sh], ALU.add),
                 reads=[("pfx", cur)], writes=[("pfx", nxt)])
            S.op("dve", lambda e, cur=cur, nxt=nxt, sh=sh: e.tensor_copy(pfx[nxt][:, 0:sh], pfx[cur][:, 0:sh]),
                 reads=[("pfx", cur)], writes=[("pfx", nxt)])
            cur = nxt
            sh *= 2
        obend = pfx[cur]
        OBK = ("pfx", cur)
        S.op("dve", lambda e: e.tensor_tensor(base2, obend, nex, ALU.subtract), reads=[OBK, "nex"], writes=["base2"])
        S.op("dve", lambda e: e.tensor_scalar(base2, base2, 128.0, float(NROWS1 - C1), ALU.mult, ALU.add), reads=["base2"], writes=["base2"])
        S.op("dve", lambda e: e.tensor_tensor(d12, iota1, base2, ALU.subtract), reads=["iota1", "base2"], writes=["d12"])
        for bq in range(R_OVF):
            S.op("dve", lambda e, bq=bq: e.scalar_tensor_tensor(
                junk6[bq % 2], obend, float(bq), onesf, ALU.is_le, ALU.mult, accum_out=bexp[:, bq:bq + 1]),
                reads=[OBK, "onesf"], writes=[("junk6", bq % 2), ("bexp", bq)])
        BEXP = [("bexp", bq) for bq in range(R_OVF)]
        S.op("dve", lambda e: e.tensor_scalar_min(bexp, bexp, float(NE - 1)), reads=BEXP, writes=["bexpc"])
        S.op("dve", lambda e: e.scalar_tensor_tensor(
            wif, bexp, 128.0, iokp[:, 0:1].to_broadcast([128, R_OVF]), ALU.mult, ALU.add),
            reads=["bexpc", "iokp"], writes=["wif"])
        S.op("dve", lambda e: e.tensor_copy(widx, wif), reads=["wif"], writes=["widx"])
        for t in range(NT):
            b = t % 2
            a1 = a1_[b]; a2 = a2_[b]; maddr = maddr_[b]
            S.op("dve", lambda e, t=t, a1=a1: e.scalar_tensor_tensor(a1, Pos[:, t, :], float(C1), d12, ALU.is_lt, ALU.mult),
                 reads=[("Pos", t), "d12"], writes=[("a1", b)])
            S.op("dve", lambda e, t=t, a2=a2: e.tensor_tensor(a2, Pos[:, t, :], base2, ALU.add),
                 reads=[("Pos", t), "base2"], writes=[("a2", b)])
            S.op("dve", lambda e, a1=a1, a2=a2: e.tensor_tensor(a1, a1, a2, ALU.add), reads=[("a1", b), ("a2", b)], writes=[("a1", b)])
            S.op("dve", lambda e, t=t, a1=a1: e.scalar_tensor_tensor(Gw[:, t, :], a1, float(NROWS), Gw[:, t, :], ALU.is_lt, ALU.mult),
                 reads=[("Gw", t), ("a1", b)], writes=[("Gw", t)])
            S.op("dve", lambda e, t=t, a1=a1, maddr=maddr: e.scalar_tensor_tensor(maddr, Gw[:, t, :], 0.0, a1, ALU.is_gt, ALU.mult),
                 reads=[("Gw", t), ("a1", b)], writes=[("maddr", b)])
            S.op("dve", lambda e, t=t, maddr=maddr: e.max(top8a[:, t, :], maddr), reads=[("maddr", b)], writes=[("top8a", t)])
            S.op("dve", lambda e, t=t: e.tensor_copy(idx[:, t, :], top8a[:, t, :]), reads=[("top8a", t)], writes=[("idx", t)])
            for k in range(8):
                jb = k % 2
                S.op("dve", lambda e, t=t, k=k, jb=jb, maddr=maddr: e.scalar_tensor_tensor(
                    junk6[jb], maddr, top8a[:, t, k:k + 1], Gw[:, t, :], ALU.is_equal, ALU.mult, accum_out=gwk[:, t, k:k + 1]),
                    reads=[("maddr", b), ("top8a", t), ("Gw", t)], writes=[("junk6", jb), ("gwk", t, k)])
            S.dma("sp", lambda e, t=t, b=b: e.dma_start(out=h2r[b], in_=h2_scr[t * 128:(t + 1) * 128, :]),
                  writes=[("h2r", b)], slot="h2r%d" % b)
            for k in range(8):
                S.dma("pool", lambda e, t=t, b=b, k=k: e.indirect_dma_start(
                    out=xdisp[:, :], out_offset=bass.IndirectOffsetOnAxis(ap=idx[:, t, k:k + 1], axis=0),
                    in_=h2r[b], in_offset=None),
                    reads=[("h2r", b), ("idx", t)], writes=[("xdisp", t, k)], slot="sc%d_%d" % (b, k))
        S.barrier()
        A.release(m6)

        m8 = A.mark()
        NXB = 4
        xs = [A.alloc([2, D], BF16) for _ in range(NXB)]
        XT = [A.alloc([KT, 256], BF16) for _ in range(2)]
        NWB = 6
        wg = [A.alloc([KT, 256], BF16) for _ in range(NWB)]
        wu = [A.alloc([KT, 256], BF16) for _ in range(NWB)]
        wd = [A.alloc([2, D], BF16) for _ in range(NWB)]
        sgb = [A.alloc([2, 256], F32) for _ in range(2)]
        HT = [A.alloc([2, 256], BF16) for _ in range(2)]
        ysb = [A.alloc([2, D], F32) for _ in range(2)]
        w32g = [A.alloc([KT, 256], F32) for _ in range(2)]
        w32u = [A.alloc([KT, 256], F32) for _ in range(2)]
        w32d = [A.alloc([2, D], F32) for _ in range(2)]
        weg_rows = w_eg.rearrange("e (p kt) f -> (e p) (kt f)", kt=KT)
        weu_rows = w_eu.rearrange("e (p kt) f -> (e p) (kt f)", kt=KT)
        wed_rows = w_ed.rearrange("e (p j) d -> (e p) (j d)", j=2)
        S.op("dve", lambda e: e.memset(ysb[0], 0.0), writes=[("ysb", 0, 0, 0), ("ysb", 0, 0, 1), ("ysb", 0, 1, 0), ("ysb", 0, 1, 1)])
        S.dma("sp", lambda e: e.dma_start(out=yscr[0:ROW0, :], in_=ysb[0][:, 0, :]),
              reads=[("ysb", 0, 0, 0), ("ysb", 0, 0, 1)], writes=["yscr_trash"], slot="yst0_0")
        for xb_ in range(NXB):
            S.op("pool", lambda e, xb_=xb_: e.memset(xs[xb_], 0.0), writes=[("xs", xb_)])
        units = [("s", e_, ROW0 + e_ * C1, 2) for e_ in range(NE)] + [("d", bq, NROWS1 + bq * 128, 1) for bq in range(R_OVF)]

        def load_x(u):
            kind, ui, r0_, nblk = units[u]
            xb_ = u % NXB
            S.dma("sp", lambda e: e.dma_start(out=xs[xb_][:, 0, :], in_=xdisp[r0_:r0_ + 128, :]),
                  writes=[("xs", xb_, 0)], reads=[("xs", xb_)], slot="xs%d_0" % xb_)
            if nblk == 2:
                S.dma("sp", lambda e: e.dma_start(out=xs[xb_][0:C1 - 128, 1, :], in_=xdisp[r0_ + 128:r0_ + C1, :]),
                      writes=[("xs", xb_, 1)], reads=[("xs", xb_)], slot="xs%d_1" % xb_)

        for u in range(NXB - 1):
            load_x(u)
        for u, (kind, ui, r0, nblk) in enumerate(units):
            b = u % 2
            xbi = u % NXB
            wb = u % NWB
            ns = nblk * 128
            if u + NXB - 1 < len(units):
                load_x(u + NXB - 1)
            if kind == "s":
                S.dma("pool", lambda e, wb=wb, ui=ui: e.dma_start(out=wg[wb], in_=w_eg[ui].rearrange("(kt p) f -> p kt f", p=128)),
                      writes=[("wg", wb)], slot="wg%d" % wb)
                S.dma("pool", lambda e, wb=wb, ui=ui: e.dma_start(out=wu[wb], in_=w_eu[ui].rearrange("(kt p) f -> p kt f", p=128)),
                      writes=[("wu", wb)], slot="wu%d" % wb)
                S.dma("pool", lambda e, wb=wb, ui=ui: e.dma_start(out=wd[wb], in_=w_ed[ui].rearrange("(j p) d -> p j d", p=128)),
                      writes=[("wd", wb)], slot="wd%d" % wb)
            else:
                db = ui % 2
                for (dst, rows_ap, nm) in ((w32g[db], weg_rows, "g"), (w32u[db], weu_rows, "u"), (w32d[db], wed_rows, "d")):
                    S.dma("pool", lambda e, dst=dst, rows_ap=rows_ap, ui=ui: e.indirect_dma_start(
                        out=dst.rearrange("p a b -> p (a b)"), out_offset=None, in_=rows_ap[:, :],
                        in_offset=bass.IndirectOffsetOnAxis(ap=widx[:, ui:ui + 1], axis=0)),
                        reads=["widx"], writes=[("w32" + nm, db)], slot="dyn_" + nm)
                S.op("act", lambda e, db=db, wb=wb: e.copy(wg[wb], w32g[db]),
                     reads=[("w32g", db)], writes=[("wg", wb)])
                S.op("dve", lambda e, db=db, wb=wb: e.tensor_copy(wu[wb], w32u[db]),
                     reads=[("w32u", db)], writes=[("wu", wb)])
                S.op("act", lambda e, db=db, wb=wb: e.copy(wd[wb], w32d[db]),
                     reads=[("w32d", db)], writes=[("wd", wb)])
            for blk in range(nblk):
                for kt in range(KT):
                    xin_ = xs[xbi][:, blk, kt:D:KT] if kind == "d" else xs[xbi][:, blk, kt * 128:(kt + 1) * 128]
                    S.op("pe", lambda e, blk=blk, kt=kt, xin_=xin_: e.transpose(
                        psb[blk][:, kt * 128:(kt + 1) * 128], xin_, ident),
                        reads=[("xs", xbi, blk), "ident"], writes=[PS(blk)])
                if blk == 0:
                    S.op("act", lambda e, b=b: e.copy(XT[b][:, :, 0:128], psb[0][:, :].rearrange("p (a b) -> p a b", a=8)),
                         reads=[PS(0)], writes=[("XT", b, 0)])
                else:
                    S.op("act", lambda e, b=b: e.copy(XT[b][:, :, 128:256], psb[1][:, :].rearrange("p (a b) -> p a b", a=8)),
                         reads=[PS(1)], writes=[("XT", b, 1)])
            XTK = [("XT", b, blk) for blk in range(nblk)]
            for fo in range(2):
                for (wt, wk, c0) in ((wg[wb], ("wg", wb), 0), (wu[wb], ("wu", wb), 256)):
                    for kt in range(KT):
                        wcol = wt[:, kt, fo:256:2] if kind == "d" else wt[:, kt, fo * 128:(fo + 1) * 128]
                        S.op("pe", lambda e, b=b, fo=fo, wcol=wcol, c0=c0, kt=kt, ns=ns: e.matmul(
                            ps[2 + fo][:, c0:c0 + ns], wcol, XT[b][:, kt, 0:ns],
                            start=(kt == 0), stop=(kt == KT - 1)),
                            reads=[wk] + XTK, writes=[PS(2 + fo)])
                S.op("act", lambda e, b=b, fo=fo, ns=ns: e.activation(sgb[b][:, fo, 0:ns], ps[2 + fo][:, 0:ns], AF.Silu),
                     reads=[PS(2 + fo)], writes=[("sgb", b, fo)])
                S.op("dve", lambda e, b=b, fo=fo, ns=ns: e.tensor_tensor(HT[b][:, fo, 0:ns], ps[2 + fo][:, 256:256 + ns], sgb[b][:, fo, 0:ns], ALU.mult),
                     reads=[PS(2 + fo), ("sgb", b, fo)], writes=[("HT", b, fo)])
            for blk in range(nblk):
                for hf in range(2):
                    pi = 4 + 2 * blk + hf
                    for fo in range(2):
                        S.op("pe", lambda e, b=b, wb=wb, blk=blk, hf=hf, fo=fo, pi=pi: e.matmul(
                            ps[pi][:, :], HT[b][:, fo, blk * 128:(blk + 1) * 128], wd[wb][:, fo, hf * 512:(hf + 1) * 512],
                            start=(fo == 0), stop=(fo == 1)),
                            reads=[("HT", b, 0), ("HT", b, 1), ("wd", wb)], writes=[PS(pi)])
                    S.op("dve", lambda e, b=b, blk=blk, pi=pi, hf=hf: e.tensor_tensor(
                        ysb[b][:, blk, hf * 512:(hf + 1) * 512], ps[pi][:, :], ada[:, 5, hf * 512:(hf + 1) * 512], ALU.mult),
                        reads=[PS(pi)] + ADA(5), writes=[("ysb", b, blk, hf)])
            for blk in range(nblk):
                nr = 128 if (blk == 0 or kind == "d") else C1 - 128
                S.dma("sp", lambda e, b=b, blk=blk, r0=r0, nr=nr: e.dma_start(
                    out=yscr[r0 + blk * 128:r0 + blk * 128 + nr, :], in_=ysb[b][0:nr, blk, :]),
                    reads=[("ysb", b, blk, 0), ("ysb", b, blk, 1)], writes=[("yscr", u, blk)], slot="yst%d_%d" % (b, blk))
        S.barrier()
        A.release(m8)

        yg = [[A.alloc([D], F32) for _ in range(8)] for _ in range(2)]
        accA = [A.alloc([D], F32) for _ in range(2)]
        baser = [A.alloc([D], F32) for _ in range(2)]
        for b in range(2):
            for k in range(8):
                S.op("pool" if k % 2 else "dve", lambda e, b=b, k=k: e.memset(yg[b][k], 0.0), writes=[("yg", b, k, 0), ("yg", b, k, 1)])
        for t in range(NT):
            b = t % 2
            S.dma("sp", lambda e, t=t, b=b: e.dma_start(out=baser[b], in_=base_scr[t * 128:(t + 1) * 128, :]),
                  writes=[("baser", b)], slot="baser%d" % b)
            for k in range(8):
                S.dma("pool", lambda e, t=t, b=b, k=k: e.indirect_dma_start(
                    out=yg[b][k], out_offset=None, in_=yscr[:, :],
                    in_offset=bass.IndirectOffsetOnAxis(ap=idx[:, t, k:k + 1], axis=0)),
                    reads=[("yg", b, k, 0), ("yg", b, k, 1)], writes=[("yg", b, k, 0), ("yg", b, k, 1)], slot="ga%d_%d" % (b, k))
            for k in range(8):
                prev = baser[b] if k == 0 else accA[b]
                pk = ("baser", b) if k == 0 else ("accA", b)
                S.op("dve", lambda e, t=t, b=b, k=k, prev=prev: e.scalar_tensor_tensor(
                    accA[b], yg[b][k], gwk[:, t, k:k + 1], prev, ALU.mult, ALU.add),
                    reads=[("yg", b, k, 0), ("yg", b, k, 1), pk], writes=[("accA", b)])
            S.dma("sp", lambda e, t=t, b=b: e.dma_start(out=out[t * 128:(t + 1) * 128, :], in_=accA[b]),
                  reads=[("accA", b)], writes=[("out", t)], slot="ost%d" % b)
        st = S.emit()
        print("sched", st, "arena peak KiB", A.peak / 512.0)
    return nc


def _consts():
    n = np.arange(S_TOK, dtype=np.float64)
    ang = 2 * np.pi * np.outer(n, n) / float(S_TOK)
    dftc = np.cos(ang).astype(ml_dtypes.bfloat16)
    dfts = (-np.sin(ang)).astype(ml_dtypes.bfloat16)
    c = np.arange(64, dtype=np.float64)
    a64 = 2 * np.pi * np.outer(c, c) / 64.0
    d64 = np.zeros((128, 256), np.float64)
    for g in range(2):
        d64[g * 64:(g + 1) * 64, g * 64:(g + 1) * 64] = np.cos(a64)
        d64[g * 64:(g + 1) * 64, 128 + g * 64:128 + (g + 1) * 64] = np.sin(a64)
    pos = np.arange(S_TOK, dtype=np.float32)
    inv = (np.float32(10000.0) ** (-np.arange(0, 32, 2, dtype=np.float32) / np.float32(32))).astype(np.float32)
    a = pos[:, None] * inv[None, :]
    rope = np.concatenate([np.cos(a), np.sin(a)], axis=1).astype(np.float32)
    ident = np.eye(128).astype(ml_dtypes.bfloat16)
    tri = (np.arange(128)[:, None] < np.arange(128)[None, :]).astype(ml_dtypes.bfloat16)
    ones = np.ones((128, 128), ml_dtypes.bfloat16)
    iota1 = np.broadcast_to((np.arange(NE, dtype=np.float32) * C1 + ROW0)[None, :], (128, NE)).copy()
    iokp = (np.arange(8, dtype=np.float32)[None, :] * 128 + np.arange(128, dtype=np.float32)[:, None]).astype(np.float32)
    return dict(dftc=dftc, dfts=dfts, dft64=d64.astype(ml_dtypes.bfloat16), rope=rope, ident=ident,
                tri=tri, ones=ones, iota1=iota1, iokp=iokp)


_W_NAMES = ["w_ada", "b_ada", "norm1_g", "w_in", "q_a_norm_g", "w_uq", "kv_a_norm_g", "w_ukv", "q_norm_g",
            "k_norm_g", "w_proj_attn", "w_proj_fourier", "w_out", "norm2_g", "w_router", "router_bias",
            "w_exp_gate", "w_exp_up", "w_exp_down", "w_sh_gate", "w_sh_up", "w_sh_down"]


def kernel(**inputs):
    n_cores = 8
    nc = build("full")
    shared = dict(_consts())
    for k in _W_NAMES:
        a = np.asarray(inputs[k])[0]
        if a.ndim == 1:
            a = a[None, :]
        shared[k] = np.ascontiguousarray(a)
    x = np.asarray(inputs["x"])
    c = np.asarray(inputs["c"])
    in_maps = []
    for b in range(n_cores):
        m = dict(shared)
        m["x"] = np.ascontiguousarray(x[b])
        m["c8"] = np.ascontiguousarray(c[b].reshape(8, 128).T)
        in_maps.append(m)
    res = run_bass_kernel_spmd(nc, in_maps, core_ids=list(range(n_cores)))
    return np.stack([np.asarray(r["out"]) for r in res.results], axis=0).astype(np.float32)
```

```python
import numpy as np
import ml_dtypes
from contextlib import ExitStack
import concourse.bass as bass
import concourse.mybir as mybir
from concourse.bass_utils import run_bass_kernel_spmd

F32 = mybir.dt.float32
BF16 = mybir.dt.bfloat16
I32 = mybir.dt.int32
ALU = mybir.AluOpType
AF = mybir.ActivationFunctionType
AX = mybir.AxisListType

S_TOK = 2048
D = 1024
NT = 16
KT = 8
NE = 256
C1 = 248
R_OVF = 12
ROW0 = 128
NROWS1 = ROW0 + NE * C1
NROWS = NROWS1 + R_OVF * 128
EPS = 1e-6
COMPUTE = ("pe", "act", "dve", "pool")


class Sched:
    def __init__(self, nc):
        self.nc = nc
        self.ops = []
        self.last_writer = {}
        self.readers = {}
        self.dma_last = {}
        self.last_on_eng = {}
        self.slotmap = {}

    def _add(self, eng, fn, reads, writes, dma_slot=None, extra_deps=()):
        i = len(self.ops)
        deps = set(extra_deps)
        for k in list(reads) + list(writes):
            w = self.last_writer.get(k)
            if w is not None:
                deps.add(w)
        for k in writes:
            for r in self.readers.get(k, ()):
                deps.add(r)
        if dma_slot is not None:
            qk = "sw" if eng == "pool" else "hw"
            sm = self.slotmap.setdefault(qk, {})
            dma_slot = (qk, sm.setdefault(dma_slot, len(sm)))
            p = self.dma_last.get(dma_slot)
            if p is not None:
                deps.add(p)
            self.dma_last[dma_slot] = i
        elif fn is not None and eng in COMPUTE:
            self.last_on_eng[eng] = i
        deps.discard(i)
        if eng == "pe" and dma_slot is None:
            deps = {d for d in deps if not (self.ops[d]["eng"] == "pe" and self.ops[d]["dma"] is None)}
        latest = {}
        keep = set()
        for d in deps:
            od = self.ops[d]
            if od["dma"] is None and od["fn"] is not None:
                if latest.get(od["eng"], -1) < d:
                    latest[od["eng"]] = d
            else:
                keep.add(d)
        deps = keep | set(latest.values())
        self.ops.append(dict(eng=eng, fn=fn, deps=deps, dma=dma_slot, signal=False))
        for k in writes:
            self.last_writer[k] = i
            self.readers[k] = []
        for k in reads:
            lst = self.readers.setdefault(k, [])
            if dma_slot is None:
                lst[:] = [r for r in lst if not (self.ops[r]["dma"] is None and self.ops[r]["eng"] == eng)]
            lst.append(i)
        return i

    def op(self, eng, fn, reads=(), writes=()):
        return self._add(eng, fn, reads, writes)

    def dma(self, queue, fn, reads=(), writes=(), slot=None):
        return self._add(queue, fn, reads, writes, dma_slot=slot)

    def barrier(self):
        deps = set(self.last_on_eng.values()) | set(self.dma_last.values())
        for e in ("pe", "act", "dve", "pool", "sp"):
            self._add(e, None, (), (), extra_deps=deps)
        self.last_writer = {}
        self.readers = {}
        self.slotmap = {}

    def emit(self):
        nc = self.nc
        ops = self.ops
        for o in ops:
            for d in o["deps"]:
                ops[d]["signal"] = True
        seq = {e: 0 for e in COMPUTE}
        dma_cnt = {}
        for o in ops:
            if o["dma"] is not None:
                dma_cnt[o["dma"]] = dma_cnt.get(o["dma"], 0) + 1
                o["semkey"] = ("dma", o["dma"])
                o["semval"] = 16 * dma_cnt[o["dma"]]
            elif o["signal"]:
                assert o["fn"] is not None
                seq[o["eng"]] += 1
                o["semkey"] = ("eng", o["eng"])
                o["semval"] = seq[o["eng"]]
        semkeys = [("eng", e) for e in COMPUTE] + [("dma", s) for s in dma_cnt]
        with ExitStack() as es:
            sems = {}
            for n, k in enumerate(semkeys):
                sems[k] = es.enter_context(nc.semaphore("sm%d" % n))
            streams = {e: [] for e in ("pe", "act", "dve", "pool", "sp")}
            for i, o in enumerate(ops):
                streams[o["eng"]].append(i)
            block = es.enter_context(nc.Block())
            engmap = {"pe": "tensor", "act": "scalar", "dve": "vector", "pool": "gpsimd", "sp": "sync"}

            def make(ename):
                def body(eng):
                    known = {}
                    for i in streams[ename]:
                        o = ops[i]
                        need = {}
                        for d in o["deps"]:
                            od = ops[d]
                            k, v = od["semkey"], od["semval"]
                            if known.get(k, 0) >= v:
                                continue
                            if need.get(k, 0) < v:
                                need[k] = v
                        for k, v in need.items():
                            eng.wait_ge(sems[k], v)
                            known[k] = v
                        if o["fn"] is None:
                            continue
                        ins = o["fn"](eng)
                        if o["dma"] is not None:
                            ins.then_inc(sems[o["semkey"]], 16)
                        elif o["signal"]:
                            ins.then_inc(sems[o["semkey"]], 1)
                    if ename == "sp":
                        for s, c in dma_cnt.items():
                            eng.wait_ge(sems[("dma", s)], 16 * c)
                        for e in COMPUTE:
                            if seq[e] > 0:
                                eng.wait_ge(sems[("eng", e)], seq[e])
                return body

            for ename, attr in engmap.items():
                getattr(block, attr)(make(ename))
        return dict(n_ops=len(ops), seq=seq, n_sems=len(semkeys))


class Arena:
    def __init__(self, ten, nunits):
        self.t = ten
        self.n = nunits
        self.off = 0
        self.peak = 0

    def alloc(self, shape, dtype, parts=128):
        size = {F32: 4, BF16: 2, I32: 4}[dtype]
        nel = int(np.prod(shape))
        units = (nel * size + 1) // 2
        units = (units + 31) // 32 * 32
        assert self.off + units <= self.n, ("arena overflow", self.off, units, self.n)
        v = self.t[0:parts, self.off:self.off + units]
        self.off += units
        self.peak = max(self.peak, self.off)
        if size == 4:
            v = v.bitcast(dtype)
        v = v[:, 0:nel]
        if len(shape) == 2:
            v = v.rearrange("p (a b) -> p a b", a=shape[0])
        elif len(shape) == 3:
            v = v.rearrange("p (a b c) -> p a b c", a=shape[0], b=shape[1])
        return v

    def mark(self):
        return self.off

    def release(self, m):
        self.off = m


def build(stage="full"):
    nc = bass.Bass("TRN2", target_bir_lowering=False)

    def din(name, shape, dt=F32):
        return nc.dram_tensor(name, list(shape), dt, kind="ExternalInput").ap()

    x = din("x", [S_TOK, D])
    c8 = din("c8", [128, 8])
    w_ada = din("w_ada", [D, 6 * D])
    b_ada = din("b_ada", [1, 6 * D])
    norm1_g = din("norm1_g", [1, D])
    w_in = din("w_in", [D, 2976])
    q_a_norm_g = din("q_a_norm_g", [1, 256])
    w_uq = din("w_uq", [256, 768])
    kv_a_norm_g = din("kv_a_norm_g", [1, 128])
    w_ukv = din("w_ukv", [128, 1024])
    q_norm_g = din("q_norm_g", [1, 96])
    k_norm_g = din("k_norm_g", [1, 96])
    w_pa = din("w_proj_attn", [512, D])
    w_pf = din("w_proj_fourier", [512, D])
    w_out = din("w_out", [D, D])
    norm2_g = din("norm2_g", [1, D])
    w_router = din("w_router", [D, NE])
    router_bias = din("router_bias", [1, NE])
    if stage == "full":
        w_eg = din("w_exp_gate", [NE, D, 256])
        w_eu = din("w_exp_up", [NE, D, 256])
        w_ed = din("w_exp_down", [NE, 256, D])
    w_sg = din("w_sh_gate", [D, 256])
    w_su = din("w_sh_up", [D, 256])
    w_sd = din("w_sh_down", [256, D])
    dftc = din("dftc", [S_TOK, S_TOK], BF16)
    dfts = din("dfts", [S_TOK, S_TOK], BF16)
    dft64 = din("dft64", [128, 256], BF16)
    rope = din("rope", [S_TOK, 32])
    ident_d = din("ident", [128, 128], BF16)
    tri_d = din("tri", [128, 128], BF16)
    ones_d = din("ones", [128, 128], BF16)
    iota1_d = din("iota1", [128, NE])
    iokp_d = din("iokp", [128, 8])

    out = nc.dram_tensor("out", [S_TOK, D], F32, kind="ExternalOutput").ap()

    def dscr(name, shape, dt):
        return nc.dram_tensor(name, list(shape), dt).ap()

    hT_scr = dscr("hT_scr", [128, KT * S_TOK], BF16)
    FT_scr = dscr("FT_scr", [128, 4 * S_TOK], BF16)
    AT_scr = dscr("AT_scr", [128, 4 * S_TOK], BF16)
    x1_scr = dscr("x1_scr", [S_TOK, D], F32)
    base_scr = dscr("base_scr", [S_TOK, D], F32)
    h2_scr = dscr("h2_scr", [S_TOK, D], BF16)
    xdisp = dscr("xdisp", [NROWS, D], BF16)
    yscr = dscr("yscr", [NROWS, D], F32)

    NUNITS = 205 * 512
    with ExitStack() as es:
        arena_t = es.enter_context(nc.sbuf_tensor("arena", [128, NUNITS], BF16))
        ps = [es.enter_context(nc.psum_tensor("ps%d" % i, [128, 512], F32)) for i in range(8)]
        psb = [p[:].bitcast(BF16) for p in ps]
        A = Arena(arena_t, NUNITS)
        S = Sched(nc)

        def PS(i):
            return ("ps", i)

        ident = A.alloc([128], BF16)
        tri = A.alloc([128], BF16)
        ones = A.alloc([128], BF16)
        S.dma("sp", lambda e: e.dma_start(out=ident, in_=ident_d), writes=["ident"], slot="c_ident")
        S.dma("sp", lambda e: e.dma_start(out=tri, in_=tri_d), writes=["tri"], slot="c_tri")
        S.dma("sp", lambda e: e.dma_start(out=ones, in_=ones_d), writes=["ones"], slot="c_ones")
        ada = A.alloc([6, D], F32)

        def bc_load(dst, src_row, key, slot):
            S.dma("sp", lambda e: e.dma_start(out=dst, in_=src_row.partition_broadcast(128)),
                  writes=[key], slot=slot)

        m0 = A.mark()
        hT = A.alloc([KT, S_TOK], BF16)
        m1 = A.mark()
        csil = A.alloc([8], F32)
        c_sb = A.alloc([8], F32)
        crep = A.alloc([8, 128], BF16)
        wa = [A.alloc([8, D], BF16) for _ in range(2)]
        bb = [A.alloc([D], F32) for _ in range(2)]
        gn = [A.alloc([D], F32) for _ in range(2)]
        S.dma("sp", lambda e: e.dma_start(out=c_sb, in_=c8), writes=["c_sb"], slot="c_sb")
        S.op("act", lambda e: e.activation(csil, c_sb, AF.Silu), reads=["c_sb"], writes=["csil"])
        S.op("dve", lambda e: e.tensor_copy(crep, csil.unsqueeze(2).to_broadcast([128, 8, 128])),
             reads=["csil"], writes=["crep"])
        bc_load(gn[0], norm1_g[0, :], "gn0", "gn0")
        bc_load(gn[1], norm2_g[0, :], "gn1", "gn1")
        for n, j in enumerate([1, 0, 2, 4, 3, 5]):
            b = n % 2
            S.dma("pool", lambda e, j=j, b=b: e.dma_start(
                out=wa[b], in_=w_ada[:, j * D:(j + 1) * D].rearrange("(kt p) n -> p kt n", p=128)),
                writes=[("wa", b)], slot="wa%d" % b)
            bc_load(bb[b], b_ada[0, j * D:(j + 1) * D], ("bb", b), "bb%d" % b)
            for half in range(2):
                for kt in range(KT):
                    S.op("pe", lambda e, b=b, half=half, kt=kt: e.matmul(
                        ps[half][:, :], crep[:, kt, :], wa[b][:, kt, half * 512:(half + 1) * 512],
                        start=(kt == 0), stop=(kt == KT - 1)),
                        reads=["crep", ("wa", b)], writes=[PS(half)])
                S.op("dve", lambda e, b=b, half=half, j=j: e.tensor_tensor(
                    ada[:, j, half * 512:(half + 1) * 512], ps[half][:, :], bb[b][:, half * 512:(half + 1) * 512], ALU.add),
                    reads=[PS(half), ("bb", b)], writes=[("ada", j, half)])
            if j in (1, 4):
                g = gn[0] if j == 1 else gn[1]
                gk = "gn0" if j == 1 else "gn1"
                S.op("dve", lambda e, j=j, g=g: e.scalar_tensor_tensor(
                    ada[:, j, :], ada[:, j, :], 1.0, g, ALU.add, ALU.mult),
                    reads=[("ada", j, 0), ("ada", j, 1), gk], writes=[("ada", j, 0), ("ada", j, 1)])
        ADA = lambda j: [("ada", j, 0), ("ada", j, 1)]

        xb = [A.alloc([D], F32) for _ in range(2)]
        tmpf = [A.alloc([D], F32) for _ in range(2)]
        hb = [A.alloc([D], BF16) for _ in range(2)]
        sq = A.alloc([D], F32)
        st1 = A.alloc([NT, 4], F32)
        for t in range(NT):
            b = t % 2
            S.dma("sp", lambda e, t=t, b=b: e.dma_start(out=xb[b], in_=x[t * 128:(t + 1) * 128, :]),
                  writes=[("xb", b)], slot="xb%d" % b)
            S.op("act", lambda e, t=t, b=b: e.activation(sq, xb[b], AF.Square, accum_out=st1[:, t, 0:1]),
                 reads=[("xb", b)], writes=["sq", ("st1", t)])
            S.op("act", lambda e, t=t: e.activation(st1[:, t, 1:2], st1[:, t, 0:1], AF.Sqrt, bias=EPS, scale=1.0 / D),
                 reads=[("st1", t)], writes=[("st1b", t)])
            S.op("dve", lambda e, t=t: e.reciprocal(st1[:, t, 2:3], st1[:, t, 1:2]),
                 reads=[("st1b", t)], writes=[("st1c", t)])
            S.op("dve", lambda e, t=t, b=b: e.scalar_tensor_tensor(
                tmpf[b], xb[b], st1[:, t, 2:3], ada[:, 1, :], ALU.mult, ALU.mult),
                reads=[("xb", b), ("st1c", t)] + ADA(1), writes=[("tmpf", b)])
            S.op("pool", lambda e, b=b: e.tensor_tensor(hb[b], tmpf[b], ada[:, 0, :], ALU.add),
                 reads=[("tmpf", b)] + ADA(0), writes=[("hb", b)])
            pb = 2 + b
            for kt in range(KT):
                S.op("pe", lambda e, b=b, kt=kt, pb=pb: e.transpose(
                    psb[pb][:, kt * 128:(kt + 1) * 128], hb[b][:, kt * 128:(kt + 1) * 128], ident),
                    reads=[("hb", b), "ident"], writes=[PS(pb)])
            S.op("act", lambda e, t=t, pb=pb: e.copy(
                hT[:, :, t * 128:(t + 1) * 128], psb[pb][:, :].rearrange("p (a b) -> p a b", a=8)),
                reads=[PS(pb)], writes=[("hT", t)])
        HT_ALL = [("hT", t) for t in range(NT)]
        S.dma("sp", lambda e: e.dma_start(out=hT_scr.rearrange("p (a b) -> p a b", a=KT), in_=hT),
              reads=HT_ALL, writes=["hT_scr"], slot="hT_st")
        S.barrier()
        A.release(m1)

        NORM = float(1.0 / np.sqrt(float(S_TOK * 64)))
        m2 = A.mark()
        win_f = A.alloc([KT, 512], BF16)
        d64 = A.alloc([256], BF16)
        zcs = A.alloc([NT, 4, 256], BF16)
        zft = [A.alloc([512], BF16) for _ in range(2)]
        S.dma("pool", lambda e: e.dma_start(out=win_f, in_=w_in[:, 416:928].rearrange("(kt p) n -> p kt n", p=128)),
              writes=["win_f"], slot="win_f")
        S.dma("sp", lambda e: e.dma_start(out=d64, in_=dft64), writes=["d64"], slot="d64")
        n = 0
        for cc in range(4):
            for qc in range(4):
                b = n % 2
                n += 1
                for kt in range(KT):
                    S.op("pe", lambda e, b=b, cc=cc, qc=qc, kt=kt: e.matmul(
                        ps[b][:, :], win_f[:, kt, cc * 128:(cc + 1) * 128], hT[:, kt, qc * 512:(qc + 1) * 512],
                        start=(kt == 0), stop=(kt == KT - 1)),
                        reads=["win_f"] + [("hT", qc * 4 + s) for s in range(4)], writes=[PS(b)])
                S.op("act", lambda e, b=b: e.copy(zft[b], ps[b][:, :]), reads=[PS(b)], writes=[("zft", b)])
                for sub in range(4):
                    t = qc * 4 + sub
                    pb = 2 + (sub % 2)
                    S.op("pe", lambda e, b=b, sub=sub, pb=pb: e.matmul(
                        ps[pb][:, 0:256], zft[b][:, sub * 128:(sub + 1) * 128], d64, start=True, stop=True),
                        reads=[("zft", b), "d64"], writes=[PS(pb)])
                    S.op("dve", lambda e, t=t, cc=cc, pb=pb: e.tensor_scalar_mul(
                        zcs[:, t, cc, :], ps[pb][:, 0:256], NORM),
                        reads=[PS(pb)], writes=[("zcs", t, cc)])
        FT = A.alloc([4, S_TOK], BF16)
        dbuf = [A.alloc([NT, 512], BF16) for _ in range(2)]
        n = 0
        for kc in range(4):
            for cs in range(2):
                b = n % 2
                n += 1
                src = dftc if cs == 0 else dfts
                S.dma("sp", lambda e, b=b, src=src, kc=kc: e.dma_start(
                    out=dbuf[b], in_=src[:, kc * 512:(kc + 1) * 512].rearrange("(nt p) k -> p nt k", p=128)),
                    writes=[("dbuf", b)], slot="dbuf%d" % b)
                for cc in range(4):
                    for nt in range(NT):
                        S.op("pe", lambda e, b=b, cc=cc, nt=nt, cs=cs: e.matmul(
                            ps[4 + cc][:, :], zcs[:, nt, cc, cs * 128:(cs + 1) * 128], dbuf[b][:, nt, :],
                            start=(cs == 0 and nt == 0), stop=(cs == 1 and nt == NT - 1)),
                            reads=[("zcs", nt, cc), ("dbuf", b)], writes=[PS(4 + cc)])
            for cc in range(4):
                eng = "act" if cc % 2 == 0 else "dve"
                if eng == "act":
                    S.op("act", lambda e, cc=cc, kc=kc: e.copy(FT[:, cc, kc * 512:(kc + 1) * 512], ps[4 + cc][:, :]),
                         reads=[PS(4 + cc)], writes=[("FT", cc, kc)])
                else:
                    S.op("dve", lambda e, cc=cc, kc=kc: e.tensor_copy(FT[:, cc, kc * 512:(kc + 1) * 512], ps[4 + cc][:, :]),
                         reads=[PS(4 + cc)], writes=[("FT", cc, kc)])
        S.dma("sp", lambda e: e.dma_start(out=FT_scr.rearrange("p (a b) -> p a b", a=4), in_=FT),
              reads=[("FT", cc, kc) for cc in range(4) for kc in range(4)], writes=["FT_scr"], slot="FT_st")
        S.barrier()
        A.release(m2)

        qT = A.alloc([8, S_TOK], BF16)
        kT = A.alloc([8, S_TOK], BF16)
        vb = A.alloc([NT, 8, 65], BF16)
        m3 = A.mark()
        win_a = A.alloc([KT, 416], BF16)
        wuq = A.alloc([2, 768], BF16)
        wukv = A.alloc([1024], BF16)
        ropet = A.alloc([NT, 32], F32)
        gqa = A.alloc([256], F32)
        gkva = A.alloc([128], F32)
        g96 = A.alloc([2, 96], F32)
        S.dma("pool", lambda e: e.dma_start(out=win_a, in_=w_in[:, 0:416].rearrange("(kt p) n -> p kt n", p=128)),
              writes=["win_a"], slot="win_a")
        S.dma("pool", lambda e: e.dma_start(out=wuq, in_=w_uq.rearrange("(kt p) n -> p kt n", p=128)),
              writes=["wuq"], slot="wuq")
        S.dma("pool", lambda e: e.dma_start(out=wukv, in_=w_ukv), writes=["wukv"], slot="wukv")
        S.dma("sp", lambda e: e.dma_start(out=ropet, in_=rope.rearrange("(t p) c -> p t c", p=128)),
              writes=["ropet"], slot="ropet")
        bc_load(gqa, q_a_norm_g[0, :], "gqa", "gqa")
        bc_load(gkva, kv_a_norm_g[0, :], "gkva", "gkva")
        bc_load(g96[:, 0, :], q_norm_g[0, :], "g96q", "g96q")
        bc_load(g96[:, 1, :], k_norm_g[0, :], "g96k", "g96k")
        S.op("dve", lambda e: e.tensor_scalar_mul(g96[:, 0, :], g96[:, 0, :], 96.0 ** -0.5), reads=["g96q"], writes=["g96q"])
        S.op("pool", lambda e: e.memset(vb, 1.0), writes=["vb_init"])
        NB2 = 2
        sqa = [A.alloc([768], F32) for _ in range(NB2)]
        st2 = A.alloc([NT, 40], F32)
        cqb = [A.alloc([384], BF16) for _ in range(NB2)]
        cT = [A.alloc([3, 128], BF16) for _ in range(NB2)]
        kr = [A.alloc([32], F32) for _ in range(NB2)]
        kraw = [A.alloc([8, 96], F32) for _ in range(NB2)]
        nbuf = [[A.alloc([8, 96], F32) for _ in range(2)] for _ in range(NB2)]
        rt = [[A.alloc([4, 8, 16], F32) for _ in range(2)] for _ in range(NB2)]
        qkb = [[A.alloc([8, 96], BF16) for _ in range(2)] for _ in range(NB2)]

        def norm_rope(t, which, src, src_keys, ss_col, dstT):
            w = which
            tb = t % NB2
            nb_ = nbuf[tb][w]
            rt_ = rt[tb][w]
            qb_ = qkb[tb][w]
            NK = ("nbuf", tb, w)
            gbc = g96[:, w:w + 1, :].to_broadcast([128, 8, 96])
            gkey = "g96q" if w == 0 else "g96k"
            S.op("act", lambda e: e.activation(st2[:, t, ss_col + 8:ss_col + 16], st2[:, t, ss_col:ss_col + 8],
                                               AF.Sqrt, bias=EPS, scale=1.0 / 96),
                 reads=[("ss", t, w)], writes=[("sd", t, w)])
            yield
            S.op("dve", lambda e: e.reciprocal(st2[:, t, ss_col:ss_col + 8], st2[:, t, ss_col + 8:ss_col + 16]),
                 reads=[("sd", t, w)], writes=[("rs", t, w)])
            yield
            S.op("dve", lambda e: e.tensor_tensor(
                nb_, src, st2[:, t, ss_col:ss_col + 8].unsqueeze(2).to_broadcast([128, 8, 96]), ALU.mult),
                reads=src_keys + [("rs", t, w)], writes=[NK])
            yield
            S.op("pool", lambda e: e.tensor_tensor(nb_, nb_, gbc, ALU.mult),
                 reads=[NK, gkey], writes=[NK])
            yield
            cosb = ropet[:, t, 0:16].unsqueeze(1).to_broadcast([128, 8, 16])
            sinb = ropet[:, t, 16:32].unsqueeze(1).to_broadcast([128, 8, 16])
            r1 = nb_[:, :, 64:80]
            r2 = nb_[:, :, 80:96]
            S.op("act", lambda e: e.copy(qb_[:, :, 0:64], nb_[:, :, 0:64]),
                 reads=[NK], writes=[("qkb", tb, w, 0)])
            S.op("dve", lambda e: e.tensor_tensor(rt_[:, 0, :, :], r1, cosb, ALU.mult),
                 reads=[NK, "ropet"], writes=[("rt", tb, w, 0)])
            S.op("pool", lambda e: e.tensor_tensor(rt_[:, 1, :, :], r2, sinb, ALU.mult),
                 reads=[NK, "ropet"], writes=[("rt", tb, w, 1)])
            yield
            S.op("dve", lambda e: e.tensor_tensor(rt_[:, 2, :, :], r2, cosb, ALU.mult),
                 reads=[NK, "ropet"], writes=[("rt", tb, w, 2)])
            S.op("pool", lambda e: e.tensor_tensor(rt_[:, 3, :, :], r1, sinb, ALU.mult),
                 reads=[NK, "ropet"], writes=[("rt", tb, w, 3)])
            yield
            S.op("dve", lambda e: e.tensor_tensor(qb_[:, :, 64:80], rt_[:, 0, :, :], rt_[:, 1, :, :], ALU.subtract),
                 reads=[("rt", tb, w, 0), ("rt", tb, w, 1)], writes=[("qkb", tb, w, 1)])
            S.op("pool", lambda e: e.tensor_tensor(qb_[:, :, 80:96], rt_[:, 2, :, :], rt_[:, 3, :, :], ALU.add),
                 reads=[("rt", tb, w, 2), ("rt", tb, w, 3)], writes=[("qkb", tb, w, 2)])
            yield
            pb = 6 + w
            for h in range(8):
                S.op("pe", lambda e, h=h: e.transpose(psb[pb][0:96, h * 128:(h + 1) * 128], qb_[:, h, :], ident),
                     reads=[("qkb", tb, w, 0), ("qkb", tb, w, 1), ("qkb", tb, w, 2), "ident"], writes=[PS(pb)])
            S.op("act", lambda e: e.copy(dstT[0:96, :, t * 128:(t + 1) * 128],
                                         psb[pb][0:96, :].rearrange("p (a b) -> p a b", a=8)),
                 reads=[PS(pb)], writes=[("qkT", w, t)])
            yield

        def prep_tile(t):
            tb = t % NB2
            sq_ = sqa[tb]
            SQ = ("sqa", tb)
            for kt in range(KT):
                S.op("pe", lambda e, kt=kt: e.matmul(
                    ps[0][:, 0:416], hT[:, kt, t * 128:(t + 1) * 128], win_a[:, kt, :],
                    start=(kt == 0), stop=(kt == KT - 1)),
                    reads=[("hT", t), "win_a"], writes=[PS(0)])
            S.op("act", lambda e: e.activation(sq_[:, 0:256], ps[0][:, 0:256], AF.Square, accum_out=st2[:, t, 0:1]),
                 reads=[PS(0)], writes=[SQ, ("s0", t)])
            S.op("act", lambda e: e.activation(sq_[:, 256:384], ps[0][:, 256:384], AF.Square, accum_out=st2[:, t, 1:2]),
                 reads=[PS(0)], writes=[SQ, ("s1", t)])
            S.op("act", lambda e: e.activation(st2[:, t, 4:5], st2[:, t, 0:1], AF.Sqrt, bias=EPS, scale=1.0 / 256),
                 reads=[("s0", t)], writes=[("s0b", t)])
            S.op("act", lambda e: e.activation(st2[:, t, 5:6], st2[:, t, 1:2], AF.Sqrt, bias=EPS, scale=1.0 / 128),
                 reads=[("s1", t)], writes=[("s1b", t)])
            S.op("dve", lambda e: e.reciprocal(st2[:, t, 6:8], st2[:, t, 4:6]),
                 reads=[("s0b", t), ("s1b", t)], writes=[("s01c", t)])
            S.op("dve", lambda e: e.scalar_tensor_tensor(
                cqb[tb][:, 0:256], ps[0][:, 0:256], st2[:, t, 6:7], gqa, ALU.mult, ALU.mult),
                reads=[PS(0), ("s01c", t), "gqa"], writes=[("cqb0", tb)])
            S.op("dve", lambda e: e.scalar_tensor_tensor(
                cqb[tb][:, 256:384], ps[0][:, 256:384], st2[:, t, 7:8], gkva, ALU.mult, ALU.mult),
                reads=[PS(0), ("s01c", t), "gkva"], writes=[("cqb1", tb)])
            S.op("act", lambda e: e.copy(kr[tb], ps[0][:, 384:416]), reads=[PS(0)], writes=[("kr", tb)])
            for j in range(3):
                S.op("pe", lambda e, j=j: e.transpose(psb[1][:, j * 128:(j + 1) * 128], cqb[tb][:, j * 128:(j + 1) * 128], ident),
                     reads=[("cqb0", tb), ("cqb1", tb), "ident"], writes=[PS(1)])
            S.op("act", lambda e: e.copy(cT[tb], psb[1][:, 0:384].rearrange("p (a b) -> p a b", a=3)),
                 reads=[PS(1)], writes=[("cT", tb)])
            for j in range(2):
                S.op("pe", lambda e, j=j: e.matmul(ps[2][:, :], cT[tb][:, j, :], wuq[:, j, 0:512], start=(j == 0), stop=(j == 1)),
                     reads=[("cT", tb), "wuq"], writes=[PS(2)])
            for j in range(2):
                S.op("pe", lambda e, j=j: e.matmul(ps[3][:, 0:256], cT[tb][:, j, :], wuq[:, j, 512:768], start=(j == 0), stop=(j == 1)),
                     reads=[("cT", tb), "wuq"], writes=[PS(3)])
            for hf in range(2):
                S.op("pe", lambda e, hf=hf: e.matmul(ps[4 + hf][:, :], cT[tb][:, 2, :], wukv[:, hf * 512:(hf + 1) * 512], start=True, stop=True),
                     reads=[("cT", tb), "wukv"], writes=[PS(4 + hf)])
            S.op("act", lambda e: e.copy(sq_[:, 0:512], ps[2][:, :]), reads=[PS(2)], writes=[SQ])
            S.op("act", lambda e: e.copy(sq_[:, 512:768], ps[3][:, 0:256]), reads=[PS(3)], writes=[SQ])
            qraw = sq_[:, 0:768].rearrange("p (a b) -> p a b", a=8)
            S.op("dve", lambda e: e.tensor_tensor(nbuf[tb][0], qraw, qraw, ALU.mult), reads=[SQ], writes=[("nbuf", tb, 0)])
            S.op("dve", lambda e: e.tensor_reduce(st2[:, t, 8:16], nbuf[tb][0], AX.X, ALU.add),
                 reads=[("nbuf", tb, 0)], writes=[("ss", t, 0)])
            for hf in range(2):
                S.op("act", lambda e, hf=hf: e.copy(
                    kraw[tb][:, hf * 4:(hf + 1) * 4, 0:64],
                    ps[4 + hf][:, :].rearrange("p (a b) -> p a b", a=4)[:, :, 0:64]),
                    reads=[PS(4 + hf)], writes=[("kraw", tb, hf)])
                S.op("dve", lambda e, hf=hf: e.tensor_copy(
                    vb[:, t, hf * 4:(hf + 1) * 4, 0:64],
                    ps[4 + hf][:, :].rearrange("p (a b) -> p a b", a=4)[:, :, 64:128]),
                    reads=[PS(4 + hf), "vb_init"], writes=[("vb", t, hf)])
            S.op("pool", lambda e: e.tensor_copy(kraw[tb][:, :, 64:96], kr[tb].unsqueeze(1).to_broadcast([128, 8, 32])),
                 reads=[("kr", tb)], writes=[("kraw", tb, 2)])
            KR = [("kraw", tb, 0), ("kraw", tb, 1), ("kraw", tb, 2)]
            S.op("dve", lambda e: e.tensor_tensor(nbuf[tb][1], kraw[tb], kraw[tb], ALU.mult),
                 reads=KR, writes=[("nbuf", tb, 1)])
            S.op("dve", lambda e: e.tensor_reduce(st2[:, t, 24:32], nbuf[tb][1], AX.X, ALU.add),
                 reads=[("nbuf", tb, 1)], writes=[("ss", t, 1)])
            gq = norm_rope(t, 0, qraw, [SQ], 8, qT)
            gk = norm_rope(t, 1, kraw[tb], KR, 24, kT)
            alive = [gq, gk]
            while alive:
                for g in list(alive):
                    try:
                        next(g)
                    except StopIteration:
                        alive.remove(g)

        for t in range(NT):
            prep_tile(t)
        S.barrier()
        A.release(m3)

        m4 = A.mark()
        A_tok = A.alloc([NT, 512], BF16)
        pT = [A.alloc([512], BF16) for _ in range(4)]
        rden = A.alloc([2, 4], F32)
        AT = A.alloc([4, S_TOK], BF16)
        QK_ALL = lambda w: [("qkT", w, t) for t in range(NT)]
        its = [(h, qc, j) for h in range(8) for qc in range(4) for j in range(NT)]
        LOOK = 2

        def emit_qk(i):
            h, qc, j = its[i]
            sbk = i % 4
            S.op("pe", lambda e: e.matmul(
                ps[sbk][:, :], kT[0:96, h, j * 128:(j + 1) * 128], qT[0:96, h, qc * 512:(qc + 1) * 512],
                start=True, stop=True),
                reads=[("qkT", 1, j)] + [("qkT", 0, qc * 4 + s_) for s_ in range(4)], writes=[PS(sbk)])
            S.op("act", lambda e: e.activation(pT[sbk], ps[sbk][:, :], AF.Exp),
                 reads=[PS(sbk)], writes=[("pT", sbk)])

        for i in range(min(LOOK, len(its))):
            emit_qk(i)
        for i, (h, qc, j) in enumerate(its):
            if i + LOOK < len(its):
                emit_qk(i + LOOK)
            sbk = i % 4
            grp = i // NT
            ob = 4 + (grp % 2)
            for sub in range(4):
                S.op("pe", lambda e, h=h, j=j, sbk=sbk, sub=sub, ob=ob: e.matmul(
                    ps[ob][:, sub * 128:sub * 128 + 65], pT[sbk][:, sub * 128:(sub + 1) * 128], vb[:, j, h, :],
                    start=(j == 0), stop=(j == NT - 1)),
                    reads=[("pT", sbk), ("vb", j, h // 4), "vb_init"], writes=[PS(ob)])
            if j == NT - 1:
                o4 = ps[ob][:, :].rearrange("p (a b) -> p a b", a=4)
                rb = grp % 2
                S.op("dve", lambda e, o4=o4, rb=rb: e.reciprocal(rden[:, rb, :].unsqueeze(2), o4[:, :, 64:65]),
                     reads=[PS(ob)], writes=[("rden", rb)])
                S.op("dve", lambda e, o4=o4, rb=rb, h=h, qc=qc: e.tensor_tensor(
                    A_tok[:, qc * 4:(qc + 1) * 4, h * 64:(h + 1) * 64], o4[:, :, 0:64],
                    rden[:, rb, :].unsqueeze(2).to_broadcast([128, 4, 64]), ALU.mult),
                    reads=[PS(ob), ("rden", rb)], writes=[("A_tok", qc, h)])
        for t in range(NT):
            pb = 6 + (t % 2)
            for j in range(4):
                S.op("pe", lambda e, t=t, j=j, pb=pb: e.transpose(
                    psb[pb][:, j * 128:(j + 1) * 128], A_tok[:, t, j * 128:(j + 1) * 128], ident),
                    reads=[("A_tok", t // 4, h) for h in range(8)] + ["ident"], writes=[PS(pb)])
            S.op("act", lambda e, t=t, pb=pb: e.copy(
                AT[:, :, t * 128:(t + 1) * 128], psb[pb][:, 0:512].rearrange("p (a b) -> p a b", a=4)),
                reads=[PS(pb)], writes=[("AT", t)])
        S.dma("sp", lambda e: e.dma_start(out=AT_scr.rearrange("p (a b) -> p a b", a=4), in_=AT),
              reads=[("AT", t) for t in range(NT)], writes=["AT_scr"], slot="AT_st")
        S.barrier()
        A.release(m0)

        m5 = A.mark()
        win_g = A.alloc([KT, 2048], BF16)
        wpa = A.alloc([4, D], BF16)
        wpf = A.alloc([4, D], BF16)
        wout = A.alloc([KT, D], BF16)
        hTc = [A.alloc([KT, 512], BF16) for _ in range(2)]
        ATc = [A.alloc([4, 512], BF16) for _ in range(2)]
        FTc = [A.alloc([4, 512], BF16) for _ in range(2)]
        mTc = [A.alloc([KT, 512], BF16) for _ in range(2)]
        sa = [A.alloc([512], F32) for _ in range(2)]
        sf = [A.alloc([512], F32) for _ in range(2)]
        mm1 = [A.alloc([512], F32) for _ in range(2)]
        mm2 = [A.alloc([512], F32) for _ in range(2)]
        xb5 = [A.alloc([D], F32) for _ in range(4)]
        t5 = [A.alloc([D], F32) for _ in range(2)]
        x1b = [A.alloc([D], F32) for _ in range(2)]
        for half in range(2):
            S.dma("pool", lambda e, half=half: e.dma_start(
                out=win_g[:, :, half * 1024:(half + 1) * 1024],
                in_=w_in[:, 928 + half * 1024:928 + (half + 1) * 1024].rearrange("(kt p) n -> p kt n", p=128)),
                writes=[("win_g", half)], slot="win_g%d" % half)
        S.dma("pool", lambda e: e.dma_start(out=wpa, in_=w_pa.rearrange("(kt p) n -> p kt n", p=128)), writes=["wpa"], slot="wpa")
        S.dma("pool", lambda e: e.dma_start(out=wpf, in_=w_pf.rearrange("(kt p) n -> p kt n", p=128)), writes=["wpf"], slot="wpf")
        S.dma("pool", lambda e: e.dma_start(out=wout, in_=w_out.rearrange("(kt p) n -> p kt n", p=128)), writes=["wout"], slot="wout")
        nfc = 0

        def load_chunk(qc):
            cb = qc % 2
            S.dma("sp", lambda e: e.dma_start(
                out=hTc[cb], in_=hT_scr.rearrange("p (a b) -> p a b", a=KT)[:, :, qc * 512:(qc + 1) * 512]),
                reads=["hT_scr"], writes=[("hTc", cb)], slot="hTc%d" % cb)
            S.dma("sp", lambda e: e.dma_start(
                out=ATc[cb], in_=AT_scr.rearrange("p (a b) -> p a b", a=4)[:, :, qc * 512:(qc + 1) * 512]),
                reads=["AT_scr"], writes=[("ATc", cb)], slot="ATc%d" % cb)
            S.dma("sp", lambda e: e.dma_start(
                out=FTc[cb], in_=FT_scr.rearrange("p (a b) -> p a b", a=4)[:, :, qc * 512:(qc + 1) * 512]),
                reads=["FT_scr"], writes=[("FTc", cb)], slot="FTc%d" % cb)

        load_chunk(0)
        for qc in range(4):
            cb = qc % 2
            if qc + 1 < 4:
                load_chunk(qc + 1)
            for sub in range(4):
                t_ = qc * 4 + sub
                S.dma("sp", lambda e, t_=t_, sub=sub: e.dma_start(out=xb5[sub], in_=x[t_ * 128:(t_ + 1) * 128, :]),
                      writes=[("xb5", sub)], slot="xb5%d" % sub)
            for fc in range(8):
                pbase = 4 * (nfc % 2)
                eb = nfc % 2
                nfc += 1
                for kt in range(KT):
                    S.op("pe", lambda e, cb=cb, fc=fc, kt=kt, pbase=pbase: e.matmul(
                        ps[pbase][:, :], win_g[:, kt, fc * 128:(fc + 1) * 128], hTc[cb][:, kt, :],
                        start=(kt == 0), stop=(kt == KT - 1)),
                        reads=[("win_g", 0), ("hTc", cb)], writes=[PS(pbase)])
                for kt in range(KT):
                    S.op("pe", lambda e, cb=cb, fc=fc, kt=kt, pbase=pbase: e.matmul(
                        ps[pbase + 1][:, :], win_g[:, kt, 1024 + fc * 128:1024 + (fc + 1) * 128], hTc[cb][:, kt, :],
                        start=(kt == 0), stop=(kt == KT - 1)),
                        reads=[("win_g", 1), ("hTc", cb)], writes=[PS(pbase + 1)])
                for j in range(4):
                    S.op("pe", lambda e, cb=cb, fc=fc, j=j, pbase=pbase: e.matmul(
                        ps[pbase + 2][:, :], wpa[:, j, fc * 128:(fc + 1) * 128], ATc[cb][:, j, :],
                        start=(j == 0), stop=(j == 3)),
                        reads=["wpa", ("ATc", cb)], writes=[PS(pbase + 2)])
                for j in range(4):
                    S.op("pe", lambda e, cb=cb, fc=fc, j=j, pbase=pbase: e.matmul(
                        ps[pbase + 3][:, :], wpf[:, j, fc * 128:(fc + 1) * 128], FTc[cb][:, j, :],
                        start=(j == 0), stop=(j == 3)),
                        reads=["wpf", ("FTc", cb)], writes=[PS(pbase + 3)])
                S.op("act", lambda e, eb=eb, pbase=pbase: e.activation(sa[eb], ps[pbase][:, :], AF.Sigmoid),
                     reads=[PS(pbase)], writes=[("sa", eb)])
                S.op("act", lambda e, eb=eb, pbase=pbase: e.activation(sf[eb], ps[pbase + 1][:, :], AF.Sigmoid),
                     reads=[PS(pbase + 1)], writes=[("sf", eb)])
                S.op("dve", lambda e, eb=eb, pbase=pbase: e.tensor_tensor(mm1[eb], ps[pbase + 2][:, :], sa[eb], ALU.mult),
                     reads=[PS(pbase + 2), ("sa", eb)], writes=[("mm1", eb)])
                S.op("dve", lambda e, eb=eb, pbase=pbase: e.tensor_tensor(mm2[eb], ps[pbase + 3][:, :], sf[eb], ALU.mult),
                     reads=[PS(pbase + 3), ("sf", eb)], writes=[("mm2", eb)])
                S.op("pool", lambda e, eb=eb, cb=cb, fc=fc: e.tensor_tensor(mTc[cb][:, fc, :], mm1[eb], mm2[eb], ALU.add),
                     reads=[("mm1", eb), ("mm2", eb)], writes=[("mTc", cb, fc)])
            for sub in range(4):
                t = qc * 4 + sub
                xbi = t % 2
                p0 = 2 * sub
                for hf in range(2):
                    for fc in range(8):
                        S.op("pe", lambda e, cb=cb, sub=sub, hf=hf, fc=fc, p0=p0: e.matmul(
                            ps[p0 + hf][:, :], mTc[cb][:, fc, sub * 128:(sub + 1) * 128], wout[:, fc, hf * 512:(hf + 1) * 512],
                            start=(fc == 0), stop=(fc == 7)),
                            reads=[("mTc", cb, fc), "wout"], writes=[PS(p0 + hf)])
                    S.op("dve", lambda e, xbi=xbi, hf=hf, p0=p0: e.tensor_tensor(
                        t5[xbi][:, hf * 512:(hf + 1) * 512], ps[p0 + hf][:, :], ada[:, 2, hf * 512:(hf + 1) * 512], ALU.mult),
                        reads=[PS(p0 + hf)] + ADA(2), writes=[("t5", xbi, hf)])
                S.op("pool", lambda e, xbi=xbi, sub=sub: e.tensor_tensor(x1b[xbi], t5[xbi], xb5[sub], ALU.add),
                     reads=[("t5", xbi, 0), ("t5", xbi, 1), ("xb5", sub)], writes=[("x1b", xbi)])
                dst = out if stage == "x1" else x1_scr
                S.dma("sp", lambda e, t=t, xbi=xbi, dst=dst: e.dma_start(out=dst[t * 128:(t + 1) * 128, :], in_=x1b[xbi]),
                      reads=[("x1b", xbi)], writes=[("x1_scr", t)], slot="x1st%d" % xbi)
        S.barrier()
        A.release(m5)
        if stage == "x1":
            st = S.emit()
            print("sched", st, "arena peak KiB", A.peak / 512.0)
            return nc

        gwk = A.alloc([NT, 8], F32)
        idx = A.alloc([NT, 8], I32)
        widx = A.alloc([R_OVF], I32)
        m6 = A.mark()
        Gw = A.alloc([NT, NE], F32)
        Pos = A.alloc([NT, NE], F32)
        run_bc = A.alloc([NE], F32)
        iota1 = A.alloc([NE], F32)
        m7 = A.mark()
        wr_f = A.alloc([KT, NE], F32)
        wr_hi = A.alloc([KT, NE], BF16)
        wr_lo = A.alloc([KT, NE], BF16)
        wsh = A.alloc([KT, 512], BF16)
        wshd = A.alloc([2, D], BF16)
        rbias = A.alloc([NE], F32)
        tmp2 = [A.alloc([D], F32) for _ in range(2)]
        h2hi = [A.alloc([D], BF16) for _ in range(2)]
        h2lo = [A.alloc([D], BF16) for _ in range(2)]
        h2T = [A.alloc([KT, 128], BF16) for _ in range(2)]
        h2loT = [A.alloc([KT, 128], BF16) for _ in range(2)]
        sq5_ = [A.alloc([D], F32)]
        st5 = A.alloc([NT, 8], F32)
        scb_ = [A.alloc([NE], F32) for _ in range(2)]
        biased_ = [A.alloc([NE], F32) for _ in range(2)]
        wsel_ = [A.alloc([NE], F32) for _ in range(2)]
        maskb_ = [A.alloc([NE], BF16) for _ in range(2)]
        top8 = A.alloc([NT, 8], F32)
        sg_ = [A.alloc([256], F32) for _ in range(2)]
        hsh_ = [A.alloc([256], BF16) for _ in range(2)]
        hshT_ = [A.alloc([2, 128], BF16) for _ in range(2)]
        t6 = [A.alloc([D], F32) for _ in range(2)]
        S.dma("sp", lambda e: e.dma_start(out=wr_f, in_=w_router.rearrange("(kt p) n -> p kt n", p=128)), writes=["wr_f"], slot="wr_f")
        S.op("act", lambda e: e.copy(wr_hi, wr_f), reads=["wr_f"], writes=["wr_hi"])
        S.op("dve", lambda e: e.tensor_tensor(wr_lo, wr_f, wr_hi, ALU.subtract), reads=["wr_f", "wr_hi"], writes=["wr_lo"])
        S.dma("pool", lambda e: e.dma_start(out=wsh[:, :, 0:256], in_=w_sg.rearrange("(kt p) n -> p kt n", p=128)), writes=["wsh0"], slot="wsh0")
        S.dma("pool", lambda e: e.dma_start(out=wsh[:, :, 256:512], in_=w_su.rearrange("(kt p) n -> p kt n", p=128)), writes=["wsh1"], slot="wsh1")
        S.dma("pool", lambda e: e.dma_start(out=wshd, in_=w_sd.rearrange("(kt p) n -> p kt n", p=128)), writes=["wshd"], slot="wshd")
        bc_load(rbias, router_bias[0, :], "rbias", "rbias")
        S.dma("sp", lambda e: e.dma_start(out=iota1, in_=iota1_d), writes=["iota1"], slot="iota1")
        S.op("dve", lambda e: e.memset(run_bc, 0.0), writes=["run_bc"])
        x1all = A.alloc([NT, D], F32)
        sgs_ = [A.alloc([256], F32) for _ in range(2)]
        for t in range(NT):
            S.dma("sp", lambda e, t=t: e.dma_start(out=x1all[:, t, :], in_=x1_scr[t * 128:(t + 1) * 128, :]),
                  writes=[("x1t", t)], slot="x1t%d" % (t % 4))
        for t in range(NT):
            S.op("act", lambda e, t=t: e.activation(sq5_[0], x1all[:, t, :], AF.Square, accum_out=st5[:, t, 0:1]),
                 reads=[("x1t", t)], writes=["sq5", ("st5", t)])
        ST5 = [("st5", t) for t in range(NT)]
        S.op("act", lambda e: e.activation(st5[:, :, 1], st5[:, :, 0], AF.Sqrt, bias=EPS, scale=1.0 / D),
             reads=ST5, writes=["st5b"])
        S.op("dve", lambda e: e.reciprocal(st5[:, :, 2], st5[:, :, 1]), reads=["st5b"], writes=["st5c"])

        def stage5A(t):
            b = t % 2
            S.op("dve", lambda e: e.scalar_tensor_tensor(
                tmp2[b], x1all[:, t, :], st5[:, t, 2:3], ada[:, 4, :], ALU.mult, ALU.mult),
                reads=[("x1t", t), "st5c"] + ADA(4), writes=[("tmp2", b)])
            S.op("pool", lambda e: e.tensor_tensor(tmp2[b], tmp2[b], ada[:, 3, :], ALU.add),
                 reads=[("tmp2", b)] + ADA(3), writes=[("tmp2", b)])
            S.op("act", lambda e: e.copy(h2hi[b], tmp2[b]), reads=[("tmp2", b)], writes=[("h2hi", b)])
            S.op("dve", lambda e: e.tensor_tensor(h2lo[b], tmp2[b], h2hi[b], ALU.subtract),
                 reads=[("tmp2", b), ("h2hi", b)], writes=[("h2lo", b)])
            S.dma("sp", lambda e: e.dma_start(out=h2_scr[t * 128:(t + 1) * 128, :], in_=h2hi[b]),
                  reads=[("h2hi", b)], writes=[("h2_scr", t)], slot="h2st%d" % b)
            for kt in range(KT):
                S.op("pe", lambda e, kt=kt: e.transpose(psb[0][:, kt * 128:(kt + 1) * 128], h2hi[b][:, kt * 128:(kt + 1) * 128], ident),
                     reads=[("h2hi", b), "ident"], writes=[PS(0)])
            S.op("act", lambda e: e.copy(h2T[b], psb[0][:, :].rearrange("p (a b) -> p a b", a=8)),
                 reads=[PS(0)], writes=[("h2T", b)])
            for kt in range(KT):
                S.op("pe", lambda e, kt=kt: e.transpose(psb[1][:, kt * 128:(kt + 1) * 128], h2lo[b][:, kt * 128:(kt + 1) * 128], ident),
                     reads=[("h2lo", b), "ident"], writes=[PS(1)])
            S.op("dve", lambda e: e.tensor_copy(h2loT[b], psb[1][:, :].rearrange("p (a b) -> p a b", a=8)),
                 reads=[PS(1)], writes=[("h2loT", b)])

        def stage5B(t):
            b = t % 2
            scb = scb_[b]; biased = biased_[b]; wsel = wsel_[b]; maskb = maskb_[b]
            sg = sg_[b]; sgs = sgs_[b]; hsh = hsh_[b]; hshT = hshT_[b]
            nmm = 0
            for (xi, wi) in [(0, 0), (0, 1), (1, 0)]:
                for kt in range(KT):
                    lt = h2T[b] if xi == 0 else h2loT[b]
                    wt = wr_hi if wi == 0 else wr_lo
                    S.op("pe", lambda e, lt=lt, wt=wt, kt=kt, nmm=nmm: e.matmul(
                        ps[2][:, 0:NE], lt[:, kt, :], wt[:, kt, :], start=(nmm == 0), stop=(nmm == 23)),
                        reads=[("h2T", b), ("h2loT", b), "wr_hi", "wr_lo"], writes=[PS(2)])
                    nmm += 1
            for kt in range(KT):
                S.op("pe", lambda e, kt=kt: e.matmul(
                    ps[3][:, :], h2T[b][:, kt, :], wsh[:, kt, :], start=(kt == 0), stop=(kt == KT - 1)),
                    reads=[("h2T", b), "wsh0", "wsh1"], writes=[PS(3)])
            S.op("act", lambda e: e.activation(scb, ps[2][:, 0:NE], AF.Sigmoid), reads=[PS(2)], writes=[("scb", b)])
            S.op("act", lambda e: e.activation(sgs, ps[3][:, 0:256], AF.Sigmoid), reads=[PS(3)], writes=[("sgs", b)])
            S.op("dve", lambda e: e.tensor_tensor(biased, scb, rbias, ALU.add), reads=[("scb", b), "rbias"], writes=[("biased", b)])
            S.op("dve", lambda e: e.max(top8[:, t, :], biased), reads=[("biased", b)], writes=[("top8", t)])
            S.op("dve", lambda e: e.scalar_tensor_tensor(
                wsel, biased, top8[:, t, 7:8], scb, ALU.is_ge, ALU.mult, accum_out=st5[:, t, 3:4]),
                reads=[("biased", b), ("top8", t), ("scb", b)], writes=[("wsel", b), ("den", t)])
            S.op("dve", lambda e: e.tensor_scalar(maskb, biased, top8[:, t, 7:8], None, ALU.is_ge),
                 reads=[("biased", b), ("top8", t)], writes=[("maskb", b)])
            S.op("dve", lambda e: e.reciprocal(st5[:, t, 4:5], st5[:, t, 3:4]), reads=[("den", t)], writes=[("rden5", t)])
            S.op("dve", lambda e: e.tensor_scalar(Gw[:, t, :], wsel, st5[:, t, 4:5], 2.5, ALU.mult, ALU.mult),
                 reads=[("wsel", b), ("rden5", t)], writes=[("Gw", t)])
            S.op("pe", lambda e: e.matmul(ps[4][:, 0:NE], tri, maskb, start=True, stop=True),
                 reads=["tri", ("maskb", b)], writes=[PS(4)])
            S.op("pe", lambda e: e.matmul(ps[4][:, NE:2 * NE], ones, maskb, start=True, stop=True),
                 reads=["ones", ("maskb", b)], writes=[PS(4)])
            S.op("dve", lambda e: e.tensor_tensor(Pos[:, t, :], ps[4][:, 0:NE], run_bc, ALU.add),
                 reads=[PS(4), "run_bc"], writes=[("Pos", t)])
            S.op("dve", lambda e: e.tensor_tensor(run_bc, ps[4][:, NE:2 * NE], run_bc, ALU.add),
                 reads=[PS(4), "run_bc"], writes=["run_bc"])
            S.op("dve", lambda e: e.tensor_tensor(sg, ps[3][:, 0:256], sgs, ALU.mult), reads=[PS(3), ("sgs", b)], writes=[("sg", b)])
            S.op("dve", lambda e: e.tensor_tensor(hsh, ps[3][:, 256:512], sg, ALU.mult), reads=[PS(3), ("sg", b)], writes=[("hsh", b)])
            for j in range(2):
                S.op("pe", lambda e, j=j: e.transpose(psb[5][:, j * 128:(j + 1) * 128], hsh[:, j * 128:(j + 1) * 128], ident),
                     reads=[("hsh", b), "ident"], writes=[PS(5)])
            S.op("act", lambda e: e.copy(hshT, psb[5][:, 0:256].rearrange("p (a b) -> p a b", a=2)), reads=[PS(5)], writes=[("hshT", b)])
            for hf in range(2):
                for j in range(2):
                    S.op("pe", lambda e, hf=hf, j=j: e.matmul(
                        ps[6 + hf][:, :], hshT[:, j, :], wshd[:, j, hf * 512:(hf + 1) * 512], start=(j == 0), stop=(j == 1)),
                        reads=[("hshT", b), "wshd"], writes=[PS(6 + hf)])
                S.op("dve", lambda e, hf=hf: e.tensor_tensor(
                    t6[b][:, hf * 512:(hf + 1) * 512], ps[6 + hf][:, :], ada[:, 5, hf * 512:(hf + 1) * 512], ALU.mult),
                    reads=[PS(6 + hf)] + ADA(5), writes=[("t6", b, hf)])
            S.op("pool", lambda e: e.tensor_tensor(t6[b], t6[b], x1all[:, t, :], ALU.add),
                 reads=[("t6", b, 0), ("t6", b, 1), ("x1t", t)], writes=[("t6", b, 0), ("t6", b, 1)])
            S.dma("sp", lambda e: e.dma_start(out=base_scr[t * 128:(t + 1) * 128, :], in_=t6[b]),
                  reads=[("t6", b, 0), ("t6", b, 1)], writes=[("base_scr", t)], slot="bst%d" % b)

        stage5A(0)
        for t in range(NT):
            if t + 1 < NT:
                stage5A(t + 1)
            stage5B(t)
        S.barrier()
        A.release(m7)

        a1_ = [A.alloc([NE], F32) for _ in range(2)]
        a2_ = [A.alloc([NE], F32) for _ in range(2)]
        maddr_ = [A.alloc([NE], F32) for _ in range(2)]
        junk6 = [A.alloc([NE], F32) for _ in range(2)]
        top8a = A.alloc([NT, 8], F32)
        h2r = [A.alloc([D], BF16) for _ in range(2)]
        nex = A.alloc([NE], F32)
        pfx = [A.alloc([NE], F32) for _ in range(2)]
        base2 = A.alloc([NE], F32)
        d12 = A.alloc([NE], F32)
        bexp = A.alloc([R_OVF], F32)
        iokp = A.alloc([8], F32)
        wif = A.alloc([R_OVF], F32)
        onesf = A.alloc([NE], F32)
        S.op("pool", lambda e: e.memset(onesf, 1.0), writes=["onesf"])
        S.dma("sp", lambda e: e.dma_start(out=iokp, in_=iokp_d), writes=["iokp"], slot="iokp")
        S.op("dve", lambda e: e.tensor_scalar(nex, run_bc, float(C1), None, ALU.is_gt), reads=["run_bc"], writes=["nex"])
        for j in range(1, -(-(S_TOK - C1) // 128)):
            S.op("dve", lambda e, j=j: e.scalar_tensor_tensor(nex, run_bc, float(C1 + 128 * j), nex, ALU.is_gt, ALU.add),
                 reads=["run_bc", "nex"], writes=["nex"])
        S.op("dve", lambda e: e.tensor_copy(pfx[0], nex), reads=["nex"], writes=[("pfx", 0)])
        cur = 0
        sh = 1
        while sh < NE:
            nxt = 1 - cur
            S.op("dve", lambda e, cur=cur, nxt=nxt, sh=sh: e.tensor_tensor(pfx[nxt][:, sh:NE], pfx[cur][:, sh:NE], pfx[cur][:, 0:NE - sh], ALU.add),
                 reads=[("pfx", cur)], writes=[("pfx", nxt)])
            S.op("dve", lambda e, cur=cur, nxt=nxt, sh=sh: e.tensor_copy(pfx[nxt][:, 0:sh], pfx[cur][:, 0:sh]),
                 reads=[("pfx", cur)], writes=[("pfx", nxt)])
            cur = nxt
            sh *= 2
        obend = pfx[cur]
        OBK = ("pfx", cur)
        S.op("dve", lambda e: e.tensor_tensor(base2, obend, nex, ALU.subtract), reads=[OBK, "nex"], writes=["base2"])
        S.op("dve", lambda e: e.tensor_scalar(base2, base2, 128.0, float(NROWS1 - C1), ALU.mult, ALU.add), reads=["base2"], writes=["base2"])
        S.op("dve", lambda e: e.tensor_tensor(d12, iota1, base2, ALU.subtract), reads=["iota1", "base2"], writes=["d12"])
        for bq in range(R_OVF):
            S.op("dve", lambda e, bq=bq: e.scalar_tensor_tensor(
                junk6[bq % 2], obend, float(bq), onesf, ALU.is_le, ALU.mult, accum_out=bexp[:, bq:bq + 1]),
                reads=[OBK, "onesf"], writes=[("junk6", bq % 2), ("bexp", bq)])
        BEXP = [("bexp", bq) for bq in range(R_OVF)]
        S.op("dve", lambda e: e.tensor_scalar_min(bexp, bexp, float(NE - 1)), reads=BEXP, writes=["bexpc"])
        S.op("dve", lambda e: e.scalar_tensor_tensor(
            wif, bexp, 128.0, iokp[:, 0:1].to_broadcast([128, R_OVF]), ALU.mult, ALU.add),
            reads=["bexpc", "iokp"], writes=["wif"])
        S.op("dve", lambda e: e.tensor_copy(widx, wif), reads=["wif"], writes=["widx"])
        for t in range(NT):
            b = t % 2
            a1 = a1_[b]; a2 = a2_[b]; maddr = maddr_[b]
            S.op("dve", lambda e, t=t, a1=a1: e.scalar_tensor_tensor(a1, Pos[:, t, :], float(C1), d12, ALU.is_lt, ALU.mult),
                 reads=[("Pos", t), "d12"], writes=[("a1", b)])
            S.op("dve", lambda e, t=t, a2=a2: e.tensor_tensor(a2, Pos[:, t, :], base2, ALU.add),
                 reads=[("Pos", t), "base2"], writes=[("a2", b)])
            S.op("dve", lambda e, a1=a1, a2=a2: e.tensor_tensor(a1, a1, a2, ALU.add), reads=[("a1", b), ("a2", b)], writes=[("a1", b)])
            S.op("dve", lambda e, t=t, a1=a1: e.scalar_tensor_tensor(Gw[:, t, :], a1, float(NROWS), Gw[:, t, :], ALU.is_lt, ALU.mult),
                 reads=[("Gw", t), ("a1", b)], writes=[("Gw", t)])
            S.op("dve", lambda e, t=t, a1=a1, maddr=maddr: e.scalar_tensor_tensor(maddr, Gw[:, t, :], 0.0, a1, ALU.is_gt, ALU.mult),
                 reads=[("Gw", t), ("a1", b)], writes=[("maddr", b)])
            S.op("dve", lambda e, t=t, maddr=maddr: e.max(top8a[:, t, :], maddr), reads=[("maddr", b)], writes=[("top8a", t)])
            S.op("dve", lambda e, t=t: e.tensor_copy(idx[:, t, :], top8a[:, t, :]), reads=[("top8a", t)], writes=[("idx", t)])
            for k in range(8):
                jb = k % 2
                S.op("dve", lambda e, t=t, k=k, jb=jb, maddr=maddr: e.scalar_tensor_tensor(
                    junk6[jb], maddr, top8a[:, t, k:k + 1], Gw[:, t, :], ALU.is_equal, ALU.mult, accum_out=gwk[:, t, k:k + 1]),
                    reads=[("maddr", b), ("top8a", t), ("Gw", t)], writes=[("junk6", jb), ("gwk", t, k)])
            S.dma("sp", lambda e, t=t, b=b: e.dma_start(out=h2r[b], in_=h2_scr[t * 128:(t + 1) * 128, :]),
                  writes=[("h2r", b)], slot="h2r%d" % b)
            for k in range(8):
                S.dma("pool", lambda e, t=t, b=b, k=k: e.indirect_dma_start(
                    out=xdisp[:, :], out_offset=bass.IndirectOffsetOnAxis(ap=idx[:, t, k:k + 1], axis=0),
                    in_=h2r[b], in_offset=None),
                    reads=[("h2r", b), ("idx", t)], writes=[("xdisp", t, k)], slot="sc%d_%d" % (b, k))
        S.barrier()
        A.release(m6)

        m8 = A.mark()
        NXB = 4
        xs = [A.alloc([2, D], BF16) for _ in range(NXB)]
        XT = [A.alloc([KT, 256], BF16) for _ in range(2)]
        NWB = 6
        wg = [A.alloc([KT, 256], BF16) for _ in range(NWB)]
        wu = [A.alloc([KT, 256], BF16) for _ in range(NWB)]
        wd = [A.alloc([2, D], BF16) for _ in range(NWB)]
        sgb = [A.alloc([2, 256], F32) for _ in range(2)]
        HT = [A.alloc([2, 256], BF16) for _ in range(2)]
        ysb = [A.alloc([2, D], F32) for _ in range(2)]
        w32g = [A.alloc([KT, 256], F32) for _ in range(2)]
        w32u = [A.alloc([KT, 256], F32) for _ in range(2)]
        w32d = [A.alloc([2, D], F32) for _ in range(2)]
        weg_rows = w_eg.rearrange("e (p kt) f -> (e p) (kt f)", kt=KT)
        weu_rows = w_eu.rearrange("e (p kt) f -> (e p) (kt f)", kt=KT)
        wed_rows = w_ed.rearrange("e (p j) d -> (e p) (j d)", j=2)
        S.op("dve", lambda e: e.memset(ysb[0], 0.0), writes=[("ysb", 0, 0, 0), ("ysb", 0, 0, 1), ("ysb", 0, 1, 0), ("ysb", 0, 1, 1)])
        S.dma("sp", lambda e: e.dma_start(out=yscr[0:ROW0, :], in_=ysb[0][:, 0, :]),
              reads=[("ysb", 0, 0, 0), ("ysb", 0, 0, 1)], writes=["yscr_trash"], slot="yst0_0")
        for xb_ in range(NXB):
            S.op("pool", lambda e, xb_=xb_: e.memset(xs[xb_], 0.0), writes=[("xs", xb_)])
        units = [("s", e_, ROW0 + e_ * C1, 2) for e_ in range(NE)] + [("d", bq, NROWS1 + bq * 128, 1) for bq in range(R_OVF)]

        def load_x(u):
            kind, ui, r0_, nblk = units[u]
            xb_ = u % NXB
            S.dma("sp", lambda e: e.dma_start(out=xs[xb_][:, 0, :], in_=xdisp[r0_:r0_ + 128, :]),
                  writes=[("xs", xb_, 0)], reads=[("xs", xb_)], slot="xs%d_0" % xb_)
            if nblk == 2:
                S.dma("sp", lambda e: e.dma_start(out=xs[xb_][0:C1 - 128, 1, :], in_=xdisp[r0_ + 128:r0_ + C1, :]),
                      writes=[("xs", xb_, 1)], reads=[("xs", xb_)], slot="xs%d_1" % xb_)

        for u in range(NXB - 1):
            load_x(u)
        for u, (kind, ui, r0, nblk) in enumerate(units):
            b = u % 2
            xbi = u % NXB
            wb = u % NWB
            ns = nblk * 128
            if u + NXB - 1 < len(units):
                load_x(u + NXB - 1)
            if kind == "s":
                S.dma("pool", lambda e, wb=wb, ui=ui: e.dma_start(out=wg[wb], in_=w_eg[ui].rearrange("(kt p) f -> p kt f", p=128)),
                      writes=[("wg", wb)], slot="wg%d" % wb)
                S.dma("pool", lambda e, wb=wb, ui=ui: e.dma_start(out=wu[wb], in_=w_eu[ui].rearrange("(kt p) f -> p kt f", p=128)),
                      writes=[("wu", wb)], slot="wu%d" % wb)
                S.dma("pool", lambda e, wb=wb, ui=ui: e.dma_start(out=wd[wb], in_=w_ed[ui].rearrange("(j p) d -> p j d", p=128)),
                      writes=[("wd", wb)], slot="wd%d" % wb)
            else:
                db = ui % 2
                for (dst, rows_ap, nm) in ((w32g[db], weg_rows, "g"), (w32u[db], weu_rows, "u"), (w32d[db], wed_rows, "d")):
                    S.dma("pool", lambda e, dst=dst, rows_ap=rows_ap, ui=ui: e.indirect_dma_start(
                        out=dst.rearrange("p a b -> p (a b)"), out_offset=None, in_=rows_ap[:, :],
                        in_offset=bass.IndirectOffsetOnAxis(ap=widx[:, ui:ui + 1], axis=0)),
                        reads=["widx"], writes=[("w32" + nm, db)], slot="dyn_" + nm)
                S.op("act", lambda e, db=db, wb=wb: e.copy(wg[wb], w32g[db]),
                     reads=[("w32g", db)], writes=[("wg", wb)])
                S.op("dve", lambda e, db=db, wb=wb: e.tensor_copy(wu[wb], w32u[db]),
                     reads=[("w32u", db)], writes=[("wu", wb)])
                S.op("act", lambda e, db=db, wb=wb: e.copy(wd[wb], w32d[db]),
                     reads=[("w32d", db)], writes=[("wd", wb)])
            for blk in range(nblk):
                for kt in range(KT):
                    xin_ = xs[xbi][:, blk, kt:D:KT] if kind == "d" else xs[xbi][:, blk, kt * 128:(kt + 1) * 128]
                    S.op("pe", lambda e, blk=blk, kt=kt, xin_=xin_: e.transpose(
                        psb[blk][:, kt * 128:(kt + 1) * 128], xin_, ident),
                        reads=[("xs", xbi, blk), "ident"], writes=[PS(blk)])
                if blk == 0:
                    S.op("act", lambda e, b=b: e.copy(XT[b][:, :, 0:128], psb[0][:, :].rearrange("p (a b) -> p a b", a=8)),
                         reads=[PS(0)], writes=[("XT", b, 0)])
                else:
                    S.op("act", lambda e, b=b: e.copy(XT[b][:, :, 128:256], psb[1][:, :].rearrange("p (a b) -> p a b", a=8)),
                         reads=[PS(1)], writes=[("XT", b, 1)])
            XTK = [("XT", b, blk) for blk in range(nblk)]
            for fo in range(2):
                for (wt, wk, c0) in ((wg[wb], ("wg", wb), 0), (wu[wb], ("wu", wb), 256)):
                    for kt in range(KT):
                        wcol = wt[:, kt, fo:256:2] if kind == "d" else wt[:, kt, fo * 128:(fo + 1) * 128]
                        S.op("pe", lambda e, b=b, fo=fo, wcol=wcol, c0=c0, kt=kt, ns=ns: e.matmul(
                            ps[2 + fo][:, c0:c0 + ns], wcol, XT[b][:, kt, 0:ns],
                            start=(kt == 0), stop=(kt == KT - 1)),
                            reads=[wk] + XTK, writes=[PS(2 + fo)])
                S.op("act", lambda e, b=b, fo=fo, ns=ns: e.activation(sgb[b][:, fo, 0:ns], ps[2 + fo][:, 0:ns], AF.Silu),
                     reads=[PS(2 + fo)], writes=[("sgb", b, fo)])
                S.op("dve", lambda e, b=b, fo=fo, ns=ns: e.tensor_tensor(HT[b][:, fo, 0:ns], ps[2 + fo][:, 256:256 + ns], sgb[b][:, fo, 0:ns], ALU.mult),
                     reads=[PS(2 + fo), ("sgb", b, fo)], writes=[("HT", b, fo)])
            for blk in range(nblk):
                for hf in range(2):
                    pi = 4 + 2 * blk + hf
                    for fo in range(2):
                        S.op("pe", lambda e, b=b, wb=wb, blk=blk, hf=hf, fo=fo, pi=pi: e.matmul(
                            ps[pi][:, :], HT[b][:, fo, blk * 128:(blk + 1) * 128], wd[wb][:, fo, hf * 512:(hf + 1) * 512],
                            start=(fo == 0), stop=(fo == 1)),
                            reads=[("HT", b, 0), ("HT", b, 1), ("wd", wb)], writes=[PS(pi)])
                    S.op("dve", lambda e, b=b, blk=blk, pi=pi, hf=hf: e.tensor_tensor(
                        ysb[b][:, blk, hf * 512:(hf + 1) * 512], ps[pi][:, :], ada[:, 5, hf * 512:(hf + 1) * 512], ALU.mult),
                        reads=[PS(pi)] + ADA(5), writes=[("ysb", b, blk, hf)])
            for blk in range(nblk):
                nr = 128 if (blk == 0 or kind == "d") else C1 - 128
                S.dma("sp", lambda e, b=b, blk=blk, r0=r0, nr=nr: e.dma_start(
                    out=yscr[r0 + blk * 128:r0 + blk * 128 + nr, :], in_=ysb[b][0:nr, blk, :]),
                    reads=[("ysb", b, blk, 0), ("ysb", b, blk, 1)], writes=[("yscr", u, blk)], slot="yst%d_%d" % (b, blk))
        S.barrier()
        A.release(m8)

        yg = [[A.alloc([D], F32) for _ in range(8)] for _ in range(2)]
        accA = [A.alloc([D], F32) for _ in range(2)]
        accB = [A.alloc([D], F32) for _ in range(2)]
        tmpk = [A.alloc([D], F32) for _ in range(2)]
        outb = [A.alloc([D], F32) for _ in range(2)]
        baser = [A.alloc([D], F32) for _ in range(2)]
        for b in range(2):
            for k in range(8):
                S.op("pool" if k % 2 else "dve", lambda e, b=b, k=k: e.memset(yg[b][k], 0.0), writes=[("yg", b, k, 0), ("yg", b, k, 1)])
        for t in range(NT):
            b = t % 2
            S.dma("sp", lambda e, t=t, b=b: e.dma_start(out=baser[b], in_=base_scr[t * 128:(t + 1) * 128, :]),
                  writes=[("baser", b)], slot="baser%d" % b)
            for k in range(8):
                S.dma("pool", lambda e, t=t, b=b, k=k: e.indirect_dma_start(
                    out=yg[b][k], out_offset=None, in_=yscr[:, :],
                    in_offset=bass.IndirectOffsetOnAxis(ap=idx[:, t, k:k + 1], axis=0)),
                    reads=[("yg", b, k, 0), ("yg", b, k, 1)], writes=[("yg", b, k, 0), ("yg", b, k, 1)], slot="ga%d_%d" % (b, k))
            for k in range(8):
                YK = [("yg", b, k, 0), ("yg", b, k, 1)]
                if k % 2 == 0:
                    if k == 0:
                        S.op("dve", lambda e, t=t, b=b, k=k: e.tensor_scalar_mul(accA[b], yg[b][k], gwk[:, t, k:k + 1]),
                             reads=YK, writes=[("accA", b)])
                    else:
                        S.op("dve", lambda e, t=t, b=b, k=k: e.scalar_tensor_tensor(
                            accA[b], yg[b][k], gwk[:, t, k:k + 1], accA[b], ALU.mult, ALU.add),
                            reads=YK + [("accA", b)], writes=[("accA", b)])
                else:
                    dst = accB[b] if k == 1 else tmpk[b]
                    dk = ("accB", b) if k == 1 else ("tmpk", b)
                    S.op("act", lambda e, t=t, b=b, k=k, dst=dst: e.activation(dst, yg[b][k], AF.Copy, scale=gwk[:, t, k:k + 1]),
                         reads=YK, writes=[dk])
                    if k > 1:
                        S.op("dve", lambda e, b=b: e.tensor_tensor(accB[b], accB[b], tmpk[b], ALU.add),
                             reads=[("accB", b), ("tmpk", b)], writes=[("accB", b)])
            S.op("dve", lambda e, b=b: e.tensor_tensor(accA[b], accA[b], accB[b], ALU.add),
                 reads=[("accA", b), ("accB", b)], writes=[("accA", b)])
            S.op("dve", lambda e, b=b: e.tensor_tensor(outb[b], accA[b], baser[b], ALU.add),
                 reads=[("accA", b), ("baser", b)], writes=[("outb", b)])
            S.dma("sp", lambda e, t=t, b=b: e.dma_start(out=out[t * 128:(t + 1) * 128, :], in_=outb[b]),
                  reads=[("outb", b)], writes=[("out", t)], slot="ost%d" % b)
        st = S.emit()
        print("sched", st, "arena peak KiB", A.peak / 512.0)
    return nc


def _consts():
    n = np.arange(S_TOK, dtype=np.float64)
    ang = 2 * np.pi * np.outer(n, n) / float(S_TOK)
    dftc = np.cos(ang).astype(ml_dtypes.bfloat16)
    dfts = (-np.sin(ang)).astype(ml_dtypes.bfloat16)
    c = np.arange(64, dtype=np.float64)
    a64 = 2 * np.pi * np.outer(c, c) / 64.0
    d64 = np.zeros((128, 256), np.float64)
    for g in range(2):
        d64[g * 64:(g + 1) * 64, g * 64:(g + 1) * 64] = np.cos(a64)
        d64[g * 64:(g + 1) * 64, 128 + g * 64:128 + (g + 1) * 64] = np.sin(a64)
    pos = np.arange(S_TOK, dtype=np.float32)
    inv = (np.float32(10000.0) ** (-np.arange(0, 32, 2, dtype=np.float32) / np.float32(32))).astype(np.float32)
    a = pos[:, None] * inv[None, :]
    rope = np.concatenate([np.cos(a), np.sin(a)], axis=1).astype(np.float32)
    ident = np.eye(128).astype(ml_dtypes.bfloat16)
    tri = (np.arange(128)[:, None] < np.arange(128)[None, :]).astype(ml_dtypes.bfloat16)
    ones = np.ones((128, 128), ml_dtypes.bfloat16)
    iota1 = np.broadcast_to((np.arange(NE, dtype=np.float32) * C1 + ROW0)[None, :], (128, NE)).copy()
    iokp = (np.arange(8, dtype=np.float32)[None, :] * 128 + np.arange(128, dtype=np.float32)[:, None]).astype(np.float32)
    return dict(dftc=dftc, dfts=dfts, dft64=d64.astype(ml_dtypes.bfloat16), rope=rope, ident=ident,
                tri=tri, ones=ones, iota1=iota1, iokp=iokp)


_W_NAMES = ["w_ada", "b_ada", "norm1_g", "w_in", "q_a_norm_g", "w_uq", "kv_a_norm_g", "w_ukv", "q_norm_g",
            "k_norm_g", "w_proj_attn", "w_proj_fourier", "w_out", "norm2_g", "w_router", "router_bias",
            "w_exp_gate", "w_exp_up", "w_exp_down", "w_sh_gate", "w_sh_up", "w_sh_down"]


def kernel(**inputs):
    n_cores = 8
    nc = build("full")
    shared = dict(_consts())
    for k in _W_NAMES:
        a = np.asarray(inputs[k])[0]
        if a.ndim == 1:
            a = a[None, :]
        shared[k] = np.ascontiguousarray(a)
    x = np.asarray(inputs["x"])
    c = np.asarray(inputs["c"])
    in_maps = []
    for b in range(n_cores):
        m = dict(shared)
        m["x"] = np.ascontiguousarray(x[b])
        m["c8"] = np.ascontiguousarray(c[b].reshape(8, 128).T)
        in_maps.append(m)
    res = run_bass_kernel_spmd(nc, in_maps, core_ids=list(range(n_cores)))
    return np.stack([np.asarray(r["out"]) for r in res.results], axis=0).astype(np.float32)
```

```python
import numpy as np
import ml_dtypes
from contextlib import ExitStack
import concourse.bass as bass
import concourse.mybir as mybir
from concourse.bass_utils import run_bass_kernel_spmd

F32 = mybir.dt.float32
BF16 = mybir.dt.bfloat16
I32 = mybir.dt.int32
ALU = mybir.AluOpType
AF = mybir.ActivationFunctionType
AX = mybir.AxisListType

S_TOK = 2048
D = 1024
NT = 16
KT = 8
NE = 256
C1 = 248
R_OVF = 12
ROW0 = 128
NROWS1 = ROW0 + NE * C1
NROWS = NROWS1 + R_OVF * 128
EPS = 1e-6
COMPUTE = ("pe", "act", "dve", "pool")


class Sched:
    def __init__(self, nc):
        self.nc = nc
        self.ops = []
        self.last_writer = {}
        self.readers = {}
        self.dma_last = {}
        self.last_on_eng = {}
        self.slotmap = {}

    def _add(self, eng, fn, reads, writes, dma_slot=None, extra_deps=()):
        i = len(self.ops)
        deps = set(extra_deps)
        for k in list(reads) + list(writes):
            w = self.last_writer.get(k)
            if w is not None:
                deps.add(w)
        for k in writes:
            for r in self.readers.get(k, ()):
                deps.add(r)
        if dma_slot is not None:
            qk = "sw" if eng == "pool" else "hw"
            sm = self.slotmap.setdefault(qk, {})
            dma_slot = (qk, sm.setdefault(dma_slot, len(sm)))
            p = self.dma_last.get(dma_slot)
            if p is not None:
                deps.add(p)
            self.dma_last[dma_slot] = i
        elif fn is not None and eng in COMPUTE:
            self.last_on_eng[eng] = i
        deps.discard(i)
        if eng == "pe" and dma_slot is None:
            deps = {d for d in deps if not (self.ops[d]["eng"] == "pe" and self.ops[d]["dma"] is None)}
        latest = {}
        keep = set()
        for d in deps:
            od = self.ops[d]
            if od["dma"] is None and od["fn"] is not None:
                if latest.get(od["eng"], -1) < d:
                    latest[od["eng"]] = d
            else:
                keep.add(d)
        deps = keep | set(latest.values())
        self.ops.append(dict(eng=eng, fn=fn, deps=deps, dma=dma_slot, signal=False))
        for k in writes:
            self.last_writer[k] = i
            self.readers[k] = []
        for k in reads:
            lst = self.readers.setdefault(k, [])
            if dma_slot is None:
                lst[:] = [r for r in lst if not (self.ops[r]["dma"] is None and self.ops[r]["eng"] == eng)]
            lst.append(i)
        return i

    def op(self, eng, fn, reads=(), writes=()):
        return self._add(eng, fn, reads, writes)

    def dma(self, queue, fn, reads=(), writes=(), slot=None):
        return self._add(queue, fn, reads, writes, dma_slot=slot)

    def barrier(self):
        deps = set(self.last_on_eng.values()) | set(self.dma_last.values())
        for e in ("pe", "act", "dve", "pool", "sp"):
            self._add(e, None, (), (), extra_deps=deps)
        self.last_writer = {}
        self.readers = {}
        self.slotmap = {}

    def emit(self):
        nc = self.nc
        ops = self.ops
        for o in ops:
            for d in o["deps"]:
                ops[d]["signal"] = True
        seq = {e: 0 for e in COMPUTE}
        dma_cnt = {}
        for o in ops:
            if o["dma"] is not None:
                dma_cnt[o["dma"]] = dma_cnt.get(o["dma"], 0) + 1
                o["semkey"] = ("dma", o["dma"])
                o["semval"] = 16 * dma_cnt[o["dma"]]
            elif o["signal"]:
                assert o["fn"] is not None
                seq[o["eng"]] += 1
                o["semkey"] = ("eng", o["eng"])
                o["semval"] = seq[o["eng"]]
        semkeys = [("eng", e) for e in COMPUTE] + [("dma", s) for s in dma_cnt]
        with ExitStack() as es:
            sems = {}
            for n, k in enumerate(semkeys):
                sems[k] = es.enter_context(nc.semaphore("sm%d" % n))
            streams = {e: [] for e in ("pe", "act", "dve", "pool", "sp")}
            for i, o in enumerate(ops):
                streams[o["eng"]].append(i)
            block = es.enter_context(nc.Block())
            engmap = {"pe": "tensor", "act": "scalar", "dve": "vector", "pool": "gpsimd", "sp": "sync"}

            def make(ename):
                def body(eng):
                    known = {}
                    for i in streams[ename]:
                        o = ops[i]
                        need = {}
                        for d in o["deps"]:
                            od = ops[d]
                            k, v = od["semkey"], od["semval"]
                            if known.get(k, 0) >= v:
                                continue
                            if need.get(k, 0) < v:
                                need[k] = v
                        for k, v in need.items():
                            eng.wait_ge(sems[k], v)
                            known[k] = v
                        if o["fn"] is None:
                            continue
                        ins = o["fn"](eng)
                        if o["dma"] is not None:
                            ins.then_inc(sems[o["semkey"]], 16)
                        elif o["signal"]:
                            ins.then_inc(sems[o["semkey"]], 1)
                    if ename == "sp":
                        for s, c in dma_cnt.items():
                            eng.wait_ge(sems[("dma", s)], 16 * c)
                        for e in COMPUTE:
                            if seq[e] > 0:
                                eng.wait_ge(sems[("eng", e)], seq[e])
                return body

            for ename, attr in engmap.items():
                getattr(block, attr)(make(ename))
        return dict(n_ops=len(ops), seq=seq, n_sems=len(semkeys))


class Arena:
    def __init__(self, ten, nunits):
        self.t = ten
        self.n = nunits
        self.off = 0
        self.peak = 0

    def alloc(self, shape, dtype, parts=128):
        size = {F32: 4, BF16: 2, I32: 4}[dtype]
        nel = int(np.prod(shape))
        units = (nel * size + 1) // 2
        units = (units + 31) // 32 * 32
        assert self.off + units <= self.n, ("arena overflow", self.off, units, self.n)
        v = self.t[0:parts, self.off:self.off + units]
        self.off += units
        self.peak = max(self.peak, self.off)
        if size == 4:
            v = v.bitcast(dtype)
        v = v[:, 0:nel]
        if len(shape) == 2:
            v = v.rearrange("p (a b) -> p a b", a=shape[0])
        elif len(shape) == 3:
            v = v.rearrange("p (a b c) -> p a b c", a=shape[0], b=shape[1])
        return v

    def mark(self):
        return self.off

    def release(self, m):
        self.off = m


def build(stage="full"):
    nc = bass.Bass("TRN2", target_bir_lowering=False)

    def din(name, shape, dt=F32):
        return nc.dram_tensor(name, list(shape), dt, kind="ExternalInput").ap()

    x = din("x", [S_TOK, D])
    c8 = din("c8", [128, 8])
    w_ada = din("w_ada", [D, 6 * D])
    b_ada = din("b_ada", [1, 6 * D])
    norm1_g = din("norm1_g", [1, D])
    w_in = din("w_in", [D, 2976])
    q_a_norm_g = din("q_a_norm_g", [1, 256])
    w_uq = din("w_uq", [256, 768])
    kv_a_norm_g = din("kv_a_norm_g", [1, 128])
    w_ukv = din("w_ukv", [128, 1024])
    q_norm_g = din("q_norm_g", [1, 96])
    k_norm_g = din("k_norm_g", [1, 96])
    w_pa = din("w_proj_attn", [512, D])
    w_pf = din("w_proj_fourier", [512, D])
    w_out = din("w_out", [D, D])
    norm2_g = din("norm2_g", [1, D])
    w_router = din("w_router", [D, NE])
    router_bias = din("router_bias", [1, NE])
    if stage == "full":
        w_eg = din("w_exp_gate", [NE, D, 256])
        w_eu = din("w_exp_up", [NE, D, 256])
        w_ed = din("w_exp_down", [NE, 256, D])
    w_sg = din("w_sh_gate", [D, 256])
    w_su = din("w_sh_up", [D, 256])
    w_sd = din("w_sh_down", [256, D])
    dftc = din("dftc", [S_TOK, S_TOK], BF16)
    dfts = din("dfts", [S_TOK, S_TOK], BF16)
    dft64 = din("dft64", [128, 256], BF16)
    rope = din("rope", [S_TOK, 32])
    ident_d = din("ident", [128, 128], BF16)
    tri_d = din("tri", [128, 128], BF16)
    ones_d = din("ones", [128, 128], BF16)
    iota1_d = din("iota1", [128, NE])
    iokp_d = din("iokp", [128, 8])

    out = nc.dram_tensor("out", [S_TOK, D], F32, kind="ExternalOutput").ap()

    def dscr(name, shape, dt):
        return nc.dram_tensor(name, list(shape), dt).ap()

    hT_scr = dscr("hT_scr", [128, KT * S_TOK], BF16)
    FT_scr = dscr("FT_scr", [128, 4 * S_TOK], BF16)
    AT_scr = dscr("AT_scr", [128, 4 * S_TOK], BF16)
    x1_scr = dscr("x1_scr", [S_TOK, D], F32)
    base_scr = dscr("base_scr", [S_TOK, D], F32)
    h2_scr = dscr("h2_scr", [S_TOK, D], BF16)
    xdisp = dscr("xdisp", [NROWS, D], BF16)
    yscr = dscr("yscr", [NROWS, D], F32)

    NUNITS = 205 * 512
    with ExitStack() as es:
        arena_t = es.enter_context(nc.sbuf_tensor("arena", [128, NUNITS], BF16))
        ps = [es.enter_context(nc.psum_tensor("ps%d" % i, [128, 512], F32)) for i in range(8)]
        psb = [p[:].bitcast(BF16) for p in ps]
        A = Arena(arena_t, NUNITS)
        S = Sched(nc)

        def PS(i):
            return ("ps", i)

        ident = A.alloc([128], BF16)
        tri = A.alloc([128], BF16)
        ones = A.alloc([128], BF16)
        S.dma("sp", lambda e: e.dma_start(out=ident, in_=ident_d), writes=["ident"], slot="c_ident")
        S.dma("sp", lambda e: e.dma_start(out=tri, in_=tri_d), writes=["tri"], slot="c_tri")
        S.dma("sp", lambda e: e.dma_start(out=ones, in_=ones_d), writes=["ones"], slot="c_ones")
        ada = A.alloc([6, D], F32)

        def bc_load(dst, src_row, key, slot):
            S.dma("sp", lambda e: e.dma_start(out=dst, in_=src_row.partition_broadcast(128)),
                  writes=[key], slot=slot)

        m0 = A.mark()
        hT = A.alloc([KT, S_TOK], BF16)
        m1 = A.mark()
        csil = A.alloc([8], F32)
        c_sb = A.alloc([8], F32)
        crep = A.alloc([8, 128], BF16)
        wa = [A.alloc([8, D], BF16) for _ in range(2)]
        bb = [A.alloc([D], F32) for _ in range(2)]
        gn = [A.alloc([D], F32) for _ in range(2)]
        S.dma("sp", lambda e: e.dma_start(out=c_sb, in_=c8), writes=["c_sb"], slot="c_sb")
        S.op("act", lambda e: e.activation(csil, c_sb, AF.Silu), reads=["c_sb"], writes=["csil"])
        S.op("dve", lambda e: e.tensor_copy(crep, csil.unsqueeze(2).to_broadcast([128, 8, 128])),
             reads=["csil"], writes=["crep"])
        bc_load(gn[0], norm1_g[0, :], "gn0", "gn0")
        bc_load(gn[1], norm2_g[0, :], "gn1", "gn1")
        for n, j in enumerate([1, 0, 2, 4, 3, 5]):
            b = n % 2
            S.dma("pool", lambda e, j=j, b=b: e.dma_start(
                out=wa[b], in_=w_ada[:, j * D:(j + 1) * D].rearrange("(kt p) n -> p kt n", p=128)),
                writes=[("wa", b)], slot="wa%d" % b)
            bc_load(bb[b], b_ada[0, j * D:(j + 1) * D], ("bb", b), "bb%d" % b)
            for half in range(2):
                for kt in range(KT):
                    S.op("pe", lambda e, b=b, half=half, kt=kt: e.matmul(
                        ps[half][:, :], crep[:, kt, :], wa[b][:, kt, half * 512:(half + 1) * 512],
                        start=(kt == 0), stop=(kt == KT - 1)),
                        reads=["crep", ("wa", b)], writes=[PS(half)])
                S.op("dve", lambda e, b=b, half=half, j=j: e.tensor_tensor(
                    ada[:, j, half * 512:(half + 1) * 512], ps[half][:, :], bb[b][:, half * 512:(half + 1) * 512], ALU.add),
                    reads=[PS(half), ("bb", b)], writes=[("ada", j, half)])
            if j in (1, 4):
                g = gn[0] if j == 1 else gn[1]
                gk = "gn0" if j == 1 else "gn1"
                S.op("dve", lambda e, j=j, g=g: e.scalar_tensor_tensor(
                    ada[:, j, :], ada[:, j, :], 1.0, g, ALU.add, ALU.mult),
                    reads=[("ada", j, 0), ("ada", j, 1), gk], writes=[("ada", j, 0), ("ada", j, 1)])
        ADA = lambda j: [("ada", j, 0), ("ada", j, 1)]

        xb = [A.alloc([D], F32) for _ in range(2)]
        tmpf = [A.alloc([D], F32) for _ in range(2)]
        hb = [A.alloc([D], BF16) for _ in range(2)]
        sq = A.alloc([D], F32)
        st1 = A.alloc([NT, 4], F32)
        for t in range(NT):
            b = t % 2
            S.dma("sp", lambda e, t=t, b=b: e.dma_start(out=xb[b], in_=x[t * 128:(t + 1) * 128, :]),
                  writes=[("xb", b)], slot="xb%d" % b)
            S.op("act", lambda e, t=t, b=b: e.activation(sq, xb[b], AF.Square, accum_out=st1[:, t, 0:1]),
                 reads=[("xb", b)], writes=["sq", ("st1", t)])
            S.op("act", lambda e, t=t: e.activation(st1[:, t, 1:2], st1[:, t, 0:1], AF.Sqrt, bias=EPS, scale=1.0 / D),
                 reads=[("st1", t)], writes=[("st1b", t)])
            S.op("dve", lambda e, t=t: e.reciprocal(st1[:, t, 2:3], st1[:, t, 1:2]),
                 reads=[("st1b", t)], writes=[("st1c", t)])
            S.op("dve", lambda e, t=t, b=b: e.scalar_tensor_tensor(
                tmpf[b], xb[b], st1[:, t, 2:3], ada[:, 1, :], ALU.mult, ALU.mult),
                reads=[("xb", b), ("st1c", t)] + ADA(1), writes=[("tmpf", b)])
            S.op("pool", lambda e, b=b: e.tensor_tensor(hb[b], tmpf[b], ada[:, 0, :], ALU.add),
                 reads=[("tmpf", b)] + ADA(0), writes=[("hb", b)])
            pb = 2 + b
            for kt in range(KT):
                S.op("pe", lambda e, b=b, kt=kt, pb=pb: e.transpose(
                    psb[pb][:, kt * 128:(kt + 1) * 128], hb[b][:, kt * 128:(kt + 1) * 128], ident),
                    reads=[("hb", b), "ident"], writes=[PS(pb)])
            S.op("act", lambda e, t=t, pb=pb: e.copy(
                hT[:, :, t * 128:(t + 1) * 128], psb[pb][:, :].rearrange("p (a b) -> p a b", a=8)),
                reads=[PS(pb)], writes=[("hT", t)])
        HT_ALL = [("hT", t) for t in range(NT)]
        S.dma("sp", lambda e: e.dma_start(out=hT_scr.rearrange("p (a b) -> p a b", a=KT), in_=hT),
              reads=HT_ALL, writes=["hT_scr"], slot="hT_st")
        S.barrier()
        A.release(m1)

        NORM = float(1.0 / np.sqrt(float(S_TOK * 64)))
        m2 = A.mark()
        win_f = A.alloc([KT, 512], BF16)
        d64 = A.alloc([256], BF16)
        zcs = A.alloc([NT, 4, 256], BF16)
        zft = [A.alloc([512], BF16) for _ in range(2)]
        S.dma("pool", lambda e: e.dma_start(out=win_f, in_=w_in[:, 416:928].rearrange("(kt p) n -> p kt n", p=128)),
              writes=["win_f"], slot="win_f")
        S.dma("sp", lambda e: e.dma_start(out=d64, in_=dft64), writes=["d64"], slot="d64")
        n = 0
        for cc in range(4):
            for qc in range(4):
                b = n % 2
                n += 1
                for kt in range(KT):
                    S.op("pe", lambda e, b=b, cc=cc, qc=qc, kt=kt: e.matmul(
                        ps[b][:, :], win_f[:, kt, cc * 128:(cc + 1) * 128], hT[:, kt, qc * 512:(qc + 1) * 512],
                        start=(kt == 0), stop=(kt == KT - 1)),
                        reads=["win_f"] + [("hT", qc * 4 + s) for s in range(4)], writes=[PS(b)])
                S.op("act", lambda e, b=b: e.copy(zft[b], ps[b][:, :]), reads=[PS(b)], writes=[("zft", b)])
                for sub in range(4):
                    t = qc * 4 + sub
                    pb = 2 + (sub % 2)
                    S.op("pe", lambda e, b=b, sub=sub, pb=pb: e.matmul(
                        ps[pb][:, 0:256], zft[b][:, sub * 128:(sub + 1) * 128], d64, start=True, stop=True),
                        reads=[("zft", b), "d64"], writes=[PS(pb)])
                    S.op("dve", lambda e, t=t, cc=cc, pb=pb: e.tensor_scalar_mul(
                        zcs[:, t, cc, :], ps[pb][:, 0:256], NORM),
                        reads=[PS(pb)], writes=[("zcs", t, cc)])
        FT = A.alloc([4, S_TOK], BF16)
        dbuf = [A.alloc([NT, 512], BF16) for _ in range(2)]
        n = 0
        for kc in range(4):
            for cs in range(2):
                b = n % 2
                n += 1
                src = dftc if cs == 0 else dfts
                S.dma("sp", lambda e, b=b, src=src, kc=kc: e.dma_start(
                    out=dbuf[b], in_=src[:, kc * 512:(kc + 1) * 512].rearrange("(nt p) k -> p nt k", p=128)),
                    writes=[("dbuf", b)], slot="dbuf%d" % b)
                for cc in range(4):
                    for nt in range(NT):
                        S.op("pe", lambda e, b=b, cc=cc, nt=nt, cs=cs: e.matmul(
                            ps[4 + cc][:, :], zcs[:, nt, cc, cs * 128:(cs + 1) * 128], dbuf[b][:, nt, :],
                            start=(cs == 0 and nt == 0), stop=(cs == 1 and nt == NT - 1)),
                            reads=[("zcs", nt, cc), ("dbuf", b)], writes=[PS(4 + cc)])
            for cc in range(4):
                eng = "act" if cc % 2 == 0 else "dve"
                if eng == "act":
                    S.op("act", lambda e, cc=cc, kc=kc: e.copy(FT[:, cc, kc * 512:(kc + 1) * 512], ps[4 + cc][:, :]),
                         reads=[PS(4 + cc)], writes=[("FT", cc, kc)])
                else:
                    S.op("dve", lambda e, cc=cc, kc=kc: e.tensor_copy(FT[:, cc, kc * 512:(kc + 1) * 512], ps[4 + cc][:, :]),
                         reads=[PS(4 + cc)], writes=[("FT", cc, kc)])
        S.dma("sp", lambda e: e.dma_start(out=FT_scr.rearrange("p (a b) -> p a b", a=4), in_=FT),
              reads=[("FT", cc, kc) for cc in range(4) for kc in range(4)], writes=["FT_scr"], slot="FT_st")
        S.barrier()
        A.release(m2)

        qT = A.alloc([8, S_TOK], BF16)
        kT = A.alloc([8, S_TOK], BF16)
        vb = A.alloc([NT, 8, 65], BF16)
        m3 = A.mark()
        win_a = A.alloc([KT, 416], BF16)
        wuq = A.alloc([2, 768], BF16)
        wukv = A.alloc([1024], BF16)
        ropet = A.alloc([NT, 32], F32)
        gqa = A.alloc([256], F32)
        gkva = A.alloc([128], F32)
        g96 = A.alloc([2, 96], F32)
        S.dma("pool", lambda e: e.dma_start(out=win_a, in_=w_in[:, 0:416].rearrange("(kt p) n -> p kt n", p=128)),
              writes=["win_a"], slot="win_a")
        S.dma("pool", lambda e: e.dma_start(out=wuq, in_=w_uq.rearrange("(kt p) n -> p kt n", p=128)),
              writes=["wuq"], slot="wuq")
        S.dma("pool", lambda e: e.dma_start(out=wukv, in_=w_ukv), writes=["wukv"], slot="wukv")
        S.dma("sp", lambda e: e.dma_start(out=ropet, in_=rope.rearrange("(t p) c -> p t c", p=128)),
              writes=["ropet"], slot="ropet")
        bc_load(gqa, q_a_norm_g[0, :], "gqa", "gqa")
        bc_load(gkva, kv_a_norm_g[0, :], "gkva", "gkva")
        bc_load(g96[:, 0, :], q_norm_g[0, :], "g96q", "g96q")
        bc_load(g96[:, 1, :], k_norm_g[0, :], "g96k", "g96k")
        S.op("dve", lambda e: e.tensor_scalar_mul(g96[:, 0, :], g96[:, 0, :], 96.0 ** -0.5), reads=["g96q"], writes=["g96q"])
        S.op("pool", lambda e: e.memset(vb, 1.0), writes=["vb_init"])
        NB2 = 2
        sqa = [A.alloc([768], F32) for _ in range(NB2)]
        st2 = A.alloc([NT, 40], F32)
        cqb = [A.alloc([384], BF16) for _ in range(NB2)]
        cT = [A.alloc([3, 128], BF16) for _ in range(NB2)]
        kr = [A.alloc([32], F32) for _ in range(NB2)]
        kraw = [A.alloc([8, 96], F32) for _ in range(NB2)]
        nbuf = [[A.alloc([8, 96], F32) for _ in range(2)] for _ in range(NB2)]
        rt = [[A.alloc([4, 8, 16], F32) for _ in range(2)] for _ in range(NB2)]
        qkb = [[A.alloc([8, 96], BF16) for _ in range(2)] for _ in range(NB2)]

        def norm_rope(t, which, src, src_keys, ss_col, dstT):
            w = which
            tb = t % NB2
            nb_ = nbuf[tb][w]
            rt_ = rt[tb][w]
            qb_ = qkb[tb][w]
            NK = ("nbuf", tb, w)
            gbc = g96[:, w:w + 1, :].to_broadcast([128, 8, 96])
            gkey = "g96q" if w == 0 else "g96k"
            S.op("act", lambda e: e.activation(st2[:, t, ss_col + 8:ss_col + 16], st2[:, t, ss_col:ss_col + 8],
                                               AF.Sqrt, bias=EPS, scale=1.0 / 96),
                 reads=[("ss", t, w)], writes=[("sd", t, w)])
            yield
            S.op("dve", lambda e: e.reciprocal(st2[:, t, ss_col:ss_col + 8], st2[:, t, ss_col + 8:ss_col + 16]),
                 reads=[("sd", t, w)], writes=[("rs", t, w)])
            yield
            S.op("dve", lambda e: e.tensor_tensor(
                nb_, src, st2[:, t, ss_col:ss_col + 8].unsqueeze(2).to_broadcast([128, 8, 96]), ALU.mult),
                reads=src_keys + [("rs", t, w)], writes=[NK])
            yield
            S.op("pool", lambda e: e.tensor_tensor(nb_, nb_, gbc, ALU.mult),
                 reads=[NK, gkey], writes=[NK])
            yield
            cosb = ropet[:, t, 0:16].unsqueeze(1).to_broadcast([128, 8, 16])
            sinb = ropet[:, t, 16:32].unsqueeze(1).to_broadcast([128, 8, 16])
            r1 = nb_[:, :, 64:80]
            r2 = nb_[:, :, 80:96]
            S.op("act", lambda e: e.copy(qb_[:, :, 0:64], nb_[:, :, 0:64]),
                 reads=[NK], writes=[("qkb", tb, w, 0)])
            S.op("dve", lambda e: e.tensor_tensor(rt_[:, 0, :, :], r1, cosb, ALU.mult),
                 reads=[NK, "ropet"], writes=[("rt", tb, w, 0)])
            S.op("pool", lambda e: e.tensor_tensor(rt_[:, 1, :, :], r2, sinb, ALU.mult),
                 reads=[NK, "ropet"], writes=[("rt", tb, w, 1)])
            yield
            S.op("dve", lambda e: e.tensor_tensor(rt_[:, 2, :, :], r2, cosb, ALU.mult),
                 reads=[NK, "ropet"], writes=[("rt", tb, w, 2)])
            S.op("pool", lambda e: e.tensor_tensor(rt_[:, 3, :, :], r1, sinb, ALU.mult),
                 reads=[NK, "ropet"], writes=[("rt", tb, w, 3)])
            yield
            S.op("dve", lambda e: e.tensor_tensor(qb_[:, :, 64:80], rt_[:, 0, :, :], rt_[:, 1, :, :], ALU.subtract),
                 reads=[("rt", tb, w, 0), ("rt", tb, w, 1)], writes=[("qkb", tb, w, 1)])
            S.op("pool", lambda e: e.tensor_tensor(qb_[:, :, 80:96], rt_[:, 2, :, :], rt_[:, 3, :, :], ALU.add),
                 reads=[("rt", tb, w, 2), ("rt", tb, w, 3)], writes=[("qkb", tb, w, 2)])
            yield
            pb = 6 + w
            for h in range(8):
                S.op("pe", lambda e, h=h: e.transpose(psb[pb][0:96, h * 128:(h + 1) * 128], qb_[:, h, :], ident),
                     reads=[("qkb", tb, w, 0), ("qkb", tb, w, 1), ("qkb", tb, w, 2), "ident"], writes=[PS(pb)])
            S.op("act", lambda e: e.copy(dstT[0:96, :, t * 128:(t + 1) * 128],
                                         psb[pb][0:96, :].rearrange("p (a b) -> p a b", a=8)),
                 reads=[PS(pb)], writes=[("qkT", w, t)])
            yield

        def prep_tile(t):
            tb = t % NB2
            sq_ = sqa[tb]
            SQ = ("sqa", tb)
            for kt in range(KT):
                S.op("pe", lambda e, kt=kt: e.matmul(
                    ps[0][:, 0:416], hT[:, kt, t * 128:(t + 1) * 128], win_a[:, kt, :],
                    start=(kt == 0), stop=(kt == KT - 1)),
                    reads=[("hT", t), "win_a"], writes=[PS(0)])
            S.op("act", lambda e: e.activation(sq_[:, 0:256], ps[0][:, 0:256], AF.Square, accum_out=st2[:, t, 0:1]),
                 reads=[PS(0)], writes=[SQ, ("s0", t)])
            S.op("act", lambda e: e.activation(sq_[:, 256:384], ps[0][:, 256:384], AF.Square, accum_out=st2[:, t, 1:2]),
                 reads=[PS(0)], writes=[SQ, ("s1", t)])
            S.op("act", lambda e: e.activation(st2[:, t, 4:5], st2[:, t, 0:1], AF.Sqrt, bias=EPS, scale=1.0 / 256),
                 reads=[("s0", t)], writes=[("s0b", t)])
            S.op("act", lambda e: e.activation(st2[:, t, 5:6], st2[:, t, 1:2], AF.Sqrt, bias=EPS, scale=1.0 / 128),
                 reads=[("s1", t)], writes=[("s1b", t)])
            S.op("dve", lambda e: e.reciprocal(st2[:, t, 6:8], st2[:, t, 4:6]),
                 reads=[("s0b", t), ("s1b", t)], writes=[("s01c", t)])
            S.op("dve", lambda e: e.scalar_tensor_tensor(
                cqb[tb][:, 0:256], ps[0][:, 0:256], st2[:, t, 6:7], gqa, ALU.mult, ALU.mult),
                reads=[PS(0), ("s01c", t), "gqa"], writes=[("cqb0", tb)])
            S.op("dve", lambda e: e.scalar_tensor_tensor(
                cqb[tb][:, 256:384], ps[0][:, 256:384], st2[:, t, 7:8], gkva, ALU.mult, ALU.mult),
                reads=[PS(0), ("s01c", t), "gkva"], writes=[("cqb1", tb)])
            S.op("act", lambda e: e.copy(kr[tb], ps[0][:, 384:416]), reads=[PS(0)], writes=[("kr", tb)])
            for j in range(3):
                S.op("pe", lambda e, j=j: e.transpose(psb[1][:, j * 128:(j + 1) * 128], cqb[tb][:, j * 128:(j + 1) * 128], ident),
                     reads=[("cqb0", tb), ("cqb1", tb), "ident"], writes=[PS(1)])
            S.op("act", lambda e: e.copy(cT[tb], psb[1][:, 0:384].rearrange("p (a b) -> p a b", a=3)),
                 reads=[PS(1)], writes=[("cT", tb)])
            for j in range(2):
                S.op("pe", lambda e, j=j: e.matmul(ps[2][:, :], cT[tb][:, j, :], wuq[:, j, 0:512], start=(j == 0), stop=(j == 1)),
                     reads=[("cT", tb), "wuq"], writes=[PS(2)])
            for j in range(2):
                S.op("pe", lambda e, j=j: e.matmul(ps[3][:, 0:256], cT[tb][:, j, :], wuq[:, j, 512:768], start=(j == 0), stop=(j == 1)),
                     reads=[("cT", tb), "wuq"], writes=[PS(3)])
            for hf in range(2):
                S.op("pe", lambda e, hf=hf: e.matmul(ps[4 + hf][:, :], cT[tb][:, 2, :], wukv[:, hf * 512:(hf + 1) * 512], start=True, stop=True),
                     reads=[("cT", tb), "wukv"], writes=[PS(4 + hf)])
            S.op("act", lambda e: e.copy(sq_[:, 0:512], ps[2][:, :]), reads=[PS(2)], writes=[SQ])
            S.op("act", lambda e: e.copy(sq_[:, 512:768], ps[3][:, 0:256]), reads=[PS(3)], writes=[SQ])
            qraw = sq_[:, 0:768].rearrange("p (a b) -> p a b", a=8)
            S.op("dve", lambda e: e.tensor_tensor(nbuf[tb][0], qraw, qraw, ALU.mult), reads=[SQ], writes=[("nbuf", tb, 0)])
            S.op("dve", lambda e: e.tensor_reduce(st2[:, t, 8:16], nbuf[tb][0], AX.X, ALU.add),
                 reads=[("nbuf", tb, 0)], writes=[("ss", t, 0)])
            for hf in range(2):
                S.op("act", lambda e, hf=hf: e.copy(
                    kraw[tb][:, hf * 4:(hf + 1) * 4, 0:64],
                    ps[4 + hf][:, :].rearrange("p (a b) -> p a b", a=4)[:, :, 0:64]),
                    reads=[PS(4 + hf)], writes=[("kraw", tb, hf)])
                S.op("dve", lambda e, hf=hf: e.tensor_copy(
                    vb[:, t, hf * 4:(hf + 1) * 4, 0:64],
                    ps[4 + hf][:, :].rearrange("p (a b) -> p a b", a=4)[:, :, 64:128]),
                    reads=[PS(4 + hf), "vb_init"], writes=[("vb", t, hf)])
            S.op("pool", lambda e: e.tensor_copy(kraw[tb][:, :, 64:96], kr[tb].unsqueeze(1).to_broadcast([128, 8, 32])),
                 reads=[("kr", tb)], writes=[("kraw", tb, 2)])
            KR = [("kraw", tb, 0), ("kraw", tb, 1), ("kraw", tb, 2)]
            S.op("dve", lambda e: e.tensor_tensor(nbuf[tb][1], kraw[tb], kraw[tb], ALU.mult),
                 reads=KR, writes=[("nbuf", tb, 1)])
            S.op("dve", lambda e: e.tensor_reduce(st2[:, t, 24:32], nbuf[tb][1], AX.X, ALU.add),
                 reads=[("nbuf", tb, 1)], writes=[("ss", t, 1)])
            gq = norm_rope(t, 0, qraw, [SQ], 8, qT)
            gk = norm_rope(t, 1, kraw[tb], KR, 24, kT)
            alive = [gq, gk]
            while alive:
                for g in list(alive):
                    try:
                        next(g)
                    except StopIteration:
                        alive.remove(g)

        for t in range(NT):
            prep_tile(t)
        S.barrier()
        A.release(m3)

        m4 = A.mark()
        A_tok = A.alloc([NT, 512], BF16)
        pT = [A.alloc([512], BF16) for _ in range(4)]
        rden = A.alloc([2, 4], F32)
        AT = A.alloc([4, S_TOK], BF16)
        QK_ALL = lambda w: [("qkT", w, t) for t in range(NT)]
        its = [(h, qc, j) for h in range(8) for qc in range(4) for j in range(NT)]
        LOOK = 2

        def emit_qk(i):
            h, qc, j = its[i]
            sbk = i % 4
            S.op("pe", lambda e: e.matmul(
                ps[sbk][:, :], kT[0:96, h, j * 128:(j + 1) * 128], qT[0:96, h, qc * 512:(qc + 1) * 512],
                start=True, stop=True),
                reads=[("qkT", 1, j)] + [("qkT", 0, qc * 4 + s_) for s_ in range(4)], writes=[PS(sbk)])
            S.op("act", lambda e: e.activation(pT[sbk], ps[sbk][:, :], AF.Exp),
                 reads=[PS(sbk)], writes=[("pT", sbk)])

        for i in range(min(LOOK, len(its))):
            emit_qk(i)
        for i, (h, qc, j) in enumerate(its):
            if i + LOOK < len(its):
                emit_qk(i + LOOK)
            sbk = i % 4
            grp = i // NT
            ob = 4 + (grp % 2)
            for sub in range(4):
                S.op("pe", lambda e, h=h, j=j, sbk=sbk, sub=sub, ob=ob: e.matmul(
                    ps[ob][:, sub * 128:sub * 128 + 65], pT[sbk][:, sub * 128:(sub + 1) * 128], vb[:, j, h, :],
                    start=(j == 0), stop=(j == NT - 1)),
                    reads=[("pT", sbk), ("vb", j, h // 4), "vb_init"], writes=[PS(ob)])
            if j == NT - 1:
                o4 = ps[ob][:, :].rearrange("p (a b) -> p a b", a=4)
                rb = grp % 2
                S.op("dve", lambda e, o4=o4, rb=rb: e.reciprocal(rden[:, rb, :].unsqueeze(2), o4[:, :, 64:65]),
                     reads=[PS(ob)], writes=[("rden", rb)])
                S.op("dve", lambda e, o4=o4, rb=rb, h=h, qc=qc: e.tensor_tensor(
                    A_tok[:, qc * 4:(qc + 1) * 4, h * 64:(h + 1) * 64], o4[:, :, 0:64],
                    rden[:, rb, :].unsqueeze(2).to_broadcast([128, 4, 64]), ALU.mult),
                    reads=[PS(ob), ("rden", rb)], writes=[("A_tok", qc, h)])
        for t in range(NT):
            pb = 6 + (t % 2)
            for j in range(4):
                S.op("pe", lambda e, t=t, j=j, pb=pb: e.transpose(
                    psb[pb][:, j * 128:(j + 1) * 128], A_tok[:, t, j * 128:(j + 1) * 128], ident),
                    reads=[("A_tok", t // 4, h) for h in range(8)] + ["ident"], writes=[PS(pb)])
            S.op("act", lambda e, t=t, pb=pb: e.copy(
                AT[:, :, t * 128:(t + 1) * 128], psb[pb][:, 0:512].rearrange("p (a b) -> p a b", a=4)),
                reads=[PS(pb)], writes=[("AT", t)])
        S.dma("sp", lambda e: e.dma_start(out=AT_scr.rearrange("p (a b) -> p a b", a=4), in_=AT),
              reads=[("AT", t) for t in range(NT)], writes=["AT_scr"], slot="AT_st")
        S.barrier()
        A.release(m0)

        m5 = A.mark()
        win_g = A.alloc([KT, 2048], BF16)
        wpa = A.alloc([4, D], BF16)
        wpf = A.alloc([4, D], BF16)
        wout = A.alloc([KT, D], BF16)
        hTc = [A.alloc([KT, 512], BF16) for _ in range(2)]
        ATc = [A.alloc([4, 512], BF16) for _ in range(2)]
        FTc = [A.alloc([4, 512], BF16) for _ in range(2)]
        mTc = [A.alloc([KT, 512], BF16) for _ in range(2)]
        sa = [A.alloc([512], F32) for _ in range(2)]
        sf = [A.alloc([512], F32) for _ in range(2)]
        mm1 = [A.alloc([512], F32) for _ in range(2)]
        mm2 = [A.alloc([512], F32) for _ in range(2)]
        xb5 = [A.alloc([D], F32) for _ in range(4)]
        t5 = [A.alloc([D], F32) for _ in range(2)]
        x1b = [A.alloc([D], F32) for _ in range(2)]
        for half in range(2):
            S.dma("pool", lambda e, half=half: e.dma_start(
                out=win_g[:, :, half * 1024:(half + 1) * 1024],
                in_=w_in[:, 928 + half * 1024:928 + (half + 1) * 1024].rearrange("(kt p) n -> p kt n", p=128)),
                writes=[("win_g", half)], slot="win_g%d" % half)
        S.dma("pool", lambda e: e.dma_start(out=wpa, in_=w_pa.rearrange("(kt p) n -> p kt n", p=128)), writes=["wpa"], slot="wpa")
        S.dma("pool", lambda e: e.dma_start(out=wpf, in_=w_pf.rearrange("(kt p) n -> p kt n", p=128)), writes=["wpf"], slot="wpf")
        S.dma("pool", lambda e: e.dma_start(out=wout, in_=w_out.rearrange("(kt p) n -> p kt n", p=128)), writes=["wout"], slot="wout")
        nfc = 0

        def load_chunk(qc):
            cb = qc % 2
            S.dma("sp", lambda e: e.dma_start(
                out=hTc[cb], in_=hT_scr.rearrange("p (a b) -> p a b", a=KT)[:, :, qc * 512:(qc + 1) * 512]),
                reads=["hT_scr"], writes=[("hTc", cb)], slot="hTc%d" % cb)
            S.dma("sp", lambda e: e.dma_start(
                out=ATc[cb], in_=AT_scr.rearrange("p (a b) -> p a b", a=4)[:, :, qc * 512:(qc + 1) * 512]),
                reads=["AT_scr"], writes=[("ATc", cb)], slot="ATc%d" % cb)
            S.dma("sp", lambda e: e.dma_start(
                out=FTc[cb], in_=FT_scr.rearrange("p (a b) -> p a b", a=4)[:, :, qc * 512:(qc + 1) * 512]),
                reads=["FT_scr"], writes=[("FTc", cb)], slot="FTc%d" % cb)

        load_chunk(0)
        for qc in range(4):
            cb = qc % 2
            if qc + 1 < 4:
                load_chunk(qc + 1)
            for sub in range(4):
                t_ = qc * 4 + sub
                S.dma("sp", lambda e, t_=t_, sub=sub: e.dma_start(out=xb5[sub], in_=x[t_ * 128:(t_ + 1) * 128, :]),
                      writes=[("xb5", sub)], slot="xb5%d" % sub)
            for fc in range(8):
                pbase = 4 * (nfc % 2)
                eb = nfc % 2
                nfc += 1
                for kt in range(KT):
                    S.op("pe", lambda e, cb=cb, fc=fc, kt=kt, pbase=pbase: e.matmul(
                        ps[pbase][:, :], win_g[:, kt, fc * 128:(fc + 1) * 128], hTc[cb][:, kt, :],
                        start=(kt == 0), stop=(kt == KT - 1)),
                        reads=[("win_g", 0), ("hTc", cb)], writes=[PS(pbase)])
                for kt in range(KT):
                    S.op("pe", lambda e, cb=cb, fc=fc, kt=kt, pbase=pbase: e.matmul(
                        ps[pbase + 1][:, :], win_g[:, kt, 1024 + fc * 128:1024 + (fc + 1) * 128], hTc[cb][:, kt, :],
                        start=(kt == 0), stop=(kt == KT - 1)),
                        reads=[("win_g", 1), ("hTc", cb)], writes=[PS(pbase + 1)])
                for j in range(4):
                    S.op("pe", lambda e, cb=cb, fc=fc, j=j, pbase=pbase: e.matmul(
                        ps[pbase + 2][:, :], wpa[:, j, fc * 128:(fc + 1) * 128], ATc[cb][:, j, :],
                        start=(j == 0), stop=(j == 3)),
                        reads=["wpa", ("ATc", cb)], writes=[PS(pbase + 2)])
                for j in range(4):
                    S.op("pe", lambda e, cb=cb, fc=fc, j=j, pbase=pbase: e.matmul(
                        ps[pbase + 3][:, :], wpf[:, j, fc * 128:(fc + 1) * 128], FTc[cb][:, j, :],
                        start=(j == 0), stop=(j == 3)),
                        reads=["wpf", ("FTc", cb)], writes=[PS(pbase + 3)])
                S.op("act", lambda e, eb=eb, pbase=pbase: e.activation(sa[eb], ps[pbase][:, :], AF.Sigmoid),
                     reads=[PS(pbase)], writes=[("sa", eb)])
                S.op("act", lambda e, eb=eb, pbase=pbase: e.activation(sf[eb], ps[pbase + 1][:, :], AF.Sigmoid),
                     reads=[PS(pbase + 1)], writes=[("sf", eb)])
                S.op("dve", lambda e, eb=eb, pbase=pbase: e.tensor_tensor(mm1[eb], ps[pbase + 2][:, :], sa[eb], ALU.mult),
                     reads=[PS(pbase + 2), ("sa", eb)], writes=[("mm1", eb)])
                S.op("dve", lambda e, eb=eb, pbase=pbase: e.tensor_tensor(mm2[eb], ps[pbase + 3][:, :], sf[eb], ALU.mult),
                     reads=[PS(pbase + 3), ("sf", eb)], writes=[("mm2", eb)])
                S.op("pool", lambda e, eb=eb, cb=cb, fc=fc: e.tensor_tensor(mTc[cb][:, fc, :], mm1[eb], mm2[eb], ALU.add),
                     reads=[("mm1", eb), ("mm2", eb)], writes=[("mTc", cb, fc)])
            for sub in range(4):
                t = qc * 4 + sub
                xbi = t % 2
                p0 = 2 * sub
                for hf in range(2):
                    for fc in range(8):
                        S.op("pe", lambda e, cb=cb, sub=sub, hf=hf, fc=fc, p0=p0: e.matmul(
                            ps[p0 + hf][:, :], mTc[cb][:, fc, sub * 128:(sub + 1) * 128], wout[:, fc, hf * 512:(hf + 1) * 512],
                            start=(fc == 0), stop=(fc == 7)),
                            reads=[("mTc", cb, fc), "wout"], writes=[PS(p0 + hf)])
                    S.op("dve", lambda e, xbi=xbi, hf=hf, p0=p0: e.tensor_tensor(
                        t5[xbi][:, hf * 512:(hf + 1) * 512], ps[p0 + hf][:, :], ada[:, 2, hf * 512:(hf + 1) * 512], ALU.mult),
                        reads=[PS(p0 + hf)] + ADA(2), writes=[("t5", xbi, hf)])
                S.op("pool", lambda e, xbi=xbi, sub=sub: e.tensor_tensor(x1b[xbi], t5[xbi], xb5[sub], ALU.add),
                     reads=[("t5", xbi, 0), ("t5", xbi, 1), ("xb5", sub)], writes=[("x1b", xbi)])
                dst = out if stage == "x1" else x1_scr
                S.dma("sp", lambda e, t=t, xbi=xbi, dst=dst: e.dma_start(out=dst[t * 128:(t + 1) * 128, :], in_=x1b[xbi]),
                      reads=[("x1b", xbi)], writes=[("x1_scr", t)], slot="x1st%d" % xbi)
        S.barrier()
        A.release(m5)
        if stage == "x1":
            st = S.emit()
            print("sched", st, "arena peak KiB", A.peak / 512.0)
            return nc

        gwk = A.alloc([NT, 8], F32)
        idx = A.alloc([NT, 8], I32)
        widx = A.alloc([R_OVF], I32)
        m6 = A.mark()
        Gw = A.alloc([NT, NE], F32)
        Pos = A.alloc([NT, NE], F32)
        run_bc = A.alloc([NE], F32)
        iota1 = A.alloc([NE], F32)
        m7 = A.mark()
        wr_f = A.alloc([KT, NE], F32)
        wr_hi = A.alloc([KT, NE], BF16)
        wr_lo = A.alloc([KT, NE], BF16)
        wsh = A.alloc([KT, 512], BF16)
        wshd = A.alloc([2, D], BF16)
        rbias = A.alloc([NE], F32)
        tmp2 = [A.alloc([D], F32) for _ in range(2)]
        h2hi = [A.alloc([D], BF16) for _ in range(2)]
        h2lo = [A.alloc([D], BF16) for _ in range(2)]
        h2T = [A.alloc([KT, 128], BF16) for _ in range(2)]
        h2loT = [A.alloc([KT, 128], BF16) for _ in range(2)]
        sq5_ = [A.alloc([D], F32)]
        st5 = A.alloc([NT, 8], F32)
        scb_ = [A.alloc([NE], F32) for _ in range(2)]
        biased_ = [A.alloc([NE], F32) for _ in range(2)]
        wsel_ = [A.alloc([NE], F32) for _ in range(2)]
        maskb_ = [A.alloc([NE], BF16) for _ in range(2)]
        top8 = A.alloc([NT, 8], F32)
        sg_ = [A.alloc([256], F32) for _ in range(2)]
        hsh_ = [A.alloc([256], BF16) for _ in range(2)]
        hshT_ = [A.alloc([2, 128], BF16) for _ in range(2)]
        t6 = [A.alloc([D], F32) for _ in range(2)]
        S.dma("sp", lambda e: e.dma_start(out=wr_f, in_=w_router.rearrange("(kt p) n -> p kt n", p=128)), writes=["wr_f"], slot="wr_f")
        S.op("act", lambda e: e.copy(wr_hi, wr_f), reads=["wr_f"], writes=["wr_hi"])
        S.op("dve", lambda e: e.tensor_tensor(wr_lo, wr_f, wr_hi, ALU.subtract), reads=["wr_f", "wr_hi"], writes=["wr_lo"])
        S.dma("pool", lambda e: e.dma_start(out=wsh[:, :, 0:256], in_=w_sg.rearrange("(kt p) n -> p kt n", p=128)), writes=["wsh0"], slot="wsh0")
        S.dma("pool", lambda e: e.dma_start(out=wsh[:, :, 256:512], in_=w_su.rearrange("(kt p) n -> p kt n", p=128)), writes=["wsh1"], slot="wsh1")
        S.dma("pool", lambda e: e.dma_start(out=wshd, in_=w_sd.rearrange("(kt p) n -> p kt n", p=128)), writes=["wshd"], slot="wshd")
        bc_load(rbias, router_bias[0, :], "rbias", "rbias")
        S.dma("sp", lambda e: e.dma_start(out=iota1, in_=iota1_d), writes=["iota1"], slot="iota1")
        S.op("dve", lambda e: e.memset(run_bc, 0.0), writes=["run_bc"])
        x1all = A.alloc([NT, D], F32)
        sgs_ = [A.alloc([256], F32) for _ in range(2)]
        for t in range(NT):
            S.dma("sp", lambda e, t=t: e.dma_start(out=x1all[:, t, :], in_=x1_scr[t * 128:(t + 1) * 128, :]),
                  writes=[("x1t", t)], slot="x1t%d" % (t % 4))
        for t in range(NT):
            S.op("act", lambda e, t=t: e.activation(sq5_[0], x1all[:, t, :], AF.Square, accum_out=st5[:, t, 0:1]),
                 reads=[("x1t", t)], writes=["sq5", ("st5", t)])
        ST5 = [("st5", t) for t in range(NT)]
        S.op("act", lambda e: e.activation(st5[:, :, 1], st5[:, :, 0], AF.Sqrt, bias=EPS, scale=1.0 / D),
             reads=ST5, writes=["st5b"])
        S.op("dve", lambda e: e.reciprocal(st5[:, :, 2], st5[:, :, 1]), reads=["st5b"], writes=["st5c"])

        def stage5A(t):
            b = t % 2
            S.op("dve", lambda e: e.scalar_tensor_tensor(
                tmp2[b], x1all[:, t, :], st5[:, t, 2:3], ada[:, 4, :], ALU.mult, ALU.mult),
                reads=[("x1t", t), "st5c"] + ADA(4), writes=[("tmp2", b)])
            S.op("pool", lambda e: e.tensor_tensor(tmp2[b], tmp2[b], ada[:, 3, :], ALU.add),
                 reads=[("tmp2", b)] + ADA(3), writes=[("tmp2", b)])
            S.op("act", lambda e: e.copy(h2hi[b], tmp2[b]), reads=[("tmp2", b)], writes=[("h2hi", b)])
            S.op("dve", lambda e: e.tensor_tensor(h2lo[b], tmp2[b], h2hi[b], ALU.subtract),
                 reads=[("tmp2", b), ("h2hi", b)], writes=[("h2lo", b)])
            S.dma("sp", lambda e: e.dma_start(out=h2_scr[t * 128:(t + 1) * 128, :], in_=h2hi[b]),
                  reads=[("h2hi", b)], writes=[("h2_scr", t)], slot="h2st%d" % b)
            for kt in range(KT):
                S.op("pe", lambda e, kt=kt: e.transpose(psb[0][:, kt * 128:(kt + 1) * 128], h2hi[b][:, kt * 128:(kt + 1) * 128], ident),
                     reads=[("h2hi", b), "ident"], writes=[PS(0)])
            S.op("act", lambda e: e.copy(h2T[b], psb[0][:, :].rearrange("p (a b) -> p a b", a=8)),
                 reads=[PS(0)], writes=[("h2T", b)])
            for kt in range(KT):
                S.op("pe", lambda e, kt=kt: e.transpose(psb[1][:, kt * 128:(kt + 1) * 128], h2lo[b][:, kt * 128:(kt + 1) * 128], ident),
                     reads=[("h2lo", b), "ident"], writes=[PS(1)])
            S.op("dve", lambda e: e.tensor_copy(h2loT[b], psb[1][:, :].rearrange("p (a b) -> p a b", a=8)),
                 reads=[PS(1)], writes=[("h2loT", b)])

        def stage5B(t):
            b = t % 2
            scb = scb_[b]; biased = biased_[b]; wsel = wsel_[b]; maskb = maskb_[b]
            sg = sg_[b]; sgs = sgs_[b]; hsh = hsh_[b]; hshT = hshT_[b]
            nmm = 0
            for (xi, wi) in [(0, 0), (0, 1), (1, 0)]:
                for kt in range(KT):
                    lt = h2T[b] if xi == 0 else h2loT[b]
                    wt = wr_hi if wi == 0 else wr_lo
                    S.op("pe", lambda e, lt=lt, wt=wt, kt=kt, nmm=nmm: e.matmul(
                        ps[2][:, 0:NE], lt[:, kt, :], wt[:, kt, :], start=(nmm == 0), stop=(nmm == 23)),
                        reads=[("h2T", b), ("h2loT", b), "wr_hi", "wr_lo"], writes=[PS(2)])
                    nmm += 1
            for kt in range(KT):
                S.op("pe", lambda e, kt=kt: e.matmul(
                    ps[3][:, :], h2T[b][:, kt, :], wsh[:, kt, :], start=(kt == 0), stop=(kt == KT - 1)),
                    reads=[("h2T", b), "wsh0", "wsh1"], writes=[PS(3)])
            S.op("act", lambda e: e.activation(scb, ps[2][:, 0:NE], AF.Sigmoid), reads=[PS(2)], writes=[("scb", b)])
            S.op("act", lambda e: e.activation(sgs, ps[3][:, 0:256], AF.Sigmoid), reads=[PS(3)], writes=[("sgs", b)])
            S.op("dve", lambda e: e.tensor_tensor(biased, scb, rbias, ALU.add), reads=[("scb", b), "rbias"], writes=[("biased", b)])
            S.op("dve", lambda e: e.max(top8[:, t, :], biased), reads=[("biased", b)], writes=[("top8", t)])
            S.op("dve", lambda e: e.scalar_tensor_tensor(
                wsel, biased, top8[:, t, 7:8], scb, ALU.is_ge, ALU.mult, accum_out=st5[:, t, 3:4]),
                reads=[("biased", b), ("top8", t), ("scb", b)], writes=[("wsel", b), ("den", t)])
            S.op("dve", lambda e: e.tensor_scalar(maskb, biased, top8[:, t, 7:8], None, ALU.is_ge),
                 reads=[("biased", b), ("top8", t)], writes=[("maskb", b)])
            S.op("dve", lambda e: e.reciprocal(st5[:, t, 4:5], st5[:, t, 3:4]), reads=[("den", t)], writes=[("rden5", t)])
            S.op("dve", lambda e: e.tensor_scalar(Gw[:, t, :], wsel, st5[:, t, 4:5], 2.5, ALU.mult, ALU.mult),
                 reads=[("wsel", b), ("rden5", t)], writes=[("Gw", t)])
            S.op("pe", lambda e: e.matmul(ps[4][:, 0:NE], tri, maskb, start=True, stop=True),
                 reads=["tri", ("maskb", b)], writes=[PS(4)])
            S.op("pe", lambda e: e.matmul(ps[4][:, NE:2 * NE], ones, maskb, start=True, stop=True),
                 reads=["ones", ("maskb", b)], writes=[PS(4)])
            S.op("dve", lambda e: e.tensor_tensor(Pos[:, t, :], ps[4][:, 0:NE], run_bc, ALU.add),
                 reads=[PS(4), "run_bc"], writes=[("Pos", t)])
            S.op("dve", lambda e: e.tensor_tensor(run_bc, ps[4][:, NE:2 * NE], run_bc, ALU.add),
                 reads=[PS(4), "run_bc"], writes=["run_bc"])
            S.op("dve", lambda e: e.tensor_tensor(sg, ps[3][:, 0:256], sgs, ALU.mult), reads=[PS(3), ("sgs", b)], writes=[("sg", b)])
            S.op("dve", lambda e: e.tensor_tensor(hsh, ps[3][:, 256:512], sg, ALU.mult), reads=[PS(3), ("sg", b)], writes=[("hsh", b)])
            for j in range(2):
                S.op("pe", lambda e, j=j: e.transpose(psb[5][:, j * 128:(j + 1) * 128], hsh[:, j * 128:(j + 1) * 128], ident),
                     reads=[("hsh", b), "ident"], writes=[PS(5)])
            S.op("act", lambda e: e.copy(hshT, psb[5][:, 0:256].rearrange("p (a b) -> p a b", a=2)), reads=[PS(5)], writes=[("hshT", b)])
            for hf in range(2):
                for j in range(2):
                    S.op("pe", lambda e, hf=hf, j=j: e.matmul(
                        ps[6 + hf][:, :], hshT[:, j, :], wshd[:, j, hf * 512:(hf + 1) * 512], start=(j == 0), stop=(j == 1)),
                        reads=[("hshT", b), "wshd"], writes=[PS(6 + hf)])
                S.op("dve", lambda e, hf=hf: e.tensor_tensor(
                    t6[b][:, hf * 512:(hf + 1) * 512], ps[6 + hf][:, :], ada[:, 5, hf * 512:(hf + 1) * 512], ALU.mult),
                    reads=[PS(6 + hf)] + ADA(5), writes=[("t6", b, hf)])
            S.op("pool", lambda e: e.tensor_tensor(t6[b], t6[b], x1all[:, t, :], ALU.add),
                 reads=[("t6", b, 0), ("t6", b, 1), ("x1t", t)], writes=[("t6", b, 0), ("t6", b, 1)])
            S.dma("sp", lambda e: e.dma_start(out=base_scr[t * 128:(t + 1) * 128, :], in_=t6[b]),
                  reads=[("t6", b, 0), ("t6", b, 1)], writes=[("base_scr", t)], slot="bst%d" % b)

        stage5A(0)
        for t in range(NT):
            if t + 1 < NT:
                stage5A(t + 1)
            stage5B(t)
        S.barrier()
        A.release(m7)

        a1 = A.alloc([NE], F32)
        a2 = A.alloc([NE], F32)
        maddr = A.alloc([NE], F32)
        junk6 = [A.alloc([NE], F32) for _ in range(2)]
        top8a = A.alloc([NT, 8], F32)
        h2r = [A.alloc([D], BF16) for _ in range(2)]
        nex = A.alloc([NE], F32)
        pfx = [A.alloc([NE], F32) for _ in range(2)]
        base2 = A.alloc([NE], F32)
        d12 = A.alloc([NE], F32)
        bexp = A.alloc([R_OVF], F32)
        iokp = A.alloc([8], F32)
        wif = A.alloc([R_OVF], F32)
        onesf = A.alloc([NE], F32)
        S.op("pool", lambda e: e.memset(onesf, 1.0), writes=["onesf"])
        S.dma("sp", lambda e: e.dma_start(out=iokp, in_=iokp_d), writes=["iokp"], slot="iokp")
        S.op("dve", lambda e: e.tensor_scalar(nex, run_bc, float(C1), None, ALU.is_gt), reads=["run_bc"], writes=["nex"])
        for j in range(1, -(-(S_TOK - C1) // 128)):
            S.op("dve", lambda e, j=j: e.scalar_tensor_tensor(nex, run_bc, float(C1 + 128 * j), nex, ALU.is_gt, ALU.add),
                 reads=["run_bc", "nex"], writes=["nex"])
        S.op("dve", lambda e: e.tensor_copy(pfx[0], nex), reads=["nex"], writes=[("pfx", 0)])
        cur = 0
        sh = 1
        while sh < NE:
            nxt = 1 - cur
            S.op("dve", lambda e, cur=cur, nxt=nxt, sh=sh: e.tensor_tensor(pfx[nxt][:, sh:NE], pfx[cur][:, sh:NE], pfx[cur][:, 0:NE - sh], ALU.add),
                 reads=[("pfx", cur)], writes=[("pfx", nxt)])
            S.op("dve", lambda e, cur=cur, nxt=nxt, sh=sh: e.tensor_copy(pfx[nxt][:, 0:sh], pfx[cur][:, 0:sh]),
                 reads=[("pfx", cur)], writes=[("pfx", nxt)])
            cur = nxt
            sh *= 2
        obend = pfx[cur]
        OBK = ("pfx", cur)
        S.op("dve", lambda e: e.tensor_tensor(base2, obend, nex, ALU.subtract), reads=[OBK, "nex"], writes=["base2"])
        S.op("dve", lambda e: e.tensor_scalar(base2, base2, 128.0, float(NROWS1 - C1), ALU.mult, ALU.add), reads=["base2"], writes=["base2"])
        S.op("dve", lambda e: e.tensor_tensor(d12, iota1, base2, ALU.subtract), reads=["iota1", "base2"], writes=["d12"])
        for bq in range(R_OVF):
            S.op("dve", lambda e, bq=bq: e.scalar_tensor_tensor(
                junk6[bq % 2], obend, float(bq), onesf, ALU.is_le, ALU.mult, accum_out=bexp[:, bq:bq + 1]),
                reads=[OBK, "onesf"], writes=[("junk6", bq % 2), ("bexp", bq)])
        BEXP = [("bexp", bq) for bq in range(R_OVF)]
        S.op("dve", lambda e: e.tensor_scalar_min(bexp, bexp, float(NE - 1)), reads=BEXP, writes=["bexpc"])
        S.op("dve", lambda e: e.scalar_tensor_tensor(
            wif, bexp, 128.0, iokp[:, 0:1].to_broadcast([128, R_OVF]), ALU.mult, ALU.add),
            reads=["bexpc", "iokp"], writes=["wif"])
        S.op("dve", lambda e: e.tensor_copy(widx, wif), reads=["wif"], writes=["widx"])
        for t in range(NT):
            b = t % 2
            S.op("dve", lambda e, t=t: e.scalar_tensor_tensor(a1, Pos[:, t, :], float(C1), d12, ALU.is_lt, ALU.mult),
                 reads=[("Pos", t), "d12"], writes=["a1"])
            S.op("pool", lambda e, t=t: e.tensor_tensor(a2, Pos[:, t, :], base2, ALU.add),
                 reads=[("Pos", t), "base2"], writes=["a2"])
            S.op("dve", lambda e: e.tensor_tensor(a1, a1, a2, ALU.add), reads=["a1", "a2"], writes=["a1"])
            S.op("dve", lambda e, t=t: e.scalar_tensor_tensor(Gw[:, t, :], a1, float(NROWS), Gw[:, t, :], ALU.is_lt, ALU.mult),
                 reads=[("Gw", t), "a1"], writes=[("Gw", t)])
            S.op("dve", lambda e, t=t: e.scalar_tensor_tensor(maddr, Gw[:, t, :], 0.0, a1, ALU.is_gt, ALU.mult),
                 reads=[("Gw", t), "a1"], writes=["maddr"])
            S.op("dve", lambda e, t=t: e.max(top8a[:, t, :], maddr), reads=["maddr"], writes=[("top8a", t)])
            S.op("dve", lambda e, t=t: e.tensor_copy(idx[:, t, :], top8a[:, t, :]), reads=[("top8a", t)], writes=[("idx", t)])
            for k in range(8):
                jb = k % 2
                S.op("dve", lambda e, t=t, k=k, jb=jb: e.scalar_tensor_tensor(
                    junk6[jb], maddr, top8a[:, t, k:k + 1], Gw[:, t, :], ALU.is_equal, ALU.mult, accum_out=gwk[:, t, k:k + 1]),
                    reads=["maddr", ("top8a", t), ("Gw", t)], writes=[("junk6", jb), ("gwk", t, k)])
            S.dma("sp", lambda e, t=t, b=b: e.dma_start(out=h2r[b], in_=h2_scr[t * 128:(t + 1) * 128, :]),
                  writes=[("h2r", b)], slot="h2r%d" % b)
            for k in range(8):
                S.dma("pool", lambda e, t=t, b=b, k=k: e.indirect_dma_start(
                    out=xdisp[:, :], out_offset=bass.IndirectOffsetOnAxis(ap=idx[:, t, k:k + 1], axis=0),
                    in_=h2r[b], in_offset=None),
                    reads=[("h2r", b), ("idx", t)], writes=[("xdisp", t, k)], slot="sc%d_%d" % (b, k))
        S.barrier()
        A.release(m6)

        m8 = A.mark()
        NXB = 4
        xs = [A.alloc([2, D], BF16) for _ in range(NXB)]
        XT = [A.alloc([KT, 256], BF16) for _ in range(2)]
        NWB = 5
        wg = [A.alloc([KT, 256], BF16) for _ in range(NWB)]
        wu = [A.alloc([KT, 256], BF16) for _ in range(NWB)]
        wd = [A.alloc([2, D], BF16) for _ in range(NWB)]
        sgb = [A.alloc([2, 256], F32) for _ in range(2)]
        HT = [A.alloc([2, 256], BF16) for _ in range(2)]
        ysb = [A.alloc([2, D], F32) for _ in range(2)]
        w32g = [A.alloc([KT, 256], F32) for _ in range(2)]
        w32u = [A.alloc([KT, 256], F32) for _ in range(2)]
        w32d = [A.alloc([2, D], F32) for _ in range(2)]
        weg_rows = w_eg.rearrange("e (p kt) f -> (e p) (kt f)", kt=KT)
        weu_rows = w_eu.rearrange("e (p kt) f -> (e p) (kt f)", kt=KT)
        wed_rows = w_ed.rearrange("e (p j) d -> (e p) (j d)", j=2)
        S.op("dve", lambda e: e.memset(ysb[0], 0.0), writes=[("ysb", 0, 0, 0), ("ysb", 0, 0, 1), ("ysb", 0, 1, 0), ("ysb", 0, 1, 1)])
        S.dma("sp", lambda e: e.dma_start(out=yscr[0:ROW0, :], in_=ysb[0][:, 0, :]),
              reads=[("ysb", 0, 0, 0), ("ysb", 0, 0, 1)], writes=["yscr_trash"], slot="yst0_0")
        for xb_ in range(NXB):
            S.op("pool", lambda e, xb_=xb_: e.memset(xs[xb_], 0.0), writes=[("xs", xb_)])
        units = [("s", e_, ROW0 + e_ * C1, 2) for e_ in range(NE)] + [("d", bq, NROWS1 + bq * 128, 1) for bq in range(R_OVF)]

        def load_x(u):
            kind, ui, r0_, nblk = units[u]
            xb_ = u % NXB
            S.dma("sp", lambda e: e.dma_start(out=xs[xb_][:, 0, :], in_=xdisp[r0_:r0_ + 128, :]),
                  writes=[("xs", xb_, 0)], reads=[("xs", xb_)], slot="xs%d_0" % xb_)
            if nblk == 2:
                S.dma("sp", lambda e: e.dma_start(out=xs[xb_][0:C1 - 128, 1, :], in_=xdisp[r0_ + 128:r0_ + C1, :]),
                      writes=[("xs", xb_, 1)], reads=[("xs", xb_)], slot="xs%d_1" % xb_)

        for u in range(NXB - 1):
            load_x(u)
        for u, (kind, ui, r0, nblk) in enumerate(units):
            b = u % 2
            xbi = u % NXB
            wb = u % NWB
            ns = nblk * 128
            if u + NXB - 1 < len(units):
                load_x(u + NXB - 1)
            if kind == "s":
                S.dma("pool", lambda e, wb=wb, ui=ui: e.dma_start(out=wg[wb], in_=w_eg[ui].rearrange("(kt p) f -> p kt f", p=128)),
                      writes=[("wg", wb)], slot="wg%d" % wb)
                S.dma("pool", lambda e, wb=wb, ui=ui: e.dma_start(out=wu[wb], in_=w_eu[ui].rearrange("(kt p) f -> p kt f", p=128)),
                      writes=[("wu", wb)], slot="wu%d" % wb)
                S.dma("pool", lambda e, wb=wb, ui=ui: e.dma_start(out=wd[wb], in_=w_ed[ui].rearrange("(j p) d -> p j d", p=128)),
                      writes=[("wd", wb)], slot="wd%d" % wb)
            else:
                db = ui % 2
                for (dst, rows_ap, nm) in ((w32g[db], weg_rows, "g"), (w32u[db], weu_rows, "u"), (w32d[db], wed_rows, "d")):
                    S.dma("pool", lambda e, dst=dst, rows_ap=rows_ap, ui=ui: e.indirect_dma_start(
                        out=dst.rearrange("p a b -> p (a b)"), out_offset=None, in_=rows_ap[:, :],
                        in_offset=bass.IndirectOffsetOnAxis(ap=widx[:, ui:ui + 1], axis=0)),
                        reads=["widx"], writes=[("w32" + nm, db)], slot="dyn_" + nm)
                S.op("act", lambda e, db=db, wb=wb: e.copy(wg[wb], w32g[db]),
                     reads=[("w32g", db)], writes=[("wg", wb)])
                S.op("dve", lambda e, db=db, wb=wb: e.tensor_copy(wu[wb], w32u[db]),
                     reads=[("w32u", db)], writes=[("wu", wb)])
                S.op("act", lambda e, db=db, wb=wb: e.copy(wd[wb], w32d[db]),
                     reads=[("w32d", db)], writes=[("wd", wb)])
            for blk in range(nblk):
                for kt in range(KT):
                    xin_ = xs[xbi][:, blk, kt:D:KT] if kind == "d" else xs[xbi][:, blk, kt * 128:(kt + 1) * 128]
                    S.op("pe", lambda e, blk=blk, kt=kt, xin_=xin_: e.transpose(
                        psb[blk][:, kt * 128:(kt + 1) * 128], xin_, ident),
                        reads=[("xs", xbi, blk), "ident"], writes=[PS(blk)])
                if blk == 0:
                    S.op("act", lambda e, b=b: e.copy(XT[b][:, :, 0:128], psb[0][:, :].rearrange("p (a b) -> p a b", a=8)),
                         reads=[PS(0)], writes=[("XT", b, 0)])
                else:
                    S.op("dve", lambda e, b=b: e.tensor_copy(XT[b][:, :, 128:256], psb[1][:, :].rearrange("p (a b) -> p a b", a=8)),
                         reads=[PS(1)], writes=[("XT", b, 1)])
            XTK = [("XT", b, blk) for blk in range(nblk)]
            for fo in range(2):
                for (wt, wk, c0) in ((wg[wb], ("wg", wb), 0), (wu[wb], ("wu", wb), 256)):
                    for kt in range(KT):
                        wcol = wt[:, kt, fo:256:2] if kind == "d" else wt[:, kt, fo * 128:(fo + 1) * 128]
                        S.op("pe", lambda e, b=b, fo=fo, wcol=wcol, c0=c0, kt=kt, ns=ns: e.matmul(
                            ps[2 + fo][:, c0:c0 + ns], wcol, XT[b][:, kt, 0:ns],
                            start=(kt == 0), stop=(kt == KT - 1)),
                            reads=[wk] + XTK, writes=[PS(2 + fo)])
                S.op("act", lambda e, b=b, fo=fo, ns=ns: e.activation(sgb[b][:, fo, 0:ns], ps[2 + fo][:, 0:ns], AF.Silu),
                     reads=[PS(2 + fo)], writes=[("sgb", b, fo)])
                S.op("dve", lambda e, b=b, fo=fo, ns=ns: e.tensor_tensor(HT[b][:, fo, 0:ns], ps[2 + fo][:, 256:256 + ns], sgb[b][:, fo, 0:ns], ALU.mult),
                     reads=[PS(2 + fo), ("sgb", b, fo)], writes=[("HT", b, fo)])
            for blk in range(nblk):
                for hf in range(2):
                    pi = 4 + 2 * blk + hf
                    for fo in range(2):
                        S.op("pe", lambda e, b=b, wb=wb, blk=blk, hf=hf, fo=fo, pi=pi: e.matmul(
                            ps[pi][:, :], HT[b][:, fo, blk * 128:(blk + 1) * 128], wd[wb][:, fo, hf * 512:(hf + 1) * 512],
                            start=(fo == 0), stop=(fo == 1)),
                            reads=[("HT", b, 0), ("HT", b, 1), ("wd", wb)], writes=[PS(pi)])
                    if hf == 0:
                        S.op("act", lambda e, b=b, blk=blk, pi=pi: e.copy(ysb[b][:, blk, 0:512], ps[pi][:, :]),
                             reads=[PS(pi)], writes=[("ysb", b, blk, 0)])
                    else:
                        S.op("dve", lambda e, b=b, blk=blk, pi=pi: e.tensor_copy(ysb[b][:, blk, 512:1024], ps[pi][:, :]),
                             reads=[PS(pi)], writes=[("ysb", b, blk, 1)])
            for blk in range(nblk):
                nr = 128 if (blk == 0 or kind == "d") else C1 - 128
                S.dma("sp", lambda e, b=b, blk=blk, r0=r0, nr=nr: e.dma_start(
                    out=yscr[r0 + blk * 128:r0 + blk * 128 + nr, :], in_=ysb[b][0:nr, blk, :]),
                    reads=[("ysb", b, blk, 0), ("ysb", b, blk, 1)], writes=[("yscr", u, blk)], slot="yst%d_%d" % (b, blk))
        S.barrier()
        A.release(m8)

        yg = [[A.alloc([D], F32) for _ in range(8)] for _ in range(2)]
        accA = [A.alloc([D], F32) for _ in range(2)]
        accB = [A.alloc([D], F32) for _ in range(2)]
        tmpk = [A.alloc([D], F32) for _ in range(2)]
        baser = [A.alloc([D], F32) for _ in range(2)]
        outb = [A.alloc([D], F32) for _ in range(2)]
        for b in range(2):
            for k in range(8):
                S.op("pool" if k % 2 else "dve", lambda e, b=b, k=k: e.memset(yg[b][k], 0.0), writes=[("yg", b, k, 0), ("yg", b, k, 1)])
        for t in range(NT):
            b = t % 2
            S.dma("sp", lambda e, t=t, b=b: e.dma_start(out=baser[b], in_=base_scr[t * 128:(t + 1) * 128, :]),
                  writes=[("baser", b)], slot="baser%d" % b)
            for k in range(8):
                S.dma("pool", lambda e, t=t, b=b, k=k: e.indirect_dma_start(
                    out=yg[b][k], out_offset=None, in_=yscr[:, :],
                    in_offset=bass.IndirectOffsetOnAxis(ap=idx[:, t, k:k + 1], axis=0)),
                    reads=[("yg", b, k, 0), ("yg", b, k, 1)], writes=[("yg", b, k, 0), ("yg", b, k, 1)], slot="ga%d_%d" % (b, k))
            for k in range(8):
                if k % 2 == 0:
                    if k == 0:
                        S.op("dve", lambda e, t=t, b=b, k=k: e.tensor_scalar_mul(accA[b], yg[b][k], gwk[:, t, k:k + 1]),
                             reads=[("yg", b, k, 0), ("yg", b, k, 1)], writes=[("accA", b)])
                    else:
                        S.op("dve", lambda e, t=t, b=b, k=k: e.scalar_tensor_tensor(
                            accA[b], yg[b][k], gwk[:, t, k:k + 1], accA[b], ALU.mult, ALU.add),
                            reads=[("yg", b, k, 0), ("yg", b, k, 1), ("accA", b)], writes=[("accA", b)])
                else:
                    dst = accB[b] if k == 1 else tmpk[b]
                    dk = ("accB", b) if k == 1 else ("tmpk", b)
                    S.op("act", lambda e, t=t, b=b, k=k, dst=dst: e.activation(dst, yg[b][k], AF.Copy, scale=gwk[:, t, k:k + 1]),
                         reads=[("yg", b, k, 0), ("yg", b, k, 1)], writes=[dk])
                    if k > 1:
                        S.op("dve", lambda e, b=b: e.tensor_tensor(accB[b], accB[b], tmpk[b], ALU.add),
                             reads=[("accB", b), ("tmpk", b)], writes=[("accB", b)])
            S.op("dve", lambda e, b=b: e.tensor_tensor(accA[b], accA[b], accB[b], ALU.add),
                 reads=[("accA", b), ("accB", b)], writes=[("accA", b)])
            S.op("dve", lambda e, b=b: e.tensor_tensor(accA[b], accA[b], ada[:, 5, :], ALU.mult),
                 reads=[("accA", b)], writes=[("accA", b)])
            S.op("dve", lambda e, b=b: e.tensor_tensor(outb[b], accA[b], baser[b], ALU.add),
                 reads=[("accA", b), ("baser", b)], writes=[("outb", b)])
            S.dma("sp", lambda e, t=t, b=b: e.dma_start(out=out[t * 128:(t + 1) * 128, :], in_=outb[b]),
                  reads=[("outb", b)], writes=[("out", t)], slot="ost%d" % b)
        st = S.emit()
        print("sched", st, "arena peak KiB", A.peak / 512.0)
    return nc


def _consts():
    n = np.arange(S_TOK, dtype=np.float64)
    ang = 2 * np.pi * np.outer(n, n) / float(S_TOK)
    dftc = np.cos(ang).astype(ml_dtypes.bfloat16)
    dfts = (-np.sin(ang)).astype(ml_dtypes.bfloat16)
    c = np.arange(64, dtype=np.float64)
    a64 = 2 * np.pi * np.outer(c, c) / 64.0
    d64 = np.zeros((128, 256), np.float64)
    for g in range(2):
        d64[g * 64:(g + 1) * 64, g * 64:(g + 1) * 64] = np.cos(a64)
        d64[g * 64:(g + 1) * 64, 128 + g * 64:128 + (g + 1) * 64] = np.sin(a64)
    pos = np.arange(S_TOK, dtype=np.float32)
    inv = (np.float32(10000.0) ** (-np.arange(0, 32, 2, dtype=np.float32) / np.float32(32))).astype(np.float32)
    a = pos[:, None] * inv[None, :]
    rope = np.concatenate([np.cos(a), np.sin(a)], axis=1).astype(np.float32)
    ident = np.eye(128).astype(ml_dtypes.bfloat16)
    tri = (np.arange(128)[:, None] < np.arange(128)[None, :]).astype(ml_dtypes.bfloat16)
    ones = np.ones((128, 128), ml_dtypes.bfloat16)
    iota1 = np.broadcast_to((np.arange(NE, dtype=np.float32) * C1 + ROW0)[None, :], (128, NE)).copy()
    iokp = (np.arange(8, dtype=np.float32)[None, :] * 128 + np.arange(128, dtype=np.float32)[:, None]).astype(np.float32)
    return dict(dftc=dftc, dfts=dfts, dft64=d64.astype(ml_dtypes.bfloat16), rope=rope, ident=ident,
                tri=tri, ones=ones, iota1=iota1, iokp=iokp)


_W_NAMES = ["w_ada", "b_ada", "norm1_g", "w_in", "q_a_norm_g", "w_uq", "kv_a_norm_g", "w_ukv", "q_norm_g",
            "k_norm_g", "w_proj_attn", "w_proj_fourier", "w_out", "norm2_g", "w_router", "router_bias",
            "w_exp_gate", "w_exp_up", "w_exp_down", "w_sh_gate", "w_sh_up", "w_sh_down"]


def kernel(**inputs):
    n_cores = 8
    nc = build("full")
    shared = dict(_consts())
    for k in _W_NAMES:
        a = np.asarray(inputs[k])[0]
        if a.ndim == 1:
            a = a[None, :]
        shared[k] = np.ascontiguousarray(a)
    x = np.asarray(inputs["x"])
    c = np.asarray(inputs["c"])
    in_maps = []
    for b in range(n_cores):
        m = dict(shared)
        m["x"] = np.ascontiguousarray(x[b])
        m["c8"] = np.ascontiguousarray(c[b].reshape(8, 128).T)
        in_maps.append(m)
    res = run_bass_kernel_spmd(nc, in_maps, core_ids=list(range(n_cores)))
    return np.stack([np.asarray(r["out"]) for r in res.results], axis=0).astype(np.float32)
```

```python
import numpy as np
import ml_dtypes
from contextlib import ExitStack
import concourse.bass as bass
import concourse.mybir as mybir
from concourse.bass_utils import run_bass_kernel_spmd

F32 = mybir.dt.float32
BF16 = mybir.dt.bfloat16
I32 = mybir.dt.int32
ALU = mybir.AluOpType
AF = mybir.ActivationFunctionType
AX = mybir.AxisListType

S_TOK = 2048
D = 1024
NT = 16
KT = 8
NE = 256
C1 = 248
R_OVF = 12
ROW0 = 128
NROWS1 = ROW0 + NE * C1
NROWS = NROWS1 + R_OVF * 128
EPS = 1e-6
COMPUTE = ("pe", "act", "dve", "pool")


class Sched:
    def __init__(self, nc):
        self.nc = nc
        self.ops = []
        self.last_writer = {}
        self.readers = {}
        self.dma_last = {}
        self.last_on_eng = {}
        self.slotmap = {}

    def _add(self, eng, fn, reads, writes, dma_slot=None, extra_deps=()):
        i = len(self.ops)
        deps = set(extra_deps)
        for k in list(reads) + list(writes):
            w = self.last_writer.get(k)
            if w is not None:
                deps.add(w)
        for k in writes:
            for r in self.readers.get(k, ()):
                deps.add(r)
        if dma_slot is not None:
            qk = "sw" if eng == "pool" else "hw"
            sm = self.slotmap.setdefault(qk, {})
            dma_slot = (qk, sm.setdefault(dma_slot, len(sm)))
            p = self.dma_last.get(dma_slot)
            if p is not None:
                deps.add(p)
            self.dma_last[dma_slot] = i
        elif fn is not None and eng in COMPUTE:
            self.last_on_eng[eng] = i
        deps.discard(i)
        if eng == "pe" and dma_slot is None:
            deps = {d for d in deps if not (self.ops[d]["eng"] == "pe" and self.ops[d]["dma"] is None)}
        latest = {}
        keep = set()
        for d in deps:
            od = self.ops[d]
            if od["dma"] is None and od["fn"] is not None:
                if latest.get(od["eng"], -1) < d:
                    latest[od["eng"]] = d
            else:
                keep.add(d)
        deps = keep | set(latest.values())
        self.ops.append(dict(eng=eng, fn=fn, deps=deps, dma=dma_slot, signal=False))
        for k in writes:
            self.last_writer[k] = i
            self.readers[k] = []
        for k in reads:
            lst = self.readers.setdefault(k, [])
            if dma_slot is None:
                lst[:] = [r for r in lst if not (self.ops[r]["dma"] is None and self.ops[r]["eng"] == eng)]
            lst.append(i)
        return i

    def op(self, eng, fn, reads=(), writes=()):
        return self._add(eng, fn, reads, writes)

    def dma(self, queue, fn, reads=(), writes=(), slot=None):
        return self._add(queue, fn, reads, writes, dma_slot=slot)

    def barrier(self):
        deps = set(self.last_on_eng.values()) | set(self.dma_last.values())
        for e in ("pe", "act", "dve", "pool", "sp"):
            self._add(e, None, (), (), extra_deps=deps)
        self.last_writer = {}
        self.readers = {}
        self.slotmap = {}

    def emit(self):
        nc = self.nc
        ops = self.ops
        for o in ops:
            for d in o["deps"]:
                ops[d]["signal"] = True
        seq = {e: 0 for e in COMPUTE}
        dma_cnt = {}
        for o in ops:
            if o["dma"] is not None:
                dma_cnt[o["dma"]] = dma_cnt.get(o["dma"], 0) + 1
                o["semkey"] = ("dma", o["dma"])
                o["semval"] = 16 * dma_cnt[o["dma"]]
            elif o["signal"]:
                assert o["fn"] is not None
                seq[o["eng"]] += 1
                o["semkey"] = ("eng", o["eng"])
                o["semval"] = seq[o["eng"]]
        semkeys = [("eng", e) for e in COMPUTE] + [("dma", s) for s in dma_cnt]
        with ExitStack() as es:
            sems = {}
            for n, k in enumerate(semkeys):
                sems[k] = es.enter_context(nc.semaphore("sm%d" % n))
            streams = {e: [] for e in ("pe", "act", "dve", "pool", "sp")}
            for i, o in enumerate(ops):
                streams[o["eng"]].append(i)
            block = es.enter_context(nc.Block())
            engmap = {"pe": "tensor", "act": "scalar", "dve": "vector", "pool": "gpsimd", "sp": "sync"}

            def make(ename):
                def body(eng):
                    known = {}
                    for i in streams[ename]:
                        o = ops[i]
                        need = {}
                        for d in o["deps"]:
                            od = ops[d]
                            k, v = od["semkey"], od["semval"]
                            if known.get(k, 0) >= v:
                                continue
                            if need.get(k, 0) < v:
                                need[k] = v
                        for k, v in need.items():
                            eng.wait_ge(sems[k], v)
                            known[k] = v
                        if o["fn"] is None:
                            continue
                        ins = o["fn"](eng)
                        if o["dma"] is not None:
                            ins.then_inc(sems[o["semkey"]], 16)
                        elif o["signal"]:
                            ins.then_inc(sems[o["semkey"]], 1)
                    if ename == "sp":
                        for s, c in dma_cnt.items():
                            eng.wait_ge(sems[("dma", s)], 16 * c)
                        for e in COMPUTE:
                            if seq[e] > 0:
                                eng.wait_ge(sems[("eng", e)], seq[e])
                return body

            for ename, attr in engmap.items():
                getattr(block, attr)(make(ename))
        return dict(n_ops=len(ops), seq=seq, n_sems=len(semkeys))


class Arena:
    def __init__(self, ten, nunits):
        self.t = ten
        self.n = nunits
        self.off = 0
        self.peak = 0

    def alloc(self, shape, dtype, parts=128):
        size = {F32: 4, BF16: 2, I32: 4}[dtype]
        nel = int(np.prod(shape))
        units = (nel * size + 1) // 2
        units = (units + 31) // 32 * 32
        assert self.off + units <= self.n, ("arena overflow", self.off, units, self.n)
        v = self.t[0:parts, self.off:self.off + units]
        self.off += units
        self.peak = max(self.peak, self.off)
        if size == 4:
            v = v.bitcast(dtype)
        v = v[:, 0:nel]
        if len(shape) == 2:
            v = v.rearrange("p (a b) -> p a b", a=shape[0])
        elif len(shape) == 3:
            v = v.rearrange("p (a b c) -> p a b c", a=shape[0], b=shape[1])
        return v

    def mark(self):
        return self.off

    def release(self, m):
        self.off = m


def build(stage="full"):
    nc = bass.Bass("TRN2", target_bir_lowering=False)

    def din(name, shape, dt=F32):
        return nc.dram_tensor(name, list(shape), dt, kind="ExternalInput").ap()

    x = din("x", [S_TOK, D])
    c8 = din("c8", [128, 8])
    w_ada = din("w_ada", [D, 6 * D])
    b_ada = din("b_ada", [1, 6 * D])
    norm1_g = din("norm1_g", [1, D])
    w_in = din("w_in", [D, 2976])
    q_a_norm_g = din("q_a_norm_g", [1, 256])
    w_uq = din("w_uq", [256, 768])
    kv_a_norm_g = din("kv_a_norm_g", [1, 128])
    w_ukv = din("w_ukv", [128, 1024])
    q_norm_g = din("q_norm_g", [1, 96])
    k_norm_g = din("k_norm_g", [1, 96])
    w_pa = din("w_proj_attn", [512, D])
    w_pf = din("w_proj_fourier", [512, D])
    w_out = din("w_out", [D, D])
    norm2_g = din("norm2_g", [1, D])
    w_router = din("w_router", [D, NE])
    router_bias = din("router_bias", [1, NE])
    if stage == "full":
        w_eg = din("w_exp_gate", [NE, D, 256])
        w_eu = din("w_exp_up", [NE, D, 256])
        w_ed = din("w_exp_down", [NE, 256, D])
    w_sg = din("w_sh_gate", [D, 256])
    w_su = din("w_sh_up", [D, 256])
    w_sd = din("w_sh_down", [256, D])
    dftc = din("dftc", [S_TOK, S_TOK], BF16)
    dfts = din("dfts", [S_TOK, S_TOK], BF16)
    dft64 = din("dft64", [128, 256], BF16)
    rope = din("rope", [S_TOK, 32])
    ident_d = din("ident", [128, 128], BF16)
    tri_d = din("tri", [128, 128], BF16)
    ones_d = din("ones", [128, 128], BF16)
    iota1_d = din("iota1", [128, NE])
    iokp_d = din("iokp", [128, 8])

    out = nc.dram_tensor("out", [S_TOK, D], F32, kind="ExternalOutput").ap()

    def dscr(name, shape, dt):
        return nc.dram_tensor(name, list(shape), dt).ap()

    hT_scr = dscr("hT_scr", [128, KT * S_TOK], BF16)
    FT_scr = dscr("FT_scr", [128, 4 * S_TOK], BF16)
    AT_scr = dscr("AT_scr", [128, 4 * S_TOK], BF16)
    x1_scr = dscr("x1_scr", [S_TOK, D], F32)
    base_scr = dscr("base_scr", [S_TOK, D], F32)
    h2_scr = dscr("h2_scr", [S_TOK, D], BF16)
    xdisp = dscr("xdisp", [NROWS, D], BF16)
    yscr = dscr("yscr", [NROWS, D], F32)

    NUNITS = 205 * 512
    with ExitStack() as es:
        arena_t = es.enter_context(nc.sbuf_tensor("arena", [128, NUNITS], BF16))
        ps = [es.enter_context(nc.psum_tensor("ps%d" % i, [128, 512], F32)) for i in range(8)]
        psb = [p[:].bitcast(BF16) for p in ps]
        A = Arena(arena_t, NUNITS)
        S = Sched(nc)

        def PS(i):
            return ("ps", i)

        ident = A.alloc([128], BF16)
        tri = A.alloc([128], BF16)
        ones = A.alloc([128], BF16)
        S.dma("sp", lambda e: e.dma_start(out=ident, in_=ident_d), writes=["ident"], slot="c_ident")
        S.dma("sp", lambda e: e.dma_start(out=tri, in_=tri_d), writes=["tri"], slot="c_tri")
        S.dma("sp", lambda e: e.dma_start(out=ones, in_=ones_d), writes=["ones"], slot="c_ones")
        ada = A.alloc([6, D], F32)

        def bc_load(dst, src_row, key, slot):
            S.dma("sp", lambda e: e.dma_start(out=dst, in_=src_row.partition_broadcast(128)),
                  writes=[key], slot=slot)

        m0 = A.mark()
        hT = A.alloc([KT, S_TOK], BF16)
        m1 = A.mark()
        csil = A.alloc([8], F32)
        c_sb = A.alloc([8], F32)
        crep = A.alloc([8, 128], BF16)
        wa = [A.alloc([8, D], BF16) for _ in range(2)]
        bb = [A.alloc([D], F32) for _ in range(2)]
        gn = [A.alloc([D], F32) for _ in range(2)]
        S.dma("sp", lambda e: e.dma_start(out=c_sb, in_=c8), writes=["c_sb"], slot="c_sb")
        S.op("act", lambda e: e.activation(csil, c_sb, AF.Silu), reads=["c_sb"], writes=["csil"])
        S.op("dve", lambda e: e.tensor_copy(crep, csil.unsqueeze(2).to_broadcast([128, 8, 128])),
             reads=["csil"], writes=["crep"])
        bc_load(gn[0], norm1_g[0, :], "gn0", "gn0")
        bc_load(gn[1], norm2_g[0, :], "gn1", "gn1")
        for n, j in enumerate([1, 0, 2, 4, 3, 5]):
            b = n % 2
            S.dma("pool", lambda e, j=j, b=b: e.dma_start(
                out=wa[b], in_=w_ada[:, j * D:(j + 1) * D].rearrange("(kt p) n -> p kt n", p=128)),
                writes=[("wa", b)], slot="wa%d" % b)
            bc_load(bb[b], b_ada[0, j * D:(j + 1) * D], ("bb", b), "bb%d" % b)
            for half in range(2):
                for kt in range(KT):
                    S.op("pe", lambda e, b=b, half=half, kt=kt: e.matmul(
                        ps[half][:, :], crep[:, kt, :], wa[b][:, kt, half * 512:(half + 1) * 512],
                        start=(kt == 0), stop=(kt == KT - 1)),
                        reads=["crep", ("wa", b)], writes=[PS(half)])
                S.op("dve", lambda e, b=b, half=half, j=j: e.tensor_tensor(
                    ada[:, j, half * 512:(half + 1) * 512], ps[half][:, :], bb[b][:, half * 512:(half + 1) * 512], ALU.add),
                    reads=[PS(half), ("bb", b)], writes=[("ada", j, half)])
            if j in (1, 4):
                g = gn[0] if j == 1 else gn[1]
                gk = "gn0" if j == 1 else "gn1"
                S.op("dve", lambda e, j=j, g=g: e.scalar_tensor_tensor(
                    ada[:, j, :], ada[:, j, :], 1.0, g, ALU.add, ALU.mult),
                    reads=[("ada", j, 0), ("ada", j, 1), gk], writes=[("ada", j, 0), ("ada", j, 1)])
        ADA = lambda j: [("ada", j, 0), ("ada", j, 1)]

        xb = [A.alloc([D], F32) for _ in range(2)]
        tmpf = [A.alloc([D], F32) for _ in range(2)]
        hb = [A.alloc([D], BF16) for _ in range(2)]
        sq = A.alloc([D], F32)
        st1 = A.alloc([NT, 4], F32)
        for t in range(NT):
            b = t % 2
            S.dma("sp", lambda e, t=t, b=b: e.dma_start(out=xb[b], in_=x[t * 128:(t + 1) * 128, :]),
                  writes=[("xb", b)], slot="xb%d" % b)
            S.op("act", lambda e, t=t, b=b: e.activation(sq, xb[b], AF.Square, accum_out=st1[:, t, 0:1]),
                 reads=[("xb", b)], writes=["sq", ("st1", t)])
            S.op("act", lambda e, t=t: e.activation(st1[:, t, 1:2], st1[:, t, 0:1], AF.Sqrt, bias=EPS, scale=1.0 / D),
                 reads=[("st1", t)], writes=[("st1b", t)])
            S.op("dve", lambda e, t=t: e.reciprocal(st1[:, t, 2:3], st1[:, t, 1:2]),
                 reads=[("st1b", t)], writes=[("st1c", t)])
            S.op("dve", lambda e, t=t, b=b: e.scalar_tensor_tensor(
                tmpf[b], xb[b], st1[:, t, 2:3], ada[:, 1, :], ALU.mult, ALU.mult),
                reads=[("xb", b), ("st1c", t)] + ADA(1), writes=[("tmpf", b)])
            S.op("pool", lambda e, b=b: e.tensor_tensor(hb[b], tmpf[b], ada[:, 0, :], ALU.add),
                 reads=[("tmpf", b)] + ADA(0), writes=[("hb", b)])
            pb = 2 + b
            for kt in range(KT):
                S.op("pe", lambda e, b=b, kt=kt, pb=pb: e.transpose(
                    psb[pb][:, kt * 128:(kt + 1) * 128], hb[b][:, kt * 128:(kt + 1) * 128], ident),
                    reads=[("hb", b), "ident"], writes=[PS(pb)])
            S.op("act", lambda e, t=t, pb=pb: e.copy(
                hT[:, :, t * 128:(t + 1) * 128], psb[pb][:, :].rearrange("p (a b) -> p a b", a=8)),
                reads=[PS(pb)], writes=[("hT", t)])
        HT_ALL = [("hT", t) for t in range(NT)]
        S.dma("sp", lambda e: e.dma_start(out=hT_scr.rearrange("p (a b) -> p a b", a=KT), in_=hT),
              reads=HT_ALL, writes=["hT_scr"], slot="hT_st")
        S.barrier()
        A.release(m1)

        NORM = float(1.0 / np.sqrt(float(S_TOK * 64)))
        m2 = A.mark()
        win_f = A.alloc([KT, 512], BF16)
        d64 = A.alloc([256], BF16)
        zcs = A.alloc([NT, 4, 256], BF16)
        zft = [A.alloc([512], BF16) for _ in range(2)]
        S.dma("pool", lambda e: e.dma_start(out=win_f, in_=w_in[:, 416:928].rearrange("(kt p) n -> p kt n", p=128)),
              writes=["win_f"], slot="win_f")
        S.dma("sp", lambda e: e.dma_start(out=d64, in_=dft64), writes=["d64"], slot="d64")
        n = 0
        for cc in range(4):
            for qc in range(4):
                b = n % 2
                n += 1
                for kt in range(KT):
                    S.op("pe", lambda e, b=b, cc=cc, qc=qc, kt=kt: e.matmul(
                        ps[b][:, :], win_f[:, kt, cc * 128:(cc + 1) * 128], hT[:, kt, qc * 512:(qc + 1) * 512],
                        start=(kt == 0), stop=(kt == KT - 1)),
                        reads=["win_f"] + [("hT", qc * 4 + s) for s in range(4)], writes=[PS(b)])
                S.op("act", lambda e, b=b: e.copy(zft[b], ps[b][:, :]), reads=[PS(b)], writes=[("zft", b)])
                for sub in range(4):
                    t = qc * 4 + sub
                    pb = 2 + (sub % 2)
                    S.op("pe", lambda e, b=b, sub=sub, pb=pb: e.matmul(
                        ps[pb][:, 0:256], zft[b][:, sub * 128:(sub + 1) * 128], d64, start=True, stop=True),
                        reads=[("zft", b), "d64"], writes=[PS(pb)])
                    S.op("dve", lambda e, t=t, cc=cc, pb=pb: e.tensor_scalar_mul(
                        zcs[:, t, cc, :], ps[pb][:, 0:256], NORM),
                        reads=[PS(pb)], writes=[("zcs", t, cc)])
        FT = A.alloc([4, S_TOK], BF16)
        dbuf = [A.alloc([NT, 512], BF16) for _ in range(2)]
        n = 0
        for kc in range(4):
            for cs in range(2):
                b = n % 2
                n += 1
                src = dftc if cs == 0 else dfts
                S.dma("sp", lambda e, b=b, src=src, kc=kc: e.dma_start(
                    out=dbuf[b], in_=src[:, kc * 512:(kc + 1) * 512].rearrange("(nt p) k -> p nt k", p=128)),
                    writes=[("dbuf", b)], slot="dbuf%d" % b)
                for cc in range(4):
                    for nt in range(NT):
                        S.op("pe", lambda e, b=b, cc=cc, nt=nt, cs=cs: e.matmul(
                            ps[4 + cc][:, :], zcs[:, nt, cc, cs * 128:(cs + 1) * 128], dbuf[b][:, nt, :],
                            start=(cs == 0 and nt == 0), stop=(cs == 1 and nt == NT - 1)),
                            reads=[("zcs", nt, cc), ("dbuf", b)], writes=[PS(4 + cc)])
            for cc in range(4):
                eng = "act" if cc % 2 == 0 else "dve"
                if eng == "act":
                    S.op("act", lambda e, cc=cc, kc=kc: e.copy(FT[:, cc, kc * 512:(kc + 1) * 512], ps[4 + cc][:, :]),
                         reads=[PS(4 + cc)], writes=[("FT", cc, kc)])
                else:
                    S.op("dve", lambda e, cc=cc, kc=kc: e.tensor_copy(FT[:, cc, kc * 512:(kc + 1) * 512], ps[4 + cc][:, :]),
                         reads=[PS(4 + cc)], writes=[("FT", cc, kc)])
        S.dma("sp", lambda e: e.dma_start(out=FT_scr.rearrange("p (a b) -> p a b", a=4), in_=FT),
              reads=[("FT", cc, kc) for cc in range(4) for kc in range(4)], writes=["FT_scr"], slot="FT_st")
        S.barrier()
        A.release(m2)

        qT = A.alloc([8, S_TOK], BF16)
        kT = A.alloc([8, S_TOK], BF16)
        vb = A.alloc([NT, 8, 65], BF16)
        m3 = A.mark()
        win_a = A.alloc([KT, 416], BF16)
        wuq = A.alloc([2, 768], BF16)
        wukv = A.alloc([1024], BF16)
        ropet = A.alloc([NT, 32], F32)
        gqa = A.alloc([256], F32)
        gkva = A.alloc([128], F32)
        g96 = A.alloc([2, 96], F32)
        S.dma("pool", lambda e: e.dma_start(out=win_a, in_=w_in[:, 0:416].rearrange("(kt p) n -> p kt n", p=128)),
              writes=["win_a"], slot="win_a")
        S.dma("pool", lambda e: e.dma_start(out=wuq, in_=w_uq.rearrange("(kt p) n -> p kt n", p=128)),
              writes=["wuq"], slot="wuq")
        S.dma("pool", lambda e: e.dma_start(out=wukv, in_=w_ukv), writes=["wukv"], slot="wukv")
        S.dma("sp", lambda e: e.dma_start(out=ropet, in_=rope.rearrange("(t p) c -> p t c", p=128)),
              writes=["ropet"], slot="ropet")
        bc_load(gqa, q_a_norm_g[0, :], "gqa", "gqa")
        bc_load(gkva, kv_a_norm_g[0, :], "gkva", "gkva")
        bc_load(g96[:, 0, :], q_norm_g[0, :], "g96q", "g96q")
        bc_load(g96[:, 1, :], k_norm_g[0, :], "g96k", "g96k")
        S.op("dve", lambda e: e.tensor_scalar_mul(g96[:, 0, :], g96[:, 0, :], 96.0 ** -0.5), reads=["g96q"], writes=["g96q"])
        S.op("pool", lambda e: e.memset(vb, 1.0), writes=["vb_init"])
        NB2 = 2
        sqa = [A.alloc([768], F32) for _ in range(NB2)]
        st2 = A.alloc([NT, 40], F32)
        cqb = [A.alloc([384], BF16) for _ in range(NB2)]
        cT = [A.alloc([3, 128], BF16) for _ in range(NB2)]
        kr = [A.alloc([32], F32) for _ in range(NB2)]
        kraw = [A.alloc([8, 96], F32) for _ in range(NB2)]
        nbuf = [[A.alloc([8, 96], F32) for _ in range(2)] for _ in range(NB2)]
        rt = [[A.alloc([4, 8, 16], F32) for _ in range(2)] for _ in range(NB2)]
        qkb = [[A.alloc([8, 96], BF16) for _ in range(2)] for _ in range(NB2)]

        def norm_rope(t, which, src, src_keys, ss_col, dstT):
            w = which
            tb = t % NB2
            nb_ = nbuf[tb][w]
            rt_ = rt[tb][w]
            qb_ = qkb[tb][w]
            NK = ("nbuf", tb, w)
            gbc = g96[:, w:w + 1, :].to_broadcast([128, 8, 96])
            gkey = "g96q" if w == 0 else "g96k"
            S.op("act", lambda e: e.activation(st2[:, t, ss_col + 8:ss_col + 16], st2[:, t, ss_col:ss_col + 8],
                                               AF.Sqrt, bias=EPS, scale=1.0 / 96),
                 reads=[("ss", t, w)], writes=[("sd", t, w)])
            yield
            S.op("dve", lambda e: e.reciprocal(st2[:, t, ss_col:ss_col + 8], st2[:, t, ss_col + 8:ss_col + 16]),
                 reads=[("sd", t, w)], writes=[("rs", t, w)])
            yield
            S.op("dve", lambda e: e.tensor_tensor(
                nb_, src, st2[:, t, ss_col:ss_col + 8].unsqueeze(2).to_broadcast([128, 8, 96]), ALU.mult),
                reads=src_keys + [("rs", t, w)], writes=[NK])
            yield
            S.op("pool", lambda e: e.tensor_tensor(nb_, nb_, gbc, ALU.mult),
                 reads=[NK, gkey], writes=[NK])
            yield
            cosb = ropet[:, t, 0:16].unsqueeze(1).to_broadcast([128, 8, 16])
            sinb = ropet[:, t, 16:32].unsqueeze(1).to_broadcast([128, 8, 16])
            r1 = nb_[:, :, 64:80]
            r2 = nb_[:, :, 80:96]
            S.op("act", lambda e: e.copy(qb_[:, :, 0:64], nb_[:, :, 0:64]),
                 reads=[NK], writes=[("qkb", tb, w, 0)])
            S.op("dve", lambda e: e.tensor_tensor(rt_[:, 0, :, :], r1, cosb, ALU.mult),
                 reads=[NK, "ropet"], writes=[("rt", tb, w, 0)])
            S.op("pool", lambda e: e.tensor_tensor(rt_[:, 1, :, :], r2, sinb, ALU.mult),
                 reads=[NK, "ropet"], writes=[("rt", tb, w, 1)])
            yield
            S.op("dve", lambda e: e.tensor_tensor(rt_[:, 2, :, :], r2, cosb, ALU.mult),
                 reads=[NK, "ropet"], writes=[("rt", tb, w, 2)])
            S.op("pool", lambda e: e.tensor_tensor(rt_[:, 3, :, :], r1, sinb, ALU.mult),
                 reads=[NK, "ropet"], writes=[("rt", tb, w, 3)])
            yield
            S.op("dve", lambda e: e.tensor_tensor(qb_[:, :, 64:80], rt_[:, 0, :, :], rt_[:, 1, :, :], ALU.subtract),
                 reads=[("rt", tb, w, 0), ("rt", tb, w, 1)], writes=[("qkb", tb, w, 1)])
            S.op("pool", lambda e: e.tensor_tensor(qb_[:, :, 80:96], rt_[:, 2, :, :], rt_[:, 3, :, :], ALU.add),
                 reads=[("rt", tb, w, 2), ("rt", tb, w, 3)], writes=[("qkb", tb, w, 2)])
            yield
            pb = 6 + w
            for h in range(8):
                S.op("pe", lambda e, h=h: e.transpose(psb[pb][0:96, h * 128:(h + 1) * 128], qb_[:, h, :], ident),
                     reads=[("qkb", tb, w, 0), ("qkb", tb, w, 1), ("qkb", tb, w, 2), "ident"], writes=[PS(pb)])
            S.op("act", lambda e: e.copy(dstT[0:96, :, t * 128:(t + 1) * 128],
                                         psb[pb][0:96, :].rearrange("p (a b) -> p a b", a=8)),
                 reads=[PS(pb)], writes=[("qkT", w, t)])
            yield

        def prep_tile(t):
            tb = t % NB2
            sq_ = sqa[tb]
            SQ = ("sqa", tb)
            for kt in range(KT):
                S.op("pe", lambda e, kt=kt: e.matmul(
                    ps[0][:, 0:416], hT[:, kt, t * 128:(t + 1) * 128], win_a[:, kt, :],
                    start=(kt == 0), stop=(kt == KT - 1)),
                    reads=[("hT", t), "win_a"], writes=[PS(0)])
            S.op("act", lambda e: e.activation(sq_[:, 0:256], ps[0][:, 0:256], AF.Square, accum_out=st2[:, t, 0:1]),
                 reads=[PS(0)], writes=[SQ, ("s0", t)])
            S.op("act", lambda e: e.activation(sq_[:, 256:384], ps[0][:, 256:384], AF.Square, accum_out=st2[:, t, 1:2]),
                 reads=[PS(0)], writes=[SQ, ("s1", t)])
            S.op("act", lambda e: e.activation(st2[:, t, 4:5], st2[:, t, 0:1], AF.Sqrt, bias=EPS, scale=1.0 / 256),
                 reads=[("s0", t)], writes=[("s0b", t)])
            S.op("act", lambda e: e.activation(st2[:, t, 5:6], st2[:, t, 1:2], AF.Sqrt, bias=EPS, scale=1.0 / 128),
                 reads=[("s1", t)], writes=[("s1b", t)])
            S.op("dve", lambda e: e.reciprocal(st2[:, t, 6:8], st2[:, t, 4:6]),
                 reads=[("s0b", t), ("s1b", t)], writes=[("s01c", t)])
            S.op("dve", lambda e: e.scalar_tensor_tensor(
                cqb[tb][:, 0:256], ps[0][:, 0:256], st2[:, t, 6:7], gqa, ALU.mult, ALU.mult),
                reads=[PS(0), ("s01c", t), "gqa"], writes=[("cqb0", tb)])
            S.op("dve", lambda e: e.scalar_tensor_tensor(
                cqb[tb][:, 256:384], ps[0][:, 256:384], st2[:, t, 7:8], gkva, ALU.mult, ALU.mult),
                reads=[PS(0), ("s01c", t), "gkva"], writes=[("cqb1", tb)])
            S.op("act", lambda e: e.copy(kr[tb], ps[0][:, 384:416]), reads=[PS(0)], writes=[("kr", tb)])
            for j in range(3):
                S.op("pe", lambda e, j=j: e.transpose(psb[1][:, j * 128:(j + 1) * 128], cqb[tb][:, j * 128:(j + 1) * 128], ident),
                     reads=[("cqb0", tb), ("cqb1", tb), "ident"], writes=[PS(1)])
            S.op("act", lambda e: e.copy(cT[tb], psb[1][:, 0:384].rearrange("p (a b) -> p a b", a=3)),
                 reads=[PS(1)], writes=[("cT", tb)])
            for j in range(2):
                S.op("pe", lambda e, j=j: e.matmul(ps[2][:, :], cT[tb][:, j, :], wuq[:, j, 0:512], start=(j == 0), stop=(j == 1)),
                     reads=[("cT", tb), "wuq"], writes=[PS(2)])
            for j in range(2):
                S.op("pe", lambda e, j=j: e.matmul(ps[3][:, 0:256], cT[tb][:, j, :], wuq[:, j, 512:768], start=(j == 0), stop=(j == 1)),
                     reads=[("cT", tb), "wuq"], writes=[PS(3)])
            for hf in range(2):
                S.op("pe", lambda e, hf=hf: e.matmul(ps[4 + hf][:, :], cT[tb][:, 2, :], wukv[:, hf * 512:(hf + 1) * 512], start=True, stop=True),
                     reads=[("cT", tb), "wukv"], writes=[PS(4 + hf)])
            S.op("act", lambda e: e.copy(sq_[:, 0:512], ps[2][:, :]), reads=[PS(2)], writes=[SQ])
            S.op("act", lambda e: e.copy(sq_[:, 512:768], ps[3][:, 0:256]), reads=[PS(3)], writes=[SQ])
            qraw = sq_[:, 0:768].rearrange("p (a b) -> p a b", a=8)
            S.op("dve", lambda e: e.tensor_tensor(nbuf[tb][0], qraw, qraw, ALU.mult), reads=[SQ], writes=[("nbuf", tb, 0)])
            S.op("dve", lambda e: e.tensor_reduce(st2[:, t, 8:16], nbuf[tb][0], AX.X, ALU.add),
                 reads=[("nbuf", tb, 0)], writes=[("ss", t, 0)])
            for hf in range(2):
                S.op("act", lambda e, hf=hf: e.copy(
                    kraw[tb][:, hf * 4:(hf + 1) * 4, 0:64],
                    ps[4 + hf][:, :].rearrange("p (a b) -> p a b", a=4)[:, :, 0:64]),
                    reads=[PS(4 + hf)], writes=[("kraw", tb, hf)])
                S.op("dve", lambda e, hf=hf: e.tensor_copy(
                    vb[:, t, hf * 4:(hf + 1) * 4, 0:64],
                    ps[4 + hf][:, :].rearrange("p (a b) -> p a b", a=4)[:, :, 64:128]),
                    reads=[PS(4 + hf), "vb_init"], writes=[("vb", t, hf)])
            S.op("pool", lambda e: e.tensor_copy(kraw[tb][:, :, 64:96], kr[tb].unsqueeze(1).to_broadcast([128, 8, 32])),
                 reads=[("kr", tb)], writes=[("kraw", tb, 2)])
            KR = [("kraw", tb, 0), ("kraw", tb, 1), ("kraw", tb, 2)]
            S.op("dve", lambda e: e.tensor_tensor(nbuf[tb][1], kraw[tb], kraw[tb], ALU.mult),
                 reads=KR, writes=[("nbuf", tb, 1)])
            S.op("dve", lambda e: e.tensor_reduce(st2[:, t, 24:32], nbuf[tb][1], AX.X, ALU.add),
                 reads=[("nbuf", tb, 1)], writes=[("ss", t, 1)])
            gq = norm_rope(t, 0, qraw, [SQ], 8, qT)
            gk = norm_rope(t, 1, kraw[tb], KR, 24, kT)
            alive = [gq, gk]
            while alive:
                for g in list(alive):
                    try:
                        next(g)
                    except StopIteration:
                        alive.remove(g)

        for t in range(NT):
            prep_tile(t)
        S.barrier()
        A.release(m3)

        m4 = A.mark()
        A_tok = A.alloc([NT, 512], BF16)
        pT = [A.alloc([512], BF16) for _ in range(4)]
        rden = A.alloc([2, 4], F32)
        AT = A.alloc([4, S_TOK], BF16)
        QK_ALL = lambda w: [("qkT", w, t) for t in range(NT)]
        its = [(h, qc, j) for h in range(8) for qc in range(4) for j in range(NT)]
        LOOK = 2

        def emit_qk(i):
            h, qc, j = its[i]
            sbk = i % 4
            S.op("pe", lambda e: e.matmul(
                ps[sbk][:, :], kT[0:96, h, j * 128:(j + 1) * 128], qT[0:96, h, qc * 512:(qc + 1) * 512],
                start=True, stop=True),
                reads=[("qkT", 1, j)] + [("qkT", 0, qc * 4 + s_) for s_ in range(4)], writes=[PS(sbk)])
            S.op("act", lambda e: e.activation(pT[sbk], ps[sbk][:, :], AF.Exp),
                 reads=[PS(sbk)], writes=[("pT", sbk)])

        for i in range(min(LOOK, len(its))):
            emit_qk(i)
        for i, (h, qc, j) in enumerate(its):
            if i + LOOK < len(its):
                emit_qk(i + LOOK)
            sbk = i % 4
            grp = i // NT
            ob = 4 + (grp % 2)
            for sub in range(4):
                S.op("pe", lambda e, h=h, j=j, sbk=sbk, sub=sub, ob=ob: e.matmul(
                    ps[ob][:, sub * 128:sub * 128 + 65], pT[sbk][:, sub * 128:(sub + 1) * 128], vb[:, j, h, :],
                    start=(j == 0), stop=(j == NT - 1)),
                    reads=[("pT", sbk), ("vb", j, h // 4), "vb_init"], writes=[PS(ob)])
            if j == NT - 1:
                o4 = ps[ob][:, :].rearrange("p (a b) -> p a b", a=4)
                rb = grp % 2
                S.op("dve", lambda e, o4=o4, rb=rb: e.reciprocal(rden[:, rb, :].unsqueeze(2), o4[:, :, 64:65]),
                     reads=[PS(ob)], writes=[("rden", rb)])
                S.op("dve", lambda e, o4=o4, rb=rb, h=h, qc=qc: e.tensor_tensor(
                    A_tok[:, qc * 4:(qc + 1) * 4, h * 64:(h + 1) * 64], o4[:, :, 0:64],
                    rden[:, rb, :].unsqueeze(2).to_broadcast([128, 4, 64]), ALU.mult),
                    reads=[PS(ob), ("rden", rb)], writes=[("A_tok", qc, h)])
        for t in range(NT):
            pb = 6 + (t % 2)
            for j in range(4):
                S.op("pe", lambda e, t=t, j=j, pb=pb: e.transpose(
                    psb[pb][:, j * 128:(j + 1) * 128], A_tok[:, t, j * 128:(j + 1) * 128], ident),
                    reads=[("A_tok", t // 4, h) for h in range(8)] + ["ident"], writes=[PS(pb)])
            S.op("act", lambda e, t=t, pb=pb: e.copy(
                AT[:, :, t * 128:(t + 1) * 128], psb[pb][:, 0:512].rearrange("p (a b) -> p a b", a=4)),
                reads=[PS(pb)], writes=[("AT", t)])
        S.dma("sp", lambda e: e.dma_start(out=AT_scr.rearrange("p (a b) -> p a b", a=4), in_=AT),
              reads=[("AT", t) for t in range(NT)], writes=["AT_scr"], slot="AT_st")
        S.barrier()
        A.release(m0)

        m5 = A.mark()
        win_g = A.alloc([KT, 2048], BF16)
        wpa = A.alloc([4, D], BF16)
        wpf = A.alloc([4, D], BF16)
        wout = A.alloc([KT, D], BF16)
        hTc = [A.alloc([KT, 512], BF16) for _ in range(2)]
        ATc = [A.alloc([4, 512], BF16) for _ in range(2)]
        FTc = [A.alloc([4, 512], BF16) for _ in range(2)]
        mTc = [A.alloc([KT, 512], BF16) for _ in range(2)]
        sa = [A.alloc([512], F32) for _ in range(2)]
        sf = [A.alloc([512], F32) for _ in range(2)]
        mm1 = [A.alloc([512], F32) for _ in range(2)]
        mm2 = [A.alloc([512], F32) for _ in range(2)]
        xb5 = [A.alloc([D], F32) for _ in range(4)]
        t5 = [A.alloc([D], F32) for _ in range(2)]
        x1b = [A.alloc([D], F32) for _ in range(2)]
        for half in range(2):
            S.dma("pool", lambda e, half=half: e.dma_start(
                out=win_g[:, :, half * 1024:(half + 1) * 1024],
                in_=w_in[:, 928 + half * 1024:928 + (half + 1) * 1024].rearrange("(kt p) n -> p kt n", p=128)),
                writes=[("win_g", half)], slot="win_g%d" % half)
        S.dma("pool", lambda e: e.dma_start(out=wpa, in_=w_pa.rearrange("(kt p) n -> p kt n", p=128)), writes=["wpa"], slot="wpa")
        S.dma("pool", lambda e: e.dma_start(out=wpf, in_=w_pf.rearrange("(kt p) n -> p kt n", p=128)), writes=["wpf"], slot="wpf")
        S.dma("pool", lambda e: e.dma_start(out=wout, in_=w_out.rearrange("(kt p) n -> p kt n", p=128)), writes=["wout"], slot="wout")
        nfc = 0

        def load_chunk(qc):
            cb = qc % 2
            S.dma("sp", lambda e: e.dma_start(
                out=hTc[cb], in_=hT_scr.rearrange("p (a b) -> p a b", a=KT)[:, :, qc * 512:(qc + 1) * 512]),
                reads=["hT_scr"], writes=[("hTc", cb)], slot="hTc%d" % cb)
            S.dma("sp", lambda e: e.dma_start(
                out=ATc[cb], in_=AT_scr.rearrange("p (a b) -> p a b", a=4)[:, :, qc * 512:(qc + 1) * 512]),
                reads=["AT_scr"], writes=[("ATc", cb)], slot="ATc%d" % cb)
            S.dma("sp", lambda e: e.dma_start(
                out=FTc[cb], in_=FT_scr.rearrange("p (a b) -> p a b", a=4)[:, :, qc * 512:(qc + 1) * 512]),
                reads=["FT_scr"], writes=[("FTc", cb)], slot="FTc%d" % cb)

        load_chunk(0)
        for qc in range(4):
            cb = qc % 2
            if qc + 1 < 4:
                load_chunk(qc + 1)
            for sub in range(4):
                t_ = qc * 4 + sub
                S.dma("sp", lambda e, t_=t_, sub=sub: e.dma_start(out=xb5[sub], in_=x[t_ * 128:(t_ + 1) * 128, :]),
                      writes=[("xb5", sub)], slot="xb5%d" % sub)
            for fc in range(8):
                pbase = 4 * (nfc % 2)
                eb = nfc % 2
                nfc += 1
                for kt in range(KT):
                    S.op("pe", lambda e, cb=cb, fc=fc, kt=kt, pbase=pbase: e.matmul(
                        ps[pbase][:, :], win_g[:, kt, fc * 128:(fc + 1) * 128], hTc[cb][:, kt, :],
                        start=(kt == 0), stop=(kt == KT - 1)),
                        reads=[("win_g", 0), ("hTc", cb)], writes=[PS(pbase)])
                for kt in range(KT):
                    S.op("pe", lambda e, cb=cb, fc=fc, kt=kt, pbase=pbase: e.matmul(
                        ps[pbase + 1][:, :], win_g[:, kt, 1024 + fc * 128:1024 + (fc + 1) * 128], hTc[cb][:, kt, :],
                        start=(kt == 0), stop=(kt == KT - 1)),
                        reads=[("win_g", 1), ("hTc", cb)], writes=[PS(pbase + 1)])
                for j in range(4):
                    S.op("pe", lambda e, cb=cb, fc=fc, j=j, pbase=pbase: e.matmul(
                        ps[pbase + 2][:, :], wpa[:, j, fc * 128:(fc + 1) * 128], ATc[cb][:, j, :],
                        start=(j == 0), stop=(j == 3)),
                        reads=["wpa", ("ATc", cb)], writes=[PS(pbase + 2)])
                for j in range(4):
                    S.op("pe", lambda e, cb=cb, fc=fc, j=j, pbase=pbase: e.matmul(
                        ps[pbase + 3][:, :], wpf[:, j, fc * 128:(fc + 1) * 128], FTc[cb][:, j, :],
                        start=(j == 0), stop=(j == 3)),
                        reads=["wpf", ("FTc", cb)], writes=[PS(pbase + 3)])
                S.op("act", lambda e, eb=eb, pbase=pbase: e.activation(sa[eb], ps[pbase][:, :], AF.Sigmoid),
                     reads=[PS(pbase)], writes=[("sa", eb)])
                S.op("act", lambda e, eb=eb, pbase=pbase: e.activation(sf[eb], ps[pbase + 1][:, :], AF.Sigmoid),
                     reads=[PS(pbase + 1)], writes=[("sf", eb)])
                S.op("dve", lambda e, eb=eb, pbase=pbase: e.tensor_tensor(mm1[eb], ps[pbase + 2][:, :], sa[eb], ALU.mult),
                     reads=[PS(pbase + 2), ("sa", eb)], writes=[("mm1", eb)])
                S.op("dve", lambda e, eb=eb, pbase=pbase: e.tensor_tensor(mm2[eb], ps[pbase + 3][:, :], sf[eb], ALU.mult),
                     reads=[PS(pbase + 3), ("sf", eb)], writes=[("mm2", eb)])
                S.op("pool", lambda e, eb=eb, cb=cb, fc=fc: e.tensor_tensor(mTc[cb][:, fc, :], mm1[eb], mm2[eb], ALU.add),
                     reads=[("mm1", eb), ("mm2", eb)], writes=[("mTc", cb, fc)])
            for sub in range(4):
                t = qc * 4 + sub
                xbi = t % 2
                p0 = 2 * sub
                for hf in range(2):
                    for fc in range(8):
                        S.op("pe", lambda e, cb=cb, sub=sub, hf=hf, fc=fc, p0=p0: e.matmul(
                            ps[p0 + hf][:, :], mTc[cb][:, fc, sub * 128:(sub + 1) * 128], wout[:, fc, hf * 512:(hf + 1) * 512],
                            start=(fc == 0), stop=(fc == 7)),
                            reads=[("mTc", cb, fc), "wout"], writes=[PS(p0 + hf)])
                    S.op("dve", lambda e, xbi=xbi, hf=hf, p0=p0: e.tensor_tensor(
                        t5[xbi][:, hf * 512:(hf + 1) * 512], ps[p0 + hf][:, :], ada[:, 2, hf * 512:(hf + 1) * 512], ALU.mult),
                        reads=[PS(p0 + hf)] + ADA(2), writes=[("t5", xbi, hf)])
                S.op("pool", lambda e, xbi=xbi, sub=sub: e.tensor_tensor(x1b[xbi], t5[xbi], xb5[sub], ALU.add),
                     reads=[("t5", xbi, 0), ("t5", xbi, 1), ("xb5", sub)], writes=[("x1b", xbi)])
                dst = out if stage == "x1" else x1_scr
                S.dma("sp", lambda e, t=t, xbi=xbi, dst=dst: e.dma_start(out=dst[t * 128:(t + 1) * 128, :], in_=x1b[xbi]),
                      reads=[("x1b", xbi)], writes=[("x1_scr", t)], slot="x1st%d" % xbi)
        S.barrier()
        A.release(m5)
        if stage == "x1":
            st = S.emit()
            print("sched", st, "arena peak KiB", A.peak / 512.0)
            return nc

        gwk = A.alloc([NT, 8], F32)
        idx = A.alloc([NT, 8], I32)
        widx = A.alloc([R_OVF], I32)
        m6 = A.mark()
        Gw = A.alloc([NT, NE], F32)
        Pos = A.alloc([NT, NE], F32)
        run_bc = A.alloc([NE], F32)
        iota1 = A.alloc([NE], F32)
        m7 = A.mark()
        wr_f = A.alloc([KT, NE], F32)
        wr_hi = A.alloc([KT, NE], BF16)
        wr_lo = A.alloc([KT, NE], BF16)
        wsh = A.alloc([KT, 512], BF16)
        wshd = A.alloc([2, D], BF16)
        rbias = A.alloc([NE], F32)
        tmp2 = [A.alloc([D], F32) for _ in range(2)]
        h2hi = [A.alloc([D], BF16) for _ in range(2)]
        h2lo = [A.alloc([D], BF16) for _ in range(2)]
        h2T = [A.alloc([KT, 128], BF16) for _ in range(2)]
        h2loT = [A.alloc([KT, 128], BF16) for _ in range(2)]
        sq5_ = [A.alloc([D], F32)]
        st5 = A.alloc([NT, 8], F32)
        scb_ = [A.alloc([NE], F32) for _ in range(2)]
        biased_ = [A.alloc([NE], F32) for _ in range(2)]
        wsel_ = [A.alloc([NE], F32) for _ in range(2)]
        maskb_ = [A.alloc([NE], BF16) for _ in range(2)]
        top8 = A.alloc([NT, 8], F32)
        sg_ = [A.alloc([256], F32) for _ in range(2)]
        hsh_ = [A.alloc([256], BF16) for _ in range(2)]
        hshT_ = [A.alloc([2, 128], BF16) for _ in range(2)]
        t6 = [A.alloc([D], F32) for _ in range(2)]
        S.dma("sp", lambda e: e.dma_start(out=wr_f, in_=w_router.rearrange("(kt p) n -> p kt n", p=128)), writes=["wr_f"], slot="wr_f")
        S.op("act", lambda e: e.copy(wr_hi, wr_f), reads=["wr_f"], writes=["wr_hi"])
        S.op("dve", lambda e: e.tensor_tensor(wr_lo, wr_f, wr_hi, ALU.subtract), reads=["wr_f", "wr_hi"], writes=["wr_lo"])
        S.dma("pool", lambda e: e.dma_start(out=wsh[:, :, 0:256], in_=w_sg.rearrange("(kt p) n -> p kt n", p=128)), writes=["wsh0"], slot="wsh0")
        S.dma("pool", lambda e: e.dma_start(out=wsh[:, :, 256:512], in_=w_su.rearrange("(kt p) n -> p kt n", p=128)), writes=["wsh1"], slot="wsh1")
        S.dma("pool", lambda e: e.dma_start(out=wshd, in_=w_sd.rearrange("(kt p) n -> p kt n", p=128)), writes=["wshd"], slot="wshd")
        bc_load(rbias, router_bias[0, :], "rbias", "rbias")
        S.dma("sp", lambda e: e.dma_start(out=iota1, in_=iota1_d), writes=["iota1"], slot="iota1")
        S.op("dve", lambda e: e.memset(run_bc, 0.0), writes=["run_bc"])
        x1all = A.alloc([NT, D], F32)
        sgs_ = [A.alloc([256], F32) for _ in range(2)]
        for t in range(NT):
            S.dma("sp", lambda e, t=t: e.dma_start(out=x1all[:, t, :], in_=x1_scr[t * 128:(t + 1) * 128, :]),
                  writes=[("x1t", t)], slot="x1t%d" % (t % 4))
        for t in range(NT):
            S.op("act", lambda e, t=t: e.activation(sq5_[0], x1all[:, t, :], AF.Square, accum_out=st5[:, t, 0:1]),
                 reads=[("x1t", t)], writes=["sq5", ("st5", t)])
        ST5 = [("st5", t) for t in range(NT)]
        S.op("act", lambda e: e.activation(st5[:, :, 1], st5[:, :, 0], AF.Sqrt, bias=EPS, scale=1.0 / D),
             reads=ST5, writes=["st5b"])
        S.op("dve", lambda e: e.reciprocal(st5[:, :, 2], st5[:, :, 1]), reads=["st5b"], writes=["st5c"])

        def stage5A(t):
            b = t % 2
            S.op("dve", lambda e: e.scalar_tensor_tensor(
                tmp2[b], x1all[:, t, :], st5[:, t, 2:3], ada[:, 4, :], ALU.mult, ALU.mult),
                reads=[("x1t", t), "st5c"] + ADA(4), writes=[("tmp2", b)])
            S.op("pool", lambda e: e.tensor_tensor(tmp2[b], tmp2[b], ada[:, 3, :], ALU.add),
                 reads=[("tmp2", b)] + ADA(3), writes=[("tmp2", b)])
            S.op("act", lambda e: e.copy(h2hi[b], tmp2[b]), reads=[("tmp2", b)], writes=[("h2hi", b)])
            S.op("dve", lambda e: e.tensor_tensor(h2lo[b], tmp2[b], h2hi[b], ALU.subtract),
                 reads=[("tmp2", b), ("h2hi", b)], writes=[("h2lo", b)])
            S.dma("sp", lambda e: e.dma_start(out=h2_scr[t * 128:(t + 1) * 128, :], in_=h2hi[b]),
                  reads=[("h2hi", b)], writes=[("h2_scr", t)], slot="h2st%d" % b)
            for kt in range(KT):
                S.op("pe", lambda e, kt=kt: e.transpose(psb[0][:, kt * 128:(kt + 1) * 128], h2hi[b][:, kt * 128:(kt + 1) * 128], ident),
                     reads=[("h2hi", b), "ident"], writes=[PS(0)])
            S.op("act", lambda e: e.copy(h2T[b], psb[0][:, :].rearrange("p (a b) -> p a b", a=8)),
                 reads=[PS(0)], writes=[("h2T", b)])
            for kt in range(KT):
                S.op("pe", lambda e, kt=kt: e.transpose(psb[1][:, kt * 128:(kt + 1) * 128], h2lo[b][:, kt * 128:(kt + 1) * 128], ident),
                     reads=[("h2lo", b), "ident"], writes=[PS(1)])
            S.op("dve", lambda e: e.tensor_copy(h2loT[b], psb[1][:, :].rearrange("p (a b) -> p a b", a=8)),
                 reads=[PS(1)], writes=[("h2loT", b)])

        def stage5B(t):
            b = t % 2
            scb = scb_[b]; biased = biased_[b]; wsel = wsel_[b]; maskb = maskb_[b]
            sg = sg_[b]; sgs = sgs_[b]; hsh = hsh_[b]; hshT = hshT_[b]
            nmm = 0
            for (xi, wi) in [(0, 0), (0, 1), (1, 0)]:
                for kt in range(KT):
                    lt = h2T[b] if xi == 0 else h2loT[b]
                    wt = wr_hi if wi == 0 else wr_lo
                    S.op("pe", lambda e, lt=lt, wt=wt, kt=kt, nmm=nmm: e.matmul(
                        ps[2][:, 0:NE], lt[:, kt, :], wt[:, kt, :], start=(nmm == 0), stop=(nmm == 23)),
                        reads=[("h2T", b), ("h2loT", b), "wr_hi", "wr_lo"], writes=[PS(2)])
                    nmm += 1
            for kt in range(KT):
                S.op("pe", lambda e, kt=kt: e.matmul(
                    ps[3][:, :], h2T[b][:, kt, :], wsh[:, kt, :], start=(kt == 0), stop=(kt == KT - 1)),
                    reads=[("h2T", b), "wsh0", "wsh1"], writes=[PS(3)])
            S.op("act", lambda e: e.activation(scb, ps[2][:, 0:NE], AF.Sigmoid), reads=[PS(2)], writes=[("scb", b)])
            S.op("act", lambda e: e.activation(sgs, ps[3][:, 0:256], AF.Sigmoid), reads=[PS(3)], writes=[("sgs", b)])
            S.op("dve", lambda e: e.tensor_tensor(biased, scb, rbias, ALU.add), reads=[("scb", b), "rbias"], writes=[("biased", b)])
            S.op("dve", lambda e: e.max(top8[:, t, :], biased), reads=[("biased", b)], writes=[("top8", t)])
            S.op("dve", lambda e: e.scalar_tensor_tensor(
                wsel, biased, top8[:, t, 7:8], scb, ALU.is_ge, ALU.mult, accum_out=st5[:, t, 3:4]),
                reads=[("biased", b), ("top8", t), ("scb", b)], writes=[("wsel", b), ("den", t)])
            S.op("dve", lambda e: e.tensor_scalar(maskb, biased, top8[:, t, 7:8], None, ALU.is_ge),
                 reads=[("biased", b), ("top8", t)], writes=[("maskb", b)])
            S.op("dve", lambda e: e.reciprocal(st5[:, t, 4:5], st5[:, t, 3:4]), reads=[("den", t)], writes=[("rden5", t)])
            S.op("dve", lambda e: e.tensor_scalar(Gw[:, t, :], wsel, st5[:, t, 4:5], 2.5, ALU.mult, ALU.mult),
                 reads=[("wsel", b), ("rden5", t)], writes=[("Gw", t)])
            S.op("pe", lambda e: e.matmul(ps[4][:, 0:NE], tri, maskb, start=True, stop=True),
                 reads=["tri", ("maskb", b)], writes=[PS(4)])
            S.op("pe", lambda e: e.matmul(ps[4][:, NE:2 * NE], ones, maskb, start=True, stop=True),
                 reads=["ones", ("maskb", b)], writes=[PS(4)])
            S.op("dve", lambda e: e.tensor_tensor(Pos[:, t, :], ps[4][:, 0:NE], run_bc, ALU.add),
                 reads=[PS(4), "run_bc"], writes=[("Pos", t)])
            S.op("dve", lambda e: e.tensor_tensor(run_bc, ps[4][:, NE:2 * NE], run_bc, ALU.add),
                 reads=[PS(4), "run_bc"], writes=["run_bc"])
            S.op("dve", lambda e: e.tensor_tensor(sg, ps[3][:, 0:256], sgs, ALU.mult), reads=[PS(3), ("sgs", b)], writes=[("sg", b)])
            S.op("dve", lambda e: e.tensor_tensor(hsh, ps[3][:, 256:512], sg, ALU.mult), reads=[PS(3), ("sg", b)], writes=[("hsh", b)])
            for j in range(2):
                S.op("pe", lambda e, j=j: e.transpose(psb[5][:, j * 128:(j + 1) * 128], hsh[:, j * 128:(j + 1) * 128], ident),
                     reads=[("hsh", b), "ident"], writes=[PS(5)])
            S.op("act", lambda e: e.copy(hshT, psb[5][:, 0:256].rearrange("p (a b) -> p a b", a=2)), reads=[PS(5)], writes=[("hshT", b)])
            for hf in range(2):
                for j in range(2):
                    S.op("pe", lambda e, hf=hf, j=j: e.matmul(
                        ps[6 + hf][:, :], hshT[:, j, :], wshd[:, j, hf * 512:(hf + 1) * 512], start=(j == 0), stop=(j == 1)),
                        reads=[("hshT", b), "wshd"], writes=[PS(6 + hf)])
                S.op("dve", lambda e, hf=hf: e.tensor_tensor(
                    t6[b][:, hf * 512:(hf + 1) * 512], ps[6 + hf][:, :], ada[:, 5, hf * 512:(hf + 1) * 512], ALU.mult),
                    reads=[PS(6 + hf)] + ADA(5), writes=[("t6", b, hf)])
            S.op("pool", lambda e: e.tensor_tensor(t6[b], t6[b], x1all[:, t, :], ALU.add),
                 reads=[("t6", b, 0), ("t6", b, 1), ("x1t", t)], writes=[("t6", b, 0), ("t6", b, 1)])
            S.dma("sp", lambda e: e.dma_start(out=base_scr[t * 128:(t + 1) * 128, :], in_=t6[b]),
                  reads=[("t6", b, 0), ("t6", b, 1)], writes=[("base_scr", t)], slot="bst%d" % b)

        stage5A(0)
        for t in range(NT):
            if t + 1 < NT:
                stage5A(t + 1)
            stage5B(t)
        S.barrier()
        A.release(m7)

        a1_ = [A.alloc([NE], F32) for _ in range(2)]
        a2_ = [A.alloc([NE], F32) for _ in range(2)]
        maddr_ = [A.alloc([NE], F32) for _ in range(2)]
        junk6 = [A.alloc([NE], F32) for _ in range(2)]
        top8a = A.alloc([NT, 8], F32)
        h2r = [A.alloc([D], BF16) for _ in range(2)]
        nex = A.alloc([NE], F32)
        pfx = [A.alloc([NE], F32) for _ in range(2)]
        base2 = A.alloc([NE], F32)
        d12 = A.alloc([NE], F32)
        bexp = A.alloc([R_OVF], F32)
        iokp = A.alloc([8], F32)
        wif = A.alloc([R_OVF], F32)
        onesf = A.alloc([NE], F32)
        S.op("pool", lambda e: e.memset(onesf, 1.0), writes=["onesf"])
        S.dma("sp", lambda e: e.dma_start(out=iokp, in_=iokp_d), writes=["iokp"], slot="iokp")
        S.op("dve", lambda e: e.tensor_scalar(nex, run_bc, float(C1), None, ALU.is_gt), reads=["run_bc"], writes=["nex"])
        for j in range(1, -(-(S_TOK - C1) // 128)):
            S.op("dve", lambda e, j=j: e.scalar_tensor_tensor(nex, run_bc, float(C1 + 128 * j), nex, ALU.is_gt, ALU.add),
                 reads=["run_bc", "nex"], writes=["nex"])
        S.op("dve", lambda e: e.tensor_copy(pfx[0], nex), reads=["nex"], writes=[("pfx", 0)])
        cur = 0
        sh = 1
        while sh < NE:
            nxt = 1 - cur
            S.op("dve", lambda e, cur=cur, nxt=nxt, sh=sh: e.tensor_tensor(pfx[nxt][:, sh:NE], pfx[cur][:, sh:NE], pfx[cur][:, 0:NE - sh], ALU.add),
                 reads=[("pfx", cur)], writes=[("pfx", nxt)])
            S.op("dve", lambda e, cur=cur, nxt=nxt, sh=sh: e.tensor_copy(pfx[nxt][:, 0:sh], pfx[cur][:, 0:sh]),
                 reads=[("pfx", cur)], writes=[("pfx", nxt)])
            cur = nxt
            sh *= 2
        obend = pfx[cur]
        OBK = ("pfx", cur)
        S.op("dve", lambda e: e.tensor_tensor(base2, obend, nex, ALU.subtract), reads=[OBK, "nex"], writes=["base2"])
        S.op("dve", lambda e: e.tensor_scalar(base2, base2, 128.0, float(NROWS1 - C1), ALU.mult, ALU.add), reads=["base2"], writes=["base2"])
        S.op("dve", lambda e: e.tensor_tensor(d12, iota1, base2, ALU.subtract), reads=["iota1", "base2"], writes=["d12"])
        for bq in range(R_OVF):
            S.op("dve", lambda e, bq=bq: e.scalar_tensor_tensor(
                junk6[bq % 2], obend, float(bq), onesf, ALU.is_le, ALU.mult, accum_out=bexp[:, bq:bq + 1]),
                reads=[OBK, "onesf"], writes=[("junk6", bq % 2), ("bexp", bq)])
        BEXP = [("bexp", bq) for bq in range(R_OVF)]
        S.op("dve", lambda e: e.tensor_scalar_min(bexp, bexp, float(NE - 1)), reads=BEXP, writes=["bexpc"])
        S.op("dve", lambda e: e.scalar_tensor_tensor(
            wif, bexp, 128.0, iokp[:, 0:1].to_broadcast([128, R_OVF]), ALU.mult, ALU.add),
            reads=["bexpc", "iokp"], writes=["wif"])
        S.op("dve", lambda e: e.tensor_copy(widx, wif), reads=["wif"], writes=["widx"])
        for t in range(NT):
            b = t % 2
            a1 = a1_[b]; a2 = a2_[b]; maddr = maddr_[b]
            S.op("dve", lambda e, t=t, a1=a1: e.scalar_tensor_tensor(a1, Pos[:, t, :], float(C1), d12, ALU.is_lt, ALU.mult),
                 reads=[("Pos", t), "d12"], writes=[("a1", b)])
            S.op("dve", lambda e, t=t, a2=a2: e.tensor_tensor(a2, Pos[:, t, :], base2, ALU.add),
                 reads=[("Pos", t), "base2"], writes=[("a2", b)])
            S.op("dve", lambda e, a1=a1, a2=a2: e.tensor_tensor(a1, a1, a2, ALU.add), reads=[("a1", b), ("a2", b)], writes=[("a1", b)])
            S.op("dve", lambda e, t=t, a1=a1: e.scalar_tensor_tensor(Gw[:, t, :], a1, float(NROWS), Gw[:, t, :], ALU.is_lt, ALU.mult),
                 reads=[("Gw", t), ("a1", b)], writes=[("Gw", t)])
            S.op("dve", lambda e, t=t, a1=a1, maddr=maddr: e.scalar_tensor_tensor(maddr, Gw[:, t, :], 0.0, a1, ALU.is_gt, ALU.mult),
                 reads=[("Gw", t), ("a1", b)], writes=[("maddr", b)])
            S.op("dve", lambda e, t=t, maddr=maddr: e.max(top8a[:, t, :], maddr), reads=[("maddr", b)], writes=[("top8a", t)])
            S.op("dve", lambda e, t=t: e.tensor_copy(idx[:, t, :], top8a[:, t, :]), reads=[("top8a", t)], writes=[("idx", t)])
            for k in range(8):
                jb = k % 2
                S.op("dve", lambda e, t=t, k=k, jb=jb, maddr=maddr: e.scalar_tensor_tensor(
                    junk6[jb], maddr, top8a[:, t, k:k + 1], Gw[:, t, :], ALU.is_equal, ALU.mult, accum_out=gwk[:, t, k:k + 1]),
                    reads=[("maddr", b), ("top8a", t), ("Gw", t)], writes=[("junk6", jb), ("gwk", t, k)])
            S.dma("sp", lambda e, t=t, b=b: e.dma_start(out=h2r[b], in_=h2_scr[t * 128:(t + 1) * 128, :]),
                  writes=[("h2r", b)], slot="h2r%d" % b)
            for k in range(8):
                S.dma("pool", lambda e, t=t, b=b, k=k: e.indirect_dma_start(
                    out=xdisp[:, :], out_offset=bass.IndirectOffsetOnAxis(ap=idx[:, t, k:k + 1], axis=0),
                    in_=h2r[b], in_offset=None),
                    reads=[("h2r", b), ("idx", t)], writes=[("xdisp", t, k)], slot="sc%d_%d" % (b, k))
        S.barrier()
        A.release(m6)

        m8 = A.mark()
        NXB = 3
        xs = [A.alloc([2, D], BF16) for _ in range(NXB)]
        XT = [A.alloc([KT, 256], BF16) for _ in range(2)]
        NWB = 3
        wg = [A.alloc([KT, 256], BF16) for _ in range(NWB)]
        wu = [A.alloc([KT, 256], BF16) for _ in range(NWB)]
        wd = [A.alloc([2, D], BF16) for _ in range(NWB)]
        sgb = [A.alloc([2, 256], F32) for _ in range(2)]
        HT = [A.alloc([2, 256], BF16) for _ in range(2)]
        ysb = [A.alloc([2, D], F32) for _ in range(2)]
        w32g = [A.alloc([KT, 256], F32) for _ in range(2)]
        w32u = [A.alloc([KT, 256], F32) for _ in range(2)]
        w32d = [A.alloc([2, D], F32) for _ in range(2)]
        weg_rows = w_eg.rearrange("e (p kt) f -> (e p) (kt f)", kt=KT)
        weu_rows = w_eu.rearrange("e (p kt) f -> (e p) (kt f)", kt=KT)
        wed_rows = w_ed.rearrange("e (p j) d -> (e p) (j d)", j=2)
        S.op("dve", lambda e: e.memset(ysb[0], 0.0), writes=[("ysb", 0, 0, 0), ("ysb", 0, 0, 1), ("ysb", 0, 1, 0), ("ysb", 0, 1, 1)])
        S.dma("sp", lambda e: e.dma_start(out=yscr[0:ROW0, :], in_=ysb[0][:, 0, :]),
              reads=[("ysb", 0, 0, 0), ("ysb", 0, 0, 1)], writes=["yscr_trash"], slot="yst0_0")
        for xb_ in range(NXB):
            S.op("pool", lambda e, xb_=xb_: e.memset(xs[xb_], 0.0), writes=[("xs", xb_)])
        units = [("s", e_, ROW0 + e_ * C1, 2) for e_ in range(NE)] + [("d", bq, NROWS1 + bq * 128, 1) for bq in range(R_OVF)]

        def load_x(u):
            kind, ui, r0_, nblk = units[u]
            xb_ = u % NXB
            S.dma("sp", lambda e: e.dma_start(out=xs[xb_][:, 0, :], in_=xdisp[r0_:r0_ + 128, :]),
                  writes=[("xs", xb_, 0)], reads=[("xs", xb_)], slot="xs%d_0" % xb_)
            if nblk == 2:
                S.dma("sp", lambda e: e.dma_start(out=xs[xb_][0:C1 - 128, 1, :], in_=xdisp[r0_ + 128:r0_ + C1, :]),
                      writes=[("xs", xb_, 1)], reads=[("xs", xb_)], slot="xs%d_1" % xb_)

        for u in range(NXB - 1):
            load_x(u)
        for u, (kind, ui, r0, nblk) in enumerate(units):
            b = u % 2
            xbi = u % NXB
            wb = u % NWB
            ns = nblk * 128
            if u + NXB - 1 < len(units):
                load_x(u + NXB - 1)
            if kind == "s":
                S.dma("pool", lambda e, wb=wb, ui=ui: e.dma_start(out=wg[wb], in_=w_eg[ui].rearrange("(kt p) f -> p kt f", p=128)),
                      writes=[("wg", wb)], slot="wg%d" % wb)
                S.dma("pool", lambda e, wb=wb, ui=ui: e.dma_start(out=wu[wb], in_=w_eu[ui].rearrange("(kt p) f -> p kt f", p=128)),
                      writes=[("wu", wb)], slot="wu%d" % wb)
                S.dma("pool", lambda e, wb=wb, ui=ui: e.dma_start(out=wd[wb], in_=w_ed[ui].rearrange("(j p) d -> p j d", p=128)),
                      writes=[("wd", wb)], slot="wd%d" % wb)
            else:
                db = ui % 2
                for (dst, rows_ap, nm) in ((w32g[db], weg_rows, "g"), (w32u[db], weu_rows, "u"), (w32d[db], wed_rows, "d")):
                    S.dma("pool", lambda e, dst=dst, rows_ap=rows_ap, ui=ui: e.indirect_dma_start(
                        out=dst.rearrange("p a b -> p (a b)"), out_offset=None, in_=rows_ap[:, :],
                        in_offset=bass.IndirectOffsetOnAxis(ap=widx[:, ui:ui + 1], axis=0)),
                        reads=["widx"], writes=[("w32" + nm, db)], slot="dyn_" + nm)
                S.op("act", lambda e, db=db, wb=wb: e.copy(wg[wb], w32g[db]),
                     reads=[("w32g", db)], writes=[("wg", wb)])
                S.op("dve", lambda e, db=db, wb=wb: e.tensor_copy(wu[wb], w32u[db]),
                     reads=[("w32u", db)], writes=[("wu", wb)])
                S.op("act", lambda e, db=db, wb=wb: e.copy(wd[wb], w32d[db]),
                     reads=[("w32d", db)], writes=[("wd", wb)])
            for blk in range(nblk):
                for kt in range(KT):
                    xin_ = xs[xbi][:, blk, kt:D:KT] if kind == "d" else xs[xbi][:, blk, kt * 128:(kt + 1) * 128]
                    S.op("pe", lambda e, blk=blk, kt=kt, xin_=xin_: e.transpose(
                        psb[blk][:, kt * 128:(kt + 1) * 128], xin_, ident),
                        reads=[("xs", xbi, blk), "ident"], writes=[PS(blk)])
                if blk == 0:
                    S.op("act", lambda e, b=b: e.copy(XT[b][:, :, 0:128], psb[0][:, :].rearrange("p (a b) -> p a b", a=8)),
                         reads=[PS(0)], writes=[("XT", b, 0)])
                else:
                    S.op("dve", lambda e, b=b: e.tensor_copy(XT[b][:, :, 128:256], psb[1][:, :].rearrange("p (a b) -> p a b", a=8)),
                         reads=[PS(1)], writes=[("XT", b, 1)])
            XTK = [("XT", b, blk) for blk in range(nblk)]
            for fo in range(2):
                for (wt, wk, c0) in ((wg[wb], ("wg", wb), 0), (wu[wb], ("wu", wb), 256)):
                    for kt in range(KT):
                        wcol = wt[:, kt, fo:256:2] if kind == "d" else wt[:, kt, fo * 128:(fo + 1) * 128]
                        S.op("pe", lambda e, b=b, fo=fo, wcol=wcol, c0=c0, kt=kt, ns=ns: e.matmul(
                            ps[2 + fo][:, c0:c0 + ns], wcol, XT[b][:, kt, 0:ns],
                            start=(kt == 0), stop=(kt == KT - 1)),
                            reads=[wk] + XTK, writes=[PS(2 + fo)])
                S.op("act", lambda e, b=b, fo=fo, ns=ns: e.activation(sgb[b][:, fo, 0:ns], ps[2 + fo][:, 0:ns], AF.Silu),
                     reads=[PS(2 + fo)], writes=[("sgb", b, fo)])
                S.op("dve", lambda e, b=b, fo=fo, ns=ns: e.tensor_tensor(HT[b][:, fo, 0:ns], ps[2 + fo][:, 256:256 + ns], sgb[b][:, fo, 0:ns], ALU.mult),
                     reads=[PS(2 + fo), ("sgb", b, fo)], writes=[("HT", b, fo)])
            for blk in range(nblk):
                for hf in range(2):
                    pi = 4 + 2 * blk + hf
                    for fo in range(2):
                        S.op("pe", lambda e, b=b, wb=wb, blk=blk, hf=hf, fo=fo, pi=pi: e.matmul(
                            ps[pi][:, :], HT[b][:, fo, blk * 128:(blk + 1) * 128], wd[wb][:, fo, hf * 512:(hf + 1) * 512],
                            start=(fo == 0), stop=(fo == 1)),
                            reads=[("HT", b, 0), ("HT", b, 1), ("wd", wb)], writes=[PS(pi)])
                    if hf == 0:
                        S.op("act", lambda e, b=b, blk=blk, pi=pi: e.copy(ysb[b][:, blk, 0:512], ps[pi][:, :]),
                             reads=[PS(pi)], writes=[("ysb", b, blk, 0)])
                    else:
                        S.op("dve", lambda e, b=b, blk=blk, pi=pi: e.tensor_copy(ysb[b][:, blk, 512:1024], ps[pi][:, :]),
                             reads=[PS(pi)], writes=[("ysb", b, blk, 1)])
            for blk in range(nblk):
                nr = 128 if (blk == 0 or kind == "d") else C1 - 128
                S.dma("sp", lambda e, b=b, blk=blk, r0=r0, nr=nr: e.dma_start(
                    out=yscr[r0 + blk * 128:r0 + blk * 128 + nr, :], in_=ysb[b][0:nr, blk, :]),
                    reads=[("ysb", b, blk, 0), ("ysb", b, blk, 1)], writes=[("yscr", u, blk)], slot="yst%d_%d" % (b, blk))
        S.barrier()
        A.release(m8)

        yg = [[A.alloc([D], F32) for _ in range(8)] for _ in range(2)]
        accA = [A.alloc([D], F32) for _ in range(2)]
        accB = [A.alloc([D], F32) for _ in range(2)]
        tmpk = [A.alloc([D], F32) for _ in range(2)]
        baser = [A.alloc([D], F32) for _ in range(2)]
        outb = [A.alloc([D], F32) for _ in range(2)]
        for b in range(2):
            for k in range(8):
                S.op("pool" if k % 2 else "dve", lambda e, b=b, k=k: e.memset(yg[b][k], 0.0), writes=[("yg", b, k, 0), ("yg", b, k, 1)])
        for t in range(NT):
            b = t % 2
            S.dma("sp", lambda e, t=t, b=b: e.dma_start(out=baser[b], in_=base_scr[t * 128:(t + 1) * 128, :]),
                  writes=[("baser", b)], slot="baser%d" % b)
            for k in range(8):
                S.dma("pool", lambda e, t=t, b=b, k=k: e.indirect_dma_start(
                    out=yg[b][k], out_offset=None, in_=yscr[:, :],
                    in_offset=bass.IndirectOffsetOnAxis(ap=idx[:, t, k:k + 1], axis=0)),
                    reads=[("yg", b, k, 0), ("yg", b, k, 1)], writes=[("yg", b, k, 0), ("yg", b, k, 1)], slot="ga%d_%d" % (b, k))
            for k in range(8):
                if k % 2 == 0:
                    if k == 0:
                        S.op("dve", lambda e, t=t, b=b, k=k: e.tensor_scalar_mul(accA[b], yg[b][k], gwk[:, t, k:k + 1]),
                             reads=[("yg", b, k, 0), ("yg", b, k, 1)], writes=[("accA", b)])
                    else:
                        S.op("dve", lambda e, t=t, b=b, k=k: e.scalar_tensor_tensor(
                            accA[b], yg[b][k], gwk[:, t, k:k + 1], accA[b], ALU.mult, ALU.add),
                            reads=[("yg", b, k, 0), ("yg", b, k, 1), ("accA", b)], writes=[("accA", b)])
                else:
                    dst = accB[b] if k == 1 else tmpk[b]
                    dk = ("accB", b) if k == 1 else ("tmpk", b)
                    S.op("act", lambda e, t=t, b=b, k=k, dst=dst: e.activation(dst, yg[b][k], AF.Copy, scale=gwk[:, t, k:k + 1]),
                         reads=[("yg", b, k, 0), ("yg", b, k, 1)], writes=[dk])
                    if k > 1:
                        S.op("dve", lambda e, b=b: e.tensor_tensor(accB[b], accB[b], tmpk[b], ALU.add),
                             reads=[("accB", b), ("tmpk", b)], writes=[("accB", b)])
            S.op("dve", lambda e, b=b: e.tensor_tensor(accA[b], accA[b], accB[b], ALU.add),
                 reads=[("accA", b), ("accB", b)], writes=[("accA", b)])
            S.op("dve", lambda e, b=b: e.tensor_tensor(accA[b], accA[b], ada[:, 5, :], ALU.mult),
                 reads=[("accA", b)], writes=[("accA", b)])
            S.op("dve", lambda e, b=b: e.tensor_tensor(outb[b], accA[b], baser[b], ALU.add),
                 reads=[("accA", b), ("baser", b)], writes=[("outb", b)])
            S.dma("sp", lambda e, t=t, b=b: e.dma_start(out=out[t * 128:(t + 1) * 128, :], in_=outb[b]),
                  reads=[("outb", b)], writes=[("out", t)], slot="ost%d" % b)
        st = S.emit()
        print("sched", st, "arena peak KiB", A.peak / 512.0)
    return nc


def _consts():
    n = np.arange(S_TOK, dtype=np.float64)
    ang = 2 * np.pi * np.outer(n, n) / float(S_TOK)
    dftc = np.cos(ang).astype(ml_dtypes.bfloat16)
    dfts = (-np.sin(ang)).astype(ml_dtypes.bfloat16)
    c = np.arange(64, dtype=np.float64)
    a64 = 2 * np.pi * np.outer(c, c) / 64.0
    d64 = np.zeros((128, 256), np.float64)
    for g in range(2):
        d64[g * 64:(g + 1) * 64, g * 64:(g + 1) * 64] = np.cos(a64)
        d64[g * 64:(g + 1) * 64, 128 + g * 64:128 + (g + 1) * 64] = np.sin(a64)
    pos = np.arange(S_TOK, dtype=np.float32)
    inv = (np.float32(10000.0) ** (-np.arange(0, 32, 2, dtype=np.float32) / np.float32(32))).astype(np.float32)
    a = pos[:, None] * inv[None, :]
    rope = np.concatenate([np.cos(a), np.sin(a)], axis=1).astype(np.float32)
    ident = np.eye(128).astype(ml_dtypes.bfloat16)
    tri = (np.arange(128)[:, None] < np.arange(128)[None, :]).astype(ml_dtypes.bfloat16)
    ones = np.ones((128, 128), ml_dtypes.bfloat16)
    iota1 = np.broadcast_to((np.arange(NE, dtype=np.float32) * C1 + ROW0)[None, :], (128, NE)).copy()
    iokp = (np.arange(8, dtype=np.float32)[None, :] * 128 + np.arange(128, dtype=np.float32)[:, None]).astype(np.float32)
    return dict(dftc=dftc, dfts=dfts, dft64=d64.astype(ml_dtypes.bfloat16), rope=rope, ident=ident,
                tri=tri, ones=ones, iota1=iota1, iokp=iokp)


_W_NAMES = ["w_ada", "b_ada", "norm1_g", "w_in", "q_a_norm_g", "w_uq", "kv_a_norm_g", "w_ukv", "q_norm_g",
            "k_norm_g", "w_proj_attn", "w_proj_fourier", "w_out", "norm2_g", "w_router", "router_bias",
            "w_exp_gate", "w_exp_up", "w_exp_down", "w_sh_gate", "w_sh_up", "w_sh_down"]


def kernel(**inputs):
    n_cores = 8
    nc = build("full")
    shared = dict(_consts())
    for k in _W_NAMES:
        a = np.asarray(inputs[k])[0]
        if a.ndim == 1:
            a = a[None, :]
        shared[k] = np.ascontiguousarray(a)
    x = np.asarray(inputs["x"])
    c = np.asarray(inputs["c"])
    in_maps = []
    for b in range(n_cores):
        m = dict(shared)
        m["x"] = np.ascontiguousarray(x[b])
        m["c8"] = np.ascontiguousarray(c[b].reshape(8, 128).T)
        in_maps.append(m)
    res = run_bass_kernel_spmd(nc, in_maps, core_ids=list(range(n_cores)))
    return np.stack([np.asarray(r["out"]) for r in res.results], axis=0).astype(np.float32)
```
